# Optimizing a Trainium2 kernel written in Bass

```python
import jax
import jax.numpy as jnp
from jax import lax
import numpy as np

D_MODEL = 1024
BATCH = 8
SEQ = 2048
DEPTH = 4

GRID_W = 64
CTX_LEN = 256
EPS = 1e-6
N_MOD = 6

A_HEADS = 4
A_HEAD_DIM = 128
A_CHUNK = 128
A_WIDTH = A_HEADS * A_HEAD_DIM

B_HEADS = 4
B_DK = 64
B_DV = 128
B_QK = B_HEADS * B_DK
B_V = B_HEADS * B_DV
B_GATE_RANK = 16
B_GATE_TEMP = 16.0
B_CHUNK = 64
B_COLS = 2 * B_QK + 2 * B_V + 2 * B_GATE_RANK

EVEN_IN = 2 * A_WIDTH + B_COLS
EVEN_OUT = A_WIDTH + B_V

C_HEADS = 16
C_NOPE = 64
C_ROPE = 32
C_VDIM = 64
C_Q_RANK = 256
C_KV_RANK = 256
ODD_SPLITS = (C_Q_RANK, C_KV_RANK, C_ROPE)
ODD_IN = C_Q_RANK + C_KV_RANK + C_ROPE
C_OUT = C_HEADS * C_VDIM
C_SCALE = (C_NOPE + C_ROPE) ** -0.5
Q_BLOCK = 128
ROPE_BASE = 10000.0

FF_HIDDEN = 2816
N_EXPERTS = 8
TOP_K = 2
MOE_HIDDEN = 3584

N_EVEN = (DEPTH + 1) // 2
N_ODD = DEPTH // 2

kernel_name = 'hybrid_dit_gmlp_gla_mla_moe'


def split_cols(p, sizes):
    return jnp.split(p, np.cumsum(sizes)[:-1].tolist(), axis=-1)


def rms_norm(x, g):
    xf = x.astype(jnp.float32)
    y = xf * lax.rsqrt(jnp.mean(xf * xf, axis=-1, keepdims=True) + EPS)
    return (y * g.astype(jnp.float32)).astype(x.dtype)


def layer_norm(x, g, b):
    xf = x.astype(jnp.float32)
    mu = jnp.mean(xf, axis=-1, keepdims=True)
    var = jnp.mean(jnp.square(xf - mu), axis=-1, keepdims=True)
    y = (xf - mu) * lax.rsqrt(var + EPS)
    return (y * g.astype(jnp.float32) + b.astype(jnp.float32)).astype(x.dtype)


def modulate(h, shift, scale):
    return h * (1 + scale) + shift


def axial_rope_angles(grid_rows):
    half = C_ROPE // 2
    inv_freq = ROPE_BASE ** (-jnp.arange(0, half, 2, dtype=jnp.float32) / half)
    rows = jnp.repeat(jnp.arange(grid_rows, dtype=jnp.float32), GRID_W)
    cols = jnp.tile(jnp.arange(GRID_W, dtype=jnp.float32), grid_rows)
    return rows[:, None] * inv_freq, cols[:, None] * inv_freq


def rope_rotate(x, ang):
    x1, x2 = jnp.split(x, 2, axis=-1)
    cos = jnp.cos(ang).astype(x.dtype)
    sin = jnp.sin(ang).astype(x.dtype)
    return jnp.concatenate([x1 * cos - x2 * sin, x2 * cos + x1 * sin], axis=-1)


def axial_rope(x, ang_r, ang_c):
    shape = (x.shape[1],) + (1,) * (x.ndim - 3) + (ang_r.shape[-1],)
    xr, xc = jnp.split(x, 2, axis=-1)
    return jnp.concatenate([rope_rotate(xr, ang_r.reshape(shape)),
                            rope_rotate(xc, ang_c.reshape(shape))], axis=-1)


def chunk_mlp(zu, zv, ln_g, ln_b, ws, bs):
    bsz, L, _ = zu.shape
    u = jax.nn.gelu(zu)
    v = layer_norm(jax.nn.gelu(zv), ln_g, ln_b)
    v = v.reshape(bsz, L // A_CHUNK, A_CHUNK, A_HEADS, A_HEAD_DIM)
    mixed = jnp.einsum('hpq,bnqhd->bnphd', ws, v) + bs.T[None, None, :, :, None]
    return u * mixed.reshape(bsz, L, A_WIDTH)


def gla_scan(q, k, v, g, s0, need_out):
    bsz, L, H, _ = q.shape
    n = L // B_CHUNK

    def chunks(t):
        return t.reshape(bsz, n, B_CHUNK, H, t.shape[-1])

    q, k, v, g = chunks(q), chunks(k), chunks(v), chunks(g)
    b = jnp.cumsum(g, axis=2)
    b_last = b[:, :, -1:]
    chunk_states = jnp.einsum('bnjhk,bnjhv->bnhkv', k * jnp.exp(b_last - b), v)
    decay = jnp.exp(b_last[:, :, 0])

    def step(s, inp):
        dec, add = inp
        return s * dec[..., None] + add, (s if need_out else None)

    s_final, s_prev = lax.scan(step, s0, (jnp.moveaxis(decay, 1, 0), jnp.moveaxis(chunk_states, 1, 0)))
    if not need_out:
        return None, s_final
    q_dec = q * jnp.exp(b)
    k_inv = k * jnp.exp(-b)
    scores = jnp.einsum('bnihk,bnjhk->bnhij', q_dec, k_inv)
    scores = jnp.where(jnp.tril(jnp.ones((B_CHUNK, B_CHUNK), dtype=bool)), scores, 0.0)
    o_intra = jnp.einsum('bnhij,bnjhv->bnihv', scores, v)
    o_inter = jnp.einsum('bnihk,bnhkv->bnihv', q_dec, jnp.moveaxis(s_prev, 0, 1))
    return (o_intra + o_inter).reshape(bsz, L, H, v.shape[-1]), s_final


def gla_mixer(pc, pl, gate_w, gate_b, norm_g, need_ctx):
    def prep(p):
        bsz, L, _ = p.shape
        q, k, v, r, gf = split_cols(p, (B_QK, B_QK, B_V, B_V, 2 * B_GATE_RANK))
        gf = gf.reshape(bsz, L, 2, B_GATE_RANK)
        logit = jnp.einsum('bldr,drk->bldk', gf, gate_w) + gate_b
        g = jax.nn.log_sigmoid(logit.astype(jnp.float32)) / B_GATE_TEMP
        g = g.reshape(bsz, L, 2, B_HEADS, B_DK)
        q = q.astype(jnp.float32).reshape(bsz, L, B_HEADS, B_DK) * (B_DK ** -0.5)
        k = k.astype(jnp.float32).reshape(bsz, L, B_HEADS, B_DK)
        v = v.astype(jnp.float32).reshape(bsz, L, B_HEADS, B_DV)
        return q, k, v, r, g[:, :, 0], g[:, :, 1]

    def flip(t):
        return jnp.flip(t, axis=1)

    qc, kc, vc, rc, gc_f, gc_b = prep(pc)
    ql, kl, vl, rl, gl_f, gl_b = prep(pl)
    s0 = jnp.zeros((pl.shape[0], B_HEADS, B_DK, B_DV), jnp.float32)
    oc_f, sc_f = gla_scan(qc, kc, vc, gc_f, s0, need_ctx)
    oc_b, sc_b = gla_scan(flip(qc), flip(kc), flip(vc), flip(gc_b), s0, need_ctx)
    ol_f, _ = gla_scan(ql, kl, vl, gl_f, sc_f, True)
    ol_b, _ = gla_scan(flip(ql), flip(kl), flip(vl), flip(gl_b), sc_b, True)

    def finish(o, r):
        bsz, L = o.shape[:2]
        o = rms_norm(o, norm_g).reshape(bsz, L, B_V).astype(r.dtype)
        return jax.nn.silu(r) * o

    y_l = finish(ol_f + flip(ol_b), rl)
    y_c = finish(oc_f + flip(oc_b), rc) if need_ctx else None
    return y_c, y_l


def even_mixer(nc, nl, w_in, a_ln_g, a_ln_b, a_ws, a_bs, b_gate_w, b_gate_b, b_norm_g, w_out, need_ctx):
    ua_c, va_c, pb_c = split_cols(nc @ w_in, (A_WIDTH, A_WIDTH, B_COLS))
    ua_l, va_l, pb_l = split_cols(nl @ w_in, (A_WIDTH, A_WIDTH, B_COLS))
    yb_c, yb_l = gla_mixer(pb_c, pb_l, b_gate_w, b_gate_b, b_norm_g, need_ctx)
    ya_l = chunk_mlp(ua_l, va_l, a_ln_g, a_ln_b, a_ws, a_bs)
    y_l = jnp.concatenate([ya_l, yb_l], axis=-1) @ w_out
    if not need_ctx:
        return None, y_l
    ya_c = chunk_mlp(ua_c, va_c, a_ln_g, a_ln_b, a_ws, a_bs)
    y_c = jnp.concatenate([ya_c, yb_c], axis=-1) @ w_out
    return y_c, y_l


def mla_project(n, w_in, q_norm_g, w_uq, kv_norm_g, w_ukv):
    bsz, L, _ = n.shape
    cq, ckv, k_rope = split_cols(n @ w_in, ODD_SPLITS)
    q = (rms_norm(cq, q_norm_g) @ w_uq).reshape(bsz, L, C_HEADS, C_NOPE + C_ROPE)
    kv = (rms_norm(ckv, kv_norm_g) @ w_ukv).reshape(bsz, L, C_HEADS, C_NOPE + C_VDIM)
    return q[..., :C_NOPE], q[..., C_NOPE:], kv[..., :C_NOPE], k_rope, kv[..., C_NOPE:]


def mla_attend(q_nope, q_rope, k_nope, k_rope, v):
    s = (jnp.einsum('bqhd,bkhd->bhqk', q_nope, k_nope)
         + jnp.einsum('bqhr,bkr->bhqk', q_rope, k_rope))
    p = jax.nn.softmax(s.astype(jnp.float32) * C_SCALE, axis=-1).astype(v.dtype)
    return jnp.einsum('bhqk,bkhd->bqhd', p, v)


def mla_mixer(nc, nl, w_in, q_norm_g, w_uq, kv_norm_g, w_ukv, w_out, ang_r, ang_c, need_ctx):
    bsz, L, _ = nl.shape
    qn_c, qr_c, kn_c, kr_c, v_c = mla_project(nc, w_in, q_norm_g, w_uq, kv_norm_g, w_ukv)
    qn_l, qr_l, kn_l, kr_l, v_l = mla_project(nl, w_in, q_norm_g, w_uq, kv_norm_g, w_ukv)
    qr_l = axial_rope(qr_l, ang_r, ang_c)
    kr_l = axial_rope(kr_l, ang_r, ang_c)
    k_nope = jnp.concatenate([kn_c, kn_l], axis=1)
    k_rope = jnp.concatenate([kr_c, kr_l], axis=1)
    v = jnp.concatenate([v_c, v_l], axis=1)
    n_blocks = L // Q_BLOCK

    def to_blocks(t):
        return jnp.moveaxis(t.reshape((bsz, n_blocks, Q_BLOCK) + t.shape[2:]), 1, 0)

    o_l = lax.map(lambda qb: mla_attend(qb[0], qb[1], k_nope, k_rope, v),
                  (to_blocks(qn_l), to_blocks(qr_l)))
    y_l = jnp.moveaxis(o_l, 0, 1).reshape(bsz, L, C_OUT) @ w_out
    if not need_ctx:
        return None, y_l
    o_c = mla_attend(qn_c, qr_c, kn_c, kr_c, v_c)
    y_c = o_c.reshape(bsz, nc.shape[1], C_OUT) @ w_out
    return y_c, y_l


def swiglu(h, w_gate, w_up, w_down):
    return (jax.nn.silu(h @ w_gate) * (h @ w_up)) @ w_down


def moe_swiglu(h, router, w_gate, w_up, w_down):
    logits = (h @ router).astype(jnp.float32)
    top_val, top_idx = lax.top_k(logits, TOP_K)
    top_w = jax.nn.softmax(top_val, axis=-1)
    gates = jnp.einsum('...k,...ke->...e', top_w,
                       jax.nn.one_hot(top_idx, N_EXPERTS, dtype=jnp.float32)).astype(h.dtype)
    y = jnp.zeros_like(h)
    for e in range(N_EXPERTS):
        y = y + gates[..., e:e + 1] * swiglu(h, w_gate[e], w_up[e], w_down[e])
    return y


def setup_inputs(seed: int = 0) -> dict:
    key = jax.random.key(seed)
    keys = iter(jax.random.split(key, 32))
    D = D_MODEL

    def normal(shape, scale):
        return jax.random.normal(next(keys), shape, jnp.float32) * scale

    def gain(shape):
        return 1.0 + normal(shape, 0.02)

    return {
        'x': normal((BATCH, SEQ, D), 1.0),
        'c': normal((BATCH, D), 1.0),
        'ctx': normal((BATCH, CTX_LEN, D), 1.0),
        'c_ctx': normal((D,), 1.0),
        'mod_w': normal((DEPTH, D, N_MOD * D), 0.5 * D ** -0.5),
        'mod_b': normal((DEPTH, N_MOD * D), 0.02),
        'norm_mix_g': gain((DEPTH, D)),
        'norm_ffn_g': gain((DEPTH, D)),
        'final_g': gain((D,)),
        'ev_w_in': normal((N_EVEN, D, EVEN_IN), D ** -0.5),
        'a_ln_g': gain((N_EVEN, A_WIDTH)),
        'a_ln_b': normal((N_EVEN, A_WIDTH), 0.02),
        'a_ws': normal((N_EVEN, A_HEADS, A_CHUNK, A_CHUNK), A_CHUNK ** -0.5),
        'a_bs': gain((N_EVEN, A_HEADS, A_CHUNK)),
        'b_gate_w': normal((N_EVEN, 2, B_GATE_RANK, B_QK), B_GATE_RANK ** -0.5),
        'b_gate_b': normal((N_EVEN, 2, B_QK), 0.1),
        'b_norm_g': gain((N_EVEN, B_DV)),
        'ev_w_out': normal((N_EVEN, EVEN_OUT, D), EVEN_OUT ** -0.5),
        'od_w_in': normal((N_ODD, D, ODD_IN), D ** -0.5),
        'c_q_norm_g': gain((N_ODD, C_Q_RANK)),
        'c_w_uq': normal((N_ODD, C_Q_RANK, C_HEADS * (C_NOPE + C_ROPE)), C_Q_RANK ** -0.5),
        'c_kv_norm_g': gain((N_ODD, C_KV_RANK)),
        'c_w_ukv': normal((N_ODD, C_KV_RANK, C_HEADS * (C_NOPE + C_VDIM)), C_KV_RANK ** -0.5),
        'od_w_out': normal((N_ODD, C_OUT, D), C_OUT ** -0.5),
        'ff_w_gate': normal((N_EVEN, D, FF_HIDDEN), D ** -0.5),
        'ff_w_up': normal((N_EVEN, D, FF_HIDDEN), D ** -0.5),
        'ff_w_down': normal((N_EVEN, FF_HIDDEN, D), FF_HIDDEN ** -0.5),
        'moe_router': normal((N_ODD, D, N_EXPERTS), D ** -0.5),
        'moe_w_gate': normal((N_ODD, N_EXPERTS, D, MOE_HIDDEN), D ** -0.5),
        'moe_w_up': normal((N_ODD, N_EXPERTS, D, MOE_HIDDEN), D ** -0.5),
        'moe_w_down': normal((N_ODD, N_EXPERTS, MOE_HIDDEN, D), MOE_HIDDEN ** -0.5),
    }


def reference(x, c, ctx, c_ctx, mod_w, mod_b, norm_mix_g, norm_ffn_g, final_g,
              ev_w_in, a_ln_g, a_ln_b, a_ws, a_bs, b_gate_w, b_gate_b, b_norm_g, ev_w_out,
              od_w_in, c_q_norm_g, c_w_uq, c_kv_norm_g, c_w_ukv, od_w_out,
              ff_w_gate, ff_w_up, ff_w_down,
              moe_router, moe_w_gate, moe_w_up, moe_w_down):
    seq = x.shape[1]
    grid_rows = seq // GRID_W
    ang_r, ang_c = axial_rope_angles(grid_rows)
    cond_l = jax.nn.silu(c)[:, None, :]
    cond_c = jax.nn.silu(c_ctx)[None, None, :]
    h_l, h_c = x, ctx
    for layer in range(DEPTH):
        need_ctx = layer < DEPTH - 1
        i = layer // 2
        sh1_l, sc1_l, g1_l, sh2_l, sc2_l, g2_l = jnp.split(cond_l @ mod_w[layer] + mod_b[layer], N_MOD, axis=-1)
        sh1_c, sc1_c, g1_c, sh2_c, sc2_c, g2_c = jnp.split(cond_c @ mod_w[layer] + mod_b[layer], N_MOD, axis=-1)

        n_l = modulate(rms_norm(h_l, norm_mix_g[layer]), sh1_l, sc1_l)
        n_c = modulate(rms_norm(h_c, norm_mix_g[layer]), sh1_c, sc1_c)
        if layer % 2 == 0:
            y_c, y_l = even_mixer(n_c, n_l, ev_w_in[i], a_ln_g[i], a_ln_b[i], a_ws[i], a_bs[i],
                                  b_gate_w[i], b_gate_b[i], b_norm_g[i], ev_w_out[i], need_ctx)
        else:
            y_c, y_l = mla_mixer(n_c, n_l, od_w_in[i], c_q_norm_g[i], c_w_uq[i], c_kv_norm_g[i],
                                 c_w_ukv[i], od_w_out[i], ang_r, ang_c, need_ctx)
        h_l = h_l + g1_l * y_l

        m_l = modulate(rms_norm(h_l, norm_ffn_g[layer]), sh2_l, sc2_l)
        if need_ctx:
            h_c = h_c + g1_c * y_c
            m_c = modulate(rms_norm(h_c, norm_ffn_g[layer]), sh2_c, sc2_c)
            m = jnp.concatenate([m_c, m_l], axis=1)
        else:
            m = m_l
        if layer % 2 == 0:
            f = swiglu(m, ff_w_gate[i], ff_w_up[i], ff_w_down[i])
        else:
            f = moe_swiglu(m, moe_router[i], moe_w_gate[i], moe_w_up[i], moe_w_down[i])
        if need_ctx:
            n_ctx = h_c.shape[1]
            h_c = h_c + g2_c * f[:, :n_ctx]
            h_l = h_l + g2_l * f[:, n_ctx:]
        else:
            h_l = h_l + g2_l * f
    return rms_norm(h_l, final_g)
```

```python
import numpy as np
from contextlib import ExitStack
import concourse.bass as bass
import concourse.mybir as mybir
from concourse.alu_op_type import AluOpType as ALU
from concourse.bass_utils import run_bass_kernel_spmd

AF = mybir.ActivationFunctionType
F32 = mybir.dt.float32
BF16 = mybir.dt.bfloat16
PE, ACT, DVE, POOL, SP = "tensor", "scalar", "vector", "gpsimd", "sync"
ENGS = [PE, ACT, DVE, POOL, SP]
NDS = 8

D = 1024
NT = 18
T = 2304
EPS = 1e-6
FF_HIDDEN = 2816
MOE_HIDDEN = 3584
C_SCALE = 96 ** -0.5


class Op:
    __slots__ = ("eng", "fn", "deps", "need", "dma", "sig", "waits")

    def __init__(self, eng, fn, dma):
        self.eng, self.fn, self.dma = eng, fn, dma
        self.deps, self.need, self.sig, self.waits = [], False, None, []


class Ctx:
    def __init__(self, nc, es):
        self.nc = nc
        self.esem = {e: es.enter_context(nc.semaphore("s_" + e)) for e in ENGS}
        self.dsem = {e: [es.enter_context(nc.semaphore("d_%s_%d" % (e, i))) for i in range(NDS)] for e in (SP, POOL)}
        self.ecnt = {e: 0 for e in ENGS}
        self.dcnt = {e: 0 for e in self.dsem}
        self.waited = {e: {} for e in ENGS}
        self.nphase = 0


class Phase:
    def __init__(self, ctx):
        self.ctx = ctx
        self.nc = ctx.nc
        self.ops = []
        self.last_w = {}
        self.readers = {}
        self.es = ExitStack()
        self.limit = None

    def sb(self, name, shape, dt):
        return self.es.enter_context(self.nc.sbuf_tensor("%s_p%d" % (name, self.ctx.nphase), list(shape), dt))

    def add(self, eng, fn, R=(), W=(), dma=False):
        if self.limit is not None and len(self.ops) >= self.limit:
            return None
        op = Op(eng, fn, dma)
        deps = {}
        for k in R:
            w = self.last_w.get(k)
            if w is not None:
                deps[id(w)] = (w, True)
            if k.startswith("ps"):
                for r in self.readers.get(k, ()):
                    if r.eng != eng and id(r) not in deps:
                        deps[id(r)] = (r, False)
        for k in W:
            w = self.last_w.get(k)
            if w is not None and id(w) not in deps:
                deps[id(w)] = (w, False)
            for r in self.readers.get(k, ()):
                if id(r) not in deps:
                    deps[id(r)] = (r, False)
        for d, raw in deps.values():
            if (not d.dma) and (not dma) and d.eng == eng:
                if eng == PE:
                    continue
            op.deps.append(d)
            d.need = True
        for k in W:
            self.last_w[k] = op
            self.readers[k] = []
        for k in R:
            if k not in W:
                self.readers.setdefault(k, []).append(op)
        self.ops.append(op)
        return op

    def emit(self):
        c = self.ctx
        nc = self.nc
        per_eng = {e: [] for e in ENGS}
        for op in self.ops:
            pre = None
            if op.dma:
                k = c.dcnt[op.eng]
                c.dcnt[op.eng] += 1
                s = c.dsem[op.eng][k % NDS]
                op.sig = (s, 16 * (k // NDS + 1))
                if k >= NDS:
                    pre = (s, 16 * (k // NDS))
            elif op.need:
                c.ecnt[op.eng] += 1
                op.sig = (c.esem[op.eng], c.ecnt[op.eng])
            w = c.waited[op.eng]
            lst = [pre] if pre is not None else []
            lst += [d.sig for d in op.deps]
            for s, v in lst:
                if w.get(id(s), 0) >= v:
                    continue
                w[id(s)] = v
                op.waits.append((s, v))
            per_eng[op.eng].append(op)
        drains = {e: [] for e in ENGS}
        for e in c.dsem:
            for i, s in enumerate(c.dsem[e]):
                n = (c.dcnt[e] - i + NDS - 1) // NDS if c.dcnt[e] > i else 0
                if n > 0 and c.waited[e].get(id(s), 0) < 16 * n:
                    c.waited[e][id(s)] = 16 * n
                    drains[e].append((s, 16 * n))

        def run(en):
            def body(eng):
                for op in per_eng[en]:
                    for s, v in op.waits:
                        eng.wait_ge(s, v)
                    ins = op.fn(eng)
                    if op.sig is not None:
                        ins.then_inc(op.sig[0], 16 if op.dma else 1)
                for s, v in drains[en]:
                    eng.wait_ge(s, v)
            return body

        with nc.Block() as block:
            block.tensor(run(PE))
            block.scalar(run(ACT))
            block.vector(run(DVE))
            block.gpsimd(run(POOL))
            block.sync(run(SP))
        self.es.close()
        c.nphase += 1


def mm(ph, out, lhsT, rhs, start, stop, R, W):
    return ph.add(PE, lambda e: e.matmul(out, lhsT=lhsT, rhs=rhs, start=start, stop=stop), R, W)


def tr(ph, out, in_, ident, R, W):
    return ph.add(PE, lambda e: e.transpose(out=out, in_=in_, identity=ident), R, W)


def act(ph, out, in_, func, R, W, **kw):
    return ph.add(ACT, lambda e: e.activation(out=out, in_=in_, func=func, **kw), R, W)


def tt(ph, out, a, b, op, R, W, eng=DVE):
    return ph.add(eng, lambda e: e.tensor_tensor(out=out, in0=a, in1=b, op=op), R, W)


def ts(ph, out, a, s1, s2, op0, op1, R, W, eng=DVE):
    if s2 is None:
        return ph.add(eng, lambda e: e.tensor_scalar(out=out, in0=a, scalar1=s1, scalar2=None, op0=op0), R, W)
    return ph.add(eng, lambda e: e.tensor_scalar(out=out, in0=a, scalar1=s1, scalar2=s2, op0=op0, op1=op1), R, W)


def stt(ph, out, a, s, b, op0, op1, R, W, accum=None):
    if accum is None:
        return ph.add(DVE, lambda e: e.scalar_tensor_tensor(out=out, in0=a, scalar=s, in1=b, op0=op0, op1=op1), R, W)
    return ph.add(DVE, lambda e: e.scalar_tensor_tensor(out=out, in0=a, scalar=s, in1=b, op0=op0, op1=op1, accum_out=accum), R, W)


def cp(ph, out, in_, R, W, eng=DVE):
    return ph.add(eng, lambda e: e.tensor_copy(out=out, in_=in_), R, W)


def ms(ph, ap, val, W, eng=DVE):
    return ph.add(eng, lambda e: e.memset(ap, val), (), W)


def dma(ph, out, in_, R, W, eng=SP):
    return ph.add(eng, lambda e: e.dma_start(out=out, in_=in_), R, W, dma=True)


def v4(ap):
    return ap.rearrange("p (a b) -> p a b", b=128)


class K:
    pass


def gelu_tanh(ph, x, out, tmp, tk, R, W):
    tt(ph, tmp, x, x, ALU.mult, R, [tk], eng=POOL)
    ts(ph, tmp, tmp, 0.044715 * 1.5957691216057308, 1.5957691216057308, ALU.mult, ALU.add, [tk], [tk])
    tt(ph, tmp, tmp, x, ALU.mult, [tk] + list(R), [tk], eng=POOL)
    act(ph, tmp, tmp, AF.Sigmoid, [tk], [tk])
    tt(ph, out, tmp, x, ALU.mult, [tk] + list(R), W)


def norm_tile(k, ph, nb, t, layer, Amod, SHmod, nT_out, nT_key, extra_f32=None, mixed=False, prefetched=False):
    s = t % len(nb["ht"])
    ht = nb["ht"][s]
    hk = "ht%d" % s
    if not prefetched:
        dma(ph, ht[:], k.hsrc(layer, t, mixed), [], [hk])
    st = nb["stat"]
    sk = "stat"
    act(ph, nb["sq"][:], ht[:], AF.Square, [hk], ["sq", sk], accum_out=st[:, 0:1])
    act(ph, st[:, 1:2], st[:, 0:1], AF.Ln, [sk], [sk], scale=1.0 / D, bias=k.eps_t[:, 0:1])
    act(ph, st[:, 2:3], st[:, 1:2], AF.Exp, [sk], [sk], scale=-0.5)
    stt(ph, nb["tmp"][:], ht[:], st[:, 2:3], Amod, ALU.mult, ALU.mult, [hk, sk, "MOD"], ["tmp"])
    tt(ph, nb["nrm"][:], nb["tmp"][:], SHmod, ALU.add, ["tmp", "MOD"], ["nrm"])
    for j in range(8):
        b = j // 4
        tr(ph, k.ps[b][:, (j % 4) * 128:(j % 4 + 1) * 128], nb["nrm"][:, j * 128:(j + 1) * 128], k.ident, ["nrm"], ["ps%d" % b])
    act(ph, nT_out[:, 0:4, :], v4(k.ps[0][:, :]), AF.Copy, ["ps0"], [nT_key])
    cp(ph, nT_out[:, 4:8, :], v4(k.ps[1][:, :]), ["ps1"], [nT_key])
    if extra_f32 is not None:
        cp(ph, extra_f32[0][:, 0:4, :], v4(k.ps[0][:, :]), ["ps0"], [extra_f32[1]])
        act(ph, extra_f32[0][:, 4:8, :], v4(k.ps[1][:, :]), AF.Copy, ["ps1"], [extra_f32[1]])
    return hk


def norm_bufs(ph):
    return {
        "ht": [ph.sb("ht0", [128, D], F32), ph.sb("ht1", [128, D], F32)],
        "sq": ph.sb("sq", [128, D], BF16),
        "stat": ph.sb("stat", [128, 4], F32),
        "tmp": ph.sb("tmp", [128, D], F32),
        "nrm": ph.sb("nrm", [128, D], F32),
    }


def phase_setup(k):
    ph = Phase(k.ctx)
    dma(ph, k.cst[:], k.d["cst"][:, :], [], ["cst"])
    ms(ph, k.eps_t[:], EPS, ["eps"])
    craw = ph.sb("craw", [8, 2, 128], F32)
    dma(ph, craw[:, 0, :], k.d["c"].rearrange("(k p) -> k p", p=128), [], ["craw"])
    dma(ph, craw[:, 1, :], k.d["c_ctx"].rearrange("(k p) -> k p", p=128), [], ["craw"])
    cT = ph.sb("cT", [128, 2, 8], F32)
    for lc in range(2):
        tr(ph, k.ps[0][:, lc * 8:(lc + 1) * 8], craw[:, lc, :], k.cst[0:8, 0:8], ["craw", "cst"], ["ps0"])
    act(ph, cT[:].rearrange("p a b -> p (a b)"), k.ps[0][:, 0:16], AF.Silu, ["ps0"], ["cT"])
    for lc in range(2):
        for j in range(8):
            act(ph, k.condrep[:, lc, j, :], k.ones, AF.Copy, ["cT", "cst"], ["condrep"], scale=cT[:, lc, j:j + 1])
    ph.emit()


def phase_mod(k, layer, half):
    ph = Phase(k.ctx)
    wm = [ph.sb("wm0", [128, 8, 512], BF16), ph.sb("wm1", [128, 8, 512], BF16)]
    mb = [ph.sb("mb0", [128, 512], F32), ph.sb("mb1", [128, 512], F32)]
    gb = ph.sb("gb", [128, D], F32)
    gsrc = k.d["norm_mix_g"] if half == 0 else k.d["norm_ffn_g"]
    wsrc = k.d["mod_w"][layer].rearrange("(k p) n -> p k n", p=128)
    for blk in range(6):
        c0 = half * 3072 + blk * 512
        s = blk % 2
        dma(ph, wm[s][:], wsrc[:, :, c0:c0 + 512], [], ["wm%d" % s], eng=POOL)
        if blk == 0:
            dma(ph, gb[:], gsrc[layer].partition_broadcast(128), [], ["gb"])
        dma(ph, mb[s][:], k.d["mod_b"][layer, c0:c0 + 512].partition_broadcast(128), [], ["mb%d" % s])
        for lc in range(2):
            b = (blk * 2 + lc) % 4
            for j in range(8):
                mm(ph, k.ps[b][:, :], k.condrep[:, lc, j, :], wm[s][:, j, :], j == 0, j == 7, ["wm%d" % s], ["ps%d" % b])
            tt(ph, k.MOD[:, lc, blk // 2, (blk % 2) * 512:(blk % 2 + 1) * 512], k.ps[b][:, :], mb[s][:], ALU.add,
               ["ps%d" % b, "mb%d" % s], ["MOD"])
    for lc in range(2):
        stt(ph, k.MOD[:, lc, 1, :], k.MOD[:, lc, 1, :], 1.0, gb[:], ALU.add, ALU.mult, ["MOD", "gb"], ["MOD"])
    ph.emit()


def phase_even(k, layer, i, direction):
    full = direction == 0
    d = direction
    ph = Phase(k.ctx)
    import os
    if os.environ.get("KCUT"):
        ph.limit = int(os.environ["KCUT"])
    ps = k.ps
    nb = norm_bufs(ph)
    nb["ht"].append(ph.sb("ht2", [128, D], F32))
    win = k.win
    if not full:
        wsrc = k.d["ev_w_in"][i].rearrange("(k p) n -> p k n", p=128)
        dma(ph, win[:, :, 1024:2048], wsrc[:, :, 1024:2048], [], ["win"], eng=POOL)
        dma(ph, win[:, :, 2560:2592], wsrc[:, :, 2560:2592], [], ["win"], eng=POOL)
        dma(ph, win[:, :, 0:1024], wsrc[:, :, 0:1024], [], ["win2"], eng=POOL)
        dma(ph, win[:, :, 2048:2560], wsrc[:, :, 2048:2560], [], ["win2"], eng=POOL)
    gateW = ph.sb("gateW", [33, 512], F32)
    ms(ph, gateW[:], 0.0, ["gateW"])
    for dd in range(2):
        dma(ph, gateW[dd * 16:(dd + 1) * 16, dd * 256:(dd + 1) * 256], k.d["b_gate_w"][i, dd], [], ["gateW"])
    dma(ph, gateW[32:33, :], k.d["b_gate_b"][i].rearrange("a b -> (a b)").rearrange("(o n) -> o n", o=1), [], ["gateW"])
    gfa = ph.sb("gfa", [33, 128], F32)
    ms(ph, gfa[32:33, :], 1.0, ["gfa1"])
    S32 = ph.sb("S32", [128, 2, 256], F32)
    Sbf = ph.sb("Sbf", [128, 2, 256], BF16)
    ms(ph, S32[:], 0.0, ["S32"])
    ms(ph, Sbf[:], 0.0, ["Sbf"])
    nT = [ph.sb("nT0", [128, 8, 128], BF16), ph.sb("nT1", [128, 8, 128], BF16)]
    spt = ph.sb("spt", [128, 256], F32)
    Et = ph.sb("Et", [128, 256], F32)
    Eit = ph.sb("Eit", [128, 256], F32)
    ERt = ph.sb("ERt", [128, 256], F32)
    qdTm = ph.sb("qdTm", [128, 2, 2, 128], BF16)
    ms(ph, qdTm[:], 0.0, ["qdT"])
    kiT = ph.sb("kiT", [128, 256], BF16)
    krt = ph.sb("krt", [128, 256], BF16)
    vb = ph.sb("vb", [128, 512], BF16)
    sTt = ph.sb("sTt", [128, 512], BF16)
    if full:
        wout = ph.sb("wout", [128, 8, D], BF16)
        dma(ph, wout[:], k.d["ev_w_out"][i].rearrange("(k p) n -> p k n", p=128), [], ["wout"], eng=POOL)
        wsr = ph.sb("wsr", [128, 4, 128], F32)
        dma(ph, wsr[:], k.d["a_ws"][i].rearrange("h p q -> p h q"), [], ["wsr"])
        wsT = ph.sb("wsT", [128, 4, 128], BF16)
        for h in range(4):
            tr(ph, ps[2][:, h * 128:(h + 1) * 128], wsr[:, h, :], k.ident, ["wsr"], ["ps2"])
        cp(ph, wsT[:].rearrange("p a b -> p (a b)"), ps[2][:, :], ["ps2"], ["wsT"])
        bsB = ph.sb("bsB", [128, 512], F32)
        dma(ph, bsB[:], k.d["a_bs"][i].rearrange("a b -> (a b)").partition_broadcast(128), [], ["bsB"])
        lng = ph.sb("lng", [128, 512], F32)
        lnb = ph.sb("lnb", [128, 512], F32)
        dma(ph, lng[:], k.d["a_ln_g"][i].partition_broadcast(128), [], ["lng"])
        dma(ph, lnb[:], k.d["a_ln_b"][i].partition_broadcast(128), [], ["lnb"])
        bng = ph.sb("bng", [128, 4, 128], F32)
        for h in range(4):
            dma(ph, bng[:, h, :], k.d["b_norm_g"][i].partition_broadcast(128), [], ["bng"])
        xa = ph.sb("xa", [128, 512], F32)
        xt1 = ph.sb("xt1", [128, 512], F32)
        gv = ph.sb("gv", [128, 512], F32)
        bst = ph.sb("bst", [128, 8], F32)
        ost = ph.sb("ost", [128, 12], F32)
        ybt = ph.sb("ybt", [128, 512], F32)
        yT = ph.sb("yT", [128, 8, 128], BF16)
        ho = ph.sb("ho", [128, D], F32)
        osq = ph.sb("osq", [128, 128], BF16)
    TRI = k.TRI[d]
    STRI = k.STRI[d]
    MASK = k.MASK4[d]
    dcol = 127 if d == 0 else 0
    order = list(range(NT)) if d == 0 else [1, 0] + list(range(17, 1, -1))
    if full:
        xa2 = ph.sb("xa2", [128, 512], F32)
        sgr = ph.sb("sgr", [128, 512], F32)

    def rstd_ln_exp(dst, src, n, scale, R, W):
        act(ph, dst, src, AF.Ln, R, W, scale=scale, bias=k.eps_t[:, 0:1])
        act(ph, dst, dst, AF.Exp, W, W, scale=-0.5)

    if full:
        osum2 = [ph.sb("osum_a", [128, 512], F32), ph.sb("osum_b", [128, 512], F32)]
        srg2 = [ph.sb("srg_a", [128, 512], F32), ph.sb("srg_b", [128, 512], F32)]
        uT2 = [ph.sb("uT_a", [128, 512], F32), ph.sb("uT_b", [128, 512], F32)]
        vA2 = [ph.sb("vA_a", [128, 512], BF16), ph.sb("vA_b", [128, 512], BF16)]
        xt2 = ph.sb("xt2", [128, 512], F32)

    def tail(tp):
        o_ = tp % 2
        lcp = 1 if tp < 2 else 0
        for h in range(4):
            mm(ph, ps[3][:, h * 128:(h + 1) * 128], vA2[o_][:, h * 128:(h + 1) * 128], wsT[:, h, :], True, True,
               ["vA%d" % o_, "wsT"], ["ps3"])
        tt(ph, xt2[:], ps[3][:, :], bsB[:], ALU.add, ["ps3", "bsB"], ["xt2"])
        tt(ph, yT[:, 0:4, :].rearrange("p a b -> p (a b)"), xt2[:], uT2[o_][:], ALU.mult, ["xt2", "uT%d" % o_], ["yT"])
        for h in range(4):
            act(ph, osq[:], osum2[o_][:, h * 128:(h + 1) * 128], AF.Square, ["osum%d" % o_], ["osq", "ost"], accum_out=ost[:, h:h + 1])
        rstd_ln_exp(ost[:, 8:12], ost[:, 0:4], 4, 1.0 / 128, ["ost"], ["ost"])
        for h in range(4):
            stt(ph, ybt[:, h * 128:(h + 1) * 128], osum2[o_][:, h * 128:(h + 1) * 128], ost[:, 8 + h:9 + h],
                srg2[o_][:, h * 128:(h + 1) * 128], ALU.mult, ALU.mult, ["osum%d" % o_, "ost", "srg%d" % o_], ["ybt"])
        for c in range(4):
            tr(ph, ps[5][:, c * 128:(c + 1) * 128], ybt[:, c * 128:(c + 1) * 128], k.ident, ["ybt"], ["ps5"])
        act(ph, yT[:, 4:8, :], v4(ps[5][:, :]), AF.Copy, ["ps5"], ["yT"])
        for half in range(2):
            b = 6 if half == 0 else 0
            for j in range(8):
                mm(ph, ps[b][:, :], yT[:, j, :], wout[:, j, half * 512:(half + 1) * 512], j == 0, j == 7, ["yT", "wout"], ["ps%d" % b])
            tt(ph, ho[:, half * 512:(half + 1) * 512], ps[b][:, :], k.MOD[:, lcp, 2, half * 512:(half + 1) * 512], ALU.mult,
               ["ps%d" % b, "MOD"], ["ho"])
        tt(ph, ho[:], ho[:], nb["ht"][tp % 3][:], ALU.add, ["ho", "ht%d" % (tp % 3)], ["ho"])
        dma(ph, k.hbuf[tp * 128:(tp + 1) * 128, :], ho[:], ["ho"], [])

    def do_norm(tt_):
        lc_ = 1 if tt_ < 2 else 0
        return norm_tile(k, ph, nb, tt_, layer, k.MOD[:, lc_, 1, :], k.MOD[:, lc_, 0, :], nT[tt_ % 2], "nT%d" % (tt_ % 2),
                         prefetched=True)

    dma(ph, nb["ht"][order[0] % 3][:], k.hsrc(layer, order[0]), [], ["ht%d" % (order[0] % 3)])
    do_norm(order[0])
    for oi, t in enumerate(order):
        lc = 1 if t < 2 else 0
        nt_ = nT[t % 2]
        nk = "nT%d" % (t % 2)
        hk = "ht%d" % (t % 3)
        tn = order[oi + 1] if oi + 1 < len(order) else None
        if tn is not None:
            dma(ph, nb["ht"][tn % 3][:], k.hsrc(layer, tn), [], ["ht%d" % (tn % 3)])
        for c in range(4):
            for j in range(8):
                mm(ph, ps[2][:, c * 128:(c + 1) * 128], win[:, j, 1024 + c * 128:1024 + (c + 1) * 128], nt_[:, j, :],
                   j == 0, j == 7, ["win", nk], ["ps2"])
        for j in range(8):
            mm(ph, ps[3][0:32, 0:128], win[:, j, 2560:2592], nt_[:, j, :], j == 0, j == 7, ["win", nk], ["ps3"])
        for j in range(8):
            mm(ph, ps[4][:, 0:256], nt_[:, j, :], win[:, j, 1280:1536], j == 0, j == 7, ["win", nk], ["ps4"])
        for j in range(8):
            mm(ph, ps[5][:, :], nt_[:, j, :], win[:, j, 1536:2048], j == 0, j == 7, ["win", nk], ["ps5"])
        if full:
            for j in range(8):
                mm(ph, ps[6][:, :], nt_[:, j, :], win[:, j, 2048:2560], j == 0, j == 7, ["win", nk], ["ps6"])
            for j in range(8):
                mm(ph, ps[7][:, :], nt_[:, j, :], win[:, j, 512:1024], j == 0, j == 7, ["win", nk], ["ps7"])
            for c in range(4):
                for j in range(8):
                    mm(ph, ps[0][:, c * 128:(c + 1) * 128], win[:, j, c * 128:(c + 1) * 128], nt_[:, j, :],
                       j == 0, j == 7, ["win", nk], ["ps0"])
        act(ph, gfa[0:32, :], ps[3][0:32, 0:128], AF.Copy, ["ps3"], ["gfa0"])
        mm(ph, ps[1][:, 0:256], gfa[0:33, :], gateW[0:33, d * 256:(d + 1) * 256], True, True, ["gfa0", "gfa1", "gateW"], ["ps1"])
        act(ph, spt[:], ps[1][:, 0:256], AF.Exp, ["ps1"], ["spt"], scale=-1.0)
        act(ph, spt[:], spt[:], AF.Ln, ["spt"], ["spt"], bias=k.one_t[:, 0:1])
        for hp in range(2):
            mm(ph, ps[3][:, hp * 128:(hp + 1) * 128], spt[:, hp * 128:(hp + 1) * 128], TRI, True, True, ["spt", "cst"], ["ps3"])
        mm(ph, ps[1][:, 256:512], STRI, spt[:, :], True, True, ["spt", "cst"], ["ps1"])
        act(ph, Et[:], ps[3][:, 0:256], AF.Exp, ["ps3"], ["Et"])
        act(ph, Eit[:], ps[3][:, 0:256], AF.Exp, ["ps3"], ["Eit"], scale=-1.0)
        act(ph, ERt[:], ps[1][:, 256:512], AF.Exp, ["ps1"], ["ERt"])
        for hl in range(2):
            r0, r1 = hl * 64, (hl + 1) * 64
            stt(ph, qdTm[r0:r1, :, hl, :], ps[2][r0:r1, 0:256].rearrange("p (a b) -> p a b", b=128), 0.125,
                Et[r0:r1, :].rearrange("p (a b) -> p a b", b=128), ALU.mult, ALU.mult, ["ps2", "Et"], ["qdT"])
        tt(ph, kiT[:], ps[2][:, 256:512], Eit[:], ALU.mult, ["ps2", "Eit"], ["kiT"])
        tt(ph, krt[:], ps[4][:, 0:256], ERt[:], ALU.mult, ["ps4", "ERt"], ["krt"])
        act(ph, vb[:], ps[5][:, :], AF.Copy, ["ps5"], ["vb"])
        for h in range(4):
            hp, hl = h // 2, h % 2
            mm(ph, ps[4][:, h * 128:(h + 1) * 128], kiT[:, hp * 128:(hp + 1) * 128],
               qdTm[:, hp, hl, :], True, True, ["kiT", "qdT"], ["ps4"])
        tt(ph, sTt[:], ps[4][:, :], MASK, ALU.mult, ["ps4", "cst"], ["sTt"])
        for h in range(4):
            hp, hl = h // 2, h % 2
            mm(ph, ps[2][:, h * 128:(h + 1) * 128], sTt[:, h * 128:(h + 1) * 128], vb[:, h * 128:(h + 1) * 128],
               True, False, ["sTt", "vb"], ["ps2"])
            mm(ph, ps[2][:, h * 128:(h + 1) * 128], qdTm[:, hp, hl, :],
               Sbf[:, hp, hl * 128:(hl + 1) * 128], False, True, ["qdT", "Sbf"], ["ps2"])
        csb = 7 if not full else 1
        for hp in range(2):
            mm(ph, ps[csb][:, hp * 256:(hp + 1) * 256], krt[:, hp * 128:(hp + 1) * 128], vb[:, hp * 256:(hp + 1) * 256],
               True, True, ["krt", "vb"], ["ps%d" % csb])
        for hp in range(2):
            stt(ph, S32[:, hp, :], S32[:, hp, :], Et[:, hp * 128 + dcol:hp * 128 + dcol + 1], ps[csb][:, hp * 256:(hp + 1) * 256],
                ALU.mult, ALU.add, ["S32", "Et", "ps%d" % csb], ["S32"])
        act(ph, Sbf[:].rearrange("p a b -> p (a b)"), S32[:].rearrange("p a b -> p (a b)"), AF.Copy, ["S32"], ["Sbf"])
        if not full:
            cp(ph, k.OB[:, t, :], ps[2][:, :], ["ps2"], ["OB%d" % t])
            if tn is not None:
                do_norm(tn)
            continue
        ob_ = t % 2
        tt(ph, osum2[ob_][:], ps[2][:, :], k.OB[:, t, :], ALU.add, ["ps2"], ["osum%d" % ob_])
        act(ph, xa[:], ps[0][:, :], AF.Copy, ["ps0"], ["xa"])
        act(ph, xa2[:], ps[7][:, :], AF.Copy, ["ps7"], ["xa2"])
        act(ph, sgr[:], ps[6][:, :], AF.Sigmoid, ["ps6"], ["sgr"])
        stt(ph, srg2[ob_][:], ps[6][:, :], 1.0, bng[:].rearrange("p a b -> p (a b)"), ALU.mult, ALU.mult, ["ps6", "bng"],
            ["srg%d" % ob_])
        if oi >= 1:
            tail(order[oi - 1])
        if tn is not None:
            do_norm(tn)
        tt(ph, srg2[ob_][:], srg2[ob_][:], sgr[:], ALU.mult, ["srg%d" % ob_, "sgr"], ["srg%d" % ob_], eng=POOL)
        tt(ph, xt1[:], xa[:], xa[:], ALU.mult, ["xa"], ["xt1"])
        ts(ph, xt1[:], xt1[:], 0.044715 * 1.5957691216057308, 1.5957691216057308, ALU.mult, ALU.add, ["xt1"], ["xt1"])
        tt(ph, xt1[:], xt1[:], xa[:], ALU.mult, ["xt1", "xa"], ["xt1"])
        tt(ph, gv[:], xa2[:], xa2[:], ALU.mult, ["xa2"], ["gv"], eng=POOL)
        ts(ph, gv[:], gv[:], 0.044715 * 1.5957691216057308, 1.5957691216057308, ALU.mult, ALU.add, ["gv"], ["gv"])
        tt(ph, gv[:], gv[:], xa2[:], ALU.mult, ["gv", "xa2"], ["gv"], eng=POOL)
        act(ph, xt1[:], xt1[:], AF.Sigmoid, ["xt1"], ["xt1"])
        act(ph, gv[:], gv[:], AF.Sigmoid, ["gv"], ["gv"])
        tt(ph, uT2[ob_][:], xt1[:], xa[:], ALU.mult, ["xt1", "xa"], ["uT%d" % ob_])
        tt(ph, gv[:], gv[:], xa2[:], ALU.mult, ["gv", "xa2"], ["gv"])
        ph.add(DVE, lambda e: e.bn_stats(out=bst[:, 0:6], in_=gv[:]), ["gv"], ["bst"])
        ph.add(DVE, lambda e: e.bn_aggr(out=bst[:, 6:8], in_=bst[:, 0:6]), ["bst"], ["bst"])
        rstd_ln_exp(bst[:, 1:2], bst[:, 7:8], 1, 1.0, ["bst"], ["bst"])
        ts(ph, gv[:], gv[:], bst[:, 6:7], bst[:, 1:2], ALU.subtract, ALU.mult, ["gv", "bst"], ["gv"])
        tt(ph, gv[:], gv[:], lng[:], ALU.mult, ["gv", "lng"], ["gv"], eng=POOL)
        tt(ph, vA2[ob_][:], gv[:], lnb[:], ALU.add, ["gv", "lnb"], ["vA%d" % ob_])
    if full:
        tail(order[-1])
    ph.emit()


def phase_mla1(k, layer, i):
    import os
    ph = Phase(k.ctx)
    if os.environ.get("KCUT1"):
        ph.limit = int(os.environ["KCUT1"])
    ps = k.ps
    nb = norm_bufs(ph)
    wi = ph.sb("wi", [128, 8, 576], BF16)
    dma(ph, wi[:, :, 0:544], k.d["od_w_in"][i].rearrange("(k p) n -> p k n", p=128), [], ["wi"], eng=POOL)
    for g in range(2):
        cp(ph, wi[:, :, 544 + g * 16:544 + g * 16 + 8], wi[:, :, 512 + g * 16 + 8:512 + g * 16 + 16], ["wi"], ["wi"])
        cp(ph, wi[:, :, 544 + g * 16 + 8:544 + g * 16 + 16], wi[:, :, 512 + g * 16:512 + g * 16 + 8], ["wi"], ["wi"])
    wkv = ph.sb("wkv", [128, 2, 2048], BF16)
    wkvsrc = k.d["c_w_ukv"][i].rearrange("(k p) n -> p k n", p=128)
    for hh_ in range(2):
        dma(ph, wkv[:, :, hh_ * 1024:(hh_ + 1) * 1024], wkvsrc[:, :, hh_ * 1024:(hh_ + 1) * 1024], [], ["wkv"], eng=POOL)
    gq = ph.sb("gq", [128, 512], F32)
    dma(ph, gq[:, 0:256], k.d["c_q_norm_g"][i].partition_broadcast(128), [], ["gq"])
    dma(ph, gq[:, 256:512], k.d["c_kv_norm_g"][i].partition_broadcast(128), [], ["gq"])
    nT = [ph.sb("nT0", [128, 8, 128], BF16), ph.sb("nT1", [128, 8, 128], BF16)]
    cst2 = ph.sb("cst2", [128, 8], F32)
    cqn = ph.sb("cqn", [128, 512], F32)
    csq = ph.sb("csq", [128, 256], BF16)
    ckT = ph.sb("ckT", [128, 2, 128], BF16)
    ra = ph.sb("ra", [32, 128], F32)
    rb = ph.sb("rb", [32, 128], F32)
    rC = [ph.sb("rC0", [32, 128], F32), ph.sb("rC1", [32, 128], F32)]
    rS = [ph.sb("rS0", [32, 128], F32), ph.sb("rS1", [32, 128], F32)]
    def do_norm1(tt_):
        lc_ = 1 if tt_ < 2 else 0
        norm_tile(k, ph, nb, tt_, layer, k.MOD[:, lc_, 1, :], k.MOD[:, lc_, 0, :], nT[tt_ % 2], "nT%d" % (tt_ % 2))

    do_norm1(0)
    for t in range(NT):
        lc = 1 if t < 2 else 0
        nt_ = nT[t % 2]
        nk = "nT%d" % (t % 2)
        for j in range(8):
            mm(ph, ps[2][:, :], nt_[:, j, :], wi[:, j, 0:512], j == 0, j == 7, ["wi", nk], ["ps2"])
        for j in range(8):
            mm(ph, ps[3][0:32, 0:128], wi[:, j, 512:544], nt_[:, j, :], j == 0, j == 7, ["wi", nk], ["ps3"])
        for j in range(8):
            mm(ph, ps[3][0:32, 128:256], wi[:, j, 544:576], nt_[:, j, :], j == 0, j == 7, ["wi", nk], ["ps3"])
        for g in range(2):
            act(ph, csq[:], ps[2][:, g * 256:(g + 1) * 256], AF.Square, ["ps2"], ["csq", "cst2"], accum_out=cst2[:, g:g + 1])
        act(ph, cst2[:, 2:4], cst2[:, 0:2], AF.Sqrt, ["cst2"], ["cst2"], scale=1.0 / 256, bias=k.eps_t[:, 0:1])
        ph.add(DVE, lambda e: e.reciprocal(out=cst2[:, 4:6], in_=cst2[:, 2:4]), ["cst2"], ["cst2"])
        for g in range(2):
            stt(ph, cqn[:, g * 256:(g + 1) * 256], ps[2][:, g * 256:(g + 1) * 256], cst2[:, 4 + g:5 + g], gq[:, g * 256:(g + 1) * 256],
                ALU.mult, ALU.mult, ["ps2", "cst2", "gq"], ["cqn"])
        for c in range(4):
            tr(ph, ps[4][:, c * 128:(c + 1) * 128], cqn[:, c * 128:(c + 1) * 128], k.ident, ["cqn"], ["ps4"])
        if t + 1 < NT:
            do_norm1(t + 1)
        act(ph, k.CQT[:, :, t * 128:(t + 1) * 128], v4(ps[4][:, :])[:, 0:2, :], AF.Copy, ["ps4"], ["CQT"])
        cp(ph, ckT[:], v4(ps[4][:, :])[:, 2:4, :], ["ps4"], ["ckT"])
        if t >= 2:
            tl = (t - 2) * 128
            rs_ = t % 2
            dma(ph, rC[rs_][:], k.d["ropeC"][:, tl:tl + 128], [], ["rC%d" % rs_])
            dma(ph, rS[rs_][:], k.d["ropeS"][:, tl:tl + 128], [], ["rS%d" % rs_])
            tt(ph, ra[:], ps[3][0:32, 0:128], rC[rs_][:], ALU.mult, ["ps3", "rC%d" % rs_], ["ra"])
            tt(ph, rb[:], ps[3][0:32, 128:256], rS[rs_][:], ALU.mult, ["ps3", "rS%d" % rs_], ["rb"])
            tt(ph, k.KR[:, t * 128:(t + 1) * 128], ra[:], rb[:], ALU.add, ["ra", "rb"], ["KR"], eng=POOL)
        else:
            cp(ph, k.KR[:, t * 128:(t + 1) * 128], ps[3][0:32, 0:128], ["ps3"], ["KR"])
        for g in range(4):
            b = 5 + (g % 2)
            for hh in range(4):
                h = g * 4 + hh
                po = (h // 8) * 64
                for kc in range(2):
                    mm(ph, ps[b][po:po + 64, hh * 128:(hh + 1) * 128], wkv[:, kc, h * 128:h * 128 + 64], ckT[:, kc, :],
                       kc == 0, kc == 1, ["wkv", "ckT"], ["ps%d" % b])
            po = (g // 2) * 64
            s0 = (g % 2) * 4
            eng_cp = act if g % 2 == 0 else None
            if g % 2 == 0:
                act(ph, k.KN[po:po + 64, s0:s0 + 4, t * 128:(t + 1) * 128], v4(ps[b][po:po + 64, :]), AF.Copy, ["ps%d" % b], ["KN"])
            else:
                cp(ph, k.KN[po:po + 64, s0:s0 + 4, t * 128:(t + 1) * 128], v4(ps[b][po:po + 64, :]), ["ps%d" % b], ["KN"])
        for half in range(2):
            b = 7 if half == 0 else 2
            rhs_v = wkv[:, :, half * 1024:(half + 1) * 1024].rearrange("p k (h x) -> p k h x", x=128)
            for kc in range(2):
                mm(ph, ps[b][:, :].rearrange("p (h x) -> p h x", x=64), ckT[:, kc, :], rhs_v[:, kc, :, 64:128], kc == 0, kc == 1,
                   ["wkv", "ckT"], ["ps%d" % b])
            if half == 0:
                act(ph, k.VA[:, t, 0:8, :], ps[b][:, :].rearrange("p (h x) -> p h x", x=64), AF.Copy, ["ps%d" % b], ["VA"])
            else:
                cp(ph, k.VA[:, t, 8:16, :], ps[b][:, :].rearrange("p (h x) -> p h x", x=64), ["ps%d" % b], ["VA"])
    cp(ph, k.MG[:, 0, :], k.MOD[:, 0, 2, :], ["MOD"], ["MG"])
    cp(ph, k.MG[:, 1, :], k.MOD[:, 1, 2, :], ["MOD"], ["MG"])
    ph.emit()


def phase_mla2(k, layer, i, need_ctx):
    import os
    ph = Phase(k.ctx)
    if os.environ.get("KCUT2"):
        ph.limit = int(os.environ["KCUT2"])
    ps = k.ps
    wq = ph.sb("wq", [128, 2, 1536], BF16)
    wqsrc = k.d["c_w_uq"][i].rearrange("(k p) n -> p k n", p=128)
    for hh_ in range(2):
        dma(ph, wq[:, :, hh_ * 768:(hh_ + 1) * 768], wqsrc[:, :, hh_ * 768:(hh_ + 1) * 768], [], ["wq"], eng=POOL)
    wqs = ph.sb("wqs", [128, 2, 16, 32], BF16)
    wq4 = wq[:].rearrange("p k (h x) -> p k h x", x=96)
    for g in range(2):
        cp(ph, wqs[:, :, :, g * 16:g * 16 + 8], wq4[:, :, :, 64 + g * 16 + 8:64 + g * 16 + 16], ["wq"], ["wqs"])
        cp(ph, wqs[:, :, :, g * 16 + 8:g * 16 + 16], wq4[:, :, :, 64 + g * 16:64 + g * 16 + 8], ["wq"], ["wqs"])
    wo = ph.sb("wo", [64, 16, D], BF16)
    dma(ph, wo[:], k.d["od_w_out"][i].rearrange("(h d) n -> d h n", d=64), [], ["wo"], eng=POOL)
    OT = ph.sb("OT", [64, 16, 512], BF16)
    QN = [ph.sb("QN%d" % q_, [128, 512], BF16) for q_ in range(4)]
    for q_ in range(4):
        ms(ph, QN[q_][:], 0.0, ["QN%d" % q_])
    QR = [ph.sb("QR0", [32, 512], BF16), ph.sb("QR1", [32, 512], BF16)]
    qa = ph.sb("qa", [32, 512], F32)
    qb = ph.sb("qb", [32, 512], F32)
    pT = [ph.sb("pT%d" % q_, [128, 512], BF16) for q_ in range(3)]
    rinv = ph.sb("rinv", [64, 512], F32)
    ht = ph.sb("ht0", [128, D], F32)
    ho = ph.sb("ho0", [128, D], F32)
    rC = ph.sb("rC", [32, 512], F32)
    rS = ph.sb("rS", [32, 512], F32)
    blocks = ([(0, 2)] if need_ctx else []) + [(2 + 4 * b, 4) for b in range(4)]
    items = [(t0, ntile, h) for (t0, ntile) in blocks for h in range(16)]
    st = {"npt": 0}

    def stage_q(idx):
        t0, ntile, h = items[idx]
        nq, q0 = ntile * 128, t0 * 128
        po = (h // 8) * 64
        s = idx % 2
        qs = s + 2 * (h // 8)
        qn, qr = QN[qs], QR[s]
        if h == 0 and t0 >= 2:
            dma(ph, rC[:, 0:nq], k.d["ropeC"][:, q0 - 256:q0 - 256 + nq], [], ["rC"])
            dma(ph, rS[:, 0:nq], k.d["ropeS"][:, q0 - 256:q0 - 256 + nq], [], ["rS"])
        for kc in range(2):
            mm(ph, ps[0][po:po + 64, 0:nq], wq[:, kc, h * 96:h * 96 + 64], k.CQT[:, kc, q0:q0 + nq], kc == 0, kc == 1,
               ["wq", "CQT"], ["ps0"])
        for kc in range(2):
            mm(ph, ps[1][0:32, 0:nq], wq[:, kc, h * 96 + 64:h * 96 + 96], k.CQT[:, kc, q0:q0 + nq], kc == 0, kc == 1,
               ["wq", "CQT"], ["ps1"])
        act(ph, qn[po:po + 64, 0:nq], ps[0][po:po + 64, 0:nq], AF.Copy, ["ps0"], ["QN%d" % qs])
        if t0 >= 2:
            for kc in range(2):
                mm(ph, ps[2][0:32, 0:nq], wqs[:, kc, h, :], k.CQT[:, kc, q0:q0 + nq], kc == 0, kc == 1, ["wqs", "CQT"], ["ps2"])
            tt(ph, qa[:, 0:nq], ps[1][0:32, 0:nq], rC[:, 0:nq], ALU.mult, ["ps1", "rC"], ["qa"])
            tt(ph, qb[:, 0:nq], ps[2][0:32, 0:nq], rS[:, 0:nq], ALU.mult, ["ps2", "rS"], ["qb"])
            tt(ph, qr[:, 0:nq], qa[:, 0:nq], qb[:, 0:nq], ALU.add, ["qa", "qb"], ["QR%d" % s], eng=POOL)
        else:
            cp(ph, qr[:, 0:nq], ps[1][0:32, 0:nq], ["ps1"], ["QR%d" % s])

    def qk(idx, kt, slot):
        t0, ntile, h = items[idx]
        nq = ntile * 128
        s = idx % 2
        qs = s + 2 * (h // 8)
        b = 3 + slot
        mm(ph, ps[b][:, 0:nq], k.KN[:, h % 8, kt * 128:(kt + 1) * 128], QN[qs][:, 0:nq], True, False,
           ["KN", "QN%d" % qs], ["ps%d" % b])
        mm(ph, ps[b][:, 0:nq], k.KR[0:32, kt * 128:(kt + 1) * 128], QR[s][0:32, 0:nq], False, True,
           ["KR", "QR%d" % s], ["ps%d" % b])
        act(ph, pT[slot][:, 0:nq], ps[b][:, 0:nq], AF.Exp, ["ps%d" % b], ["pT%d" % slot], scale=C_SCALE)

    onesb = ph.sb("onesb", [128, 64], BF16)
    ms(ph, onesb[:], 1.0, ["onesb"])

    def pv(idx, kt, slot, nkt):
        t0, ntile, h = items[idx]
        nq = ntile * 128
        ob = 6 + idx % 2
        mm(ph, ps[ob][0:64, 0:nq], k.VA[:, kt, h, :], pT[slot][:, 0:nq], kt == 0, kt == nkt - 1,
           ["VA", "pT%d" % slot], ["ps%d" % ob])
        mm(ph, ps[ob][64:128, 0:nq], onesb[:, :], pT[slot][:, 0:nq], kt == 0, kt == nkt - 1,
           ["onesb", "pT%d" % slot], ["ps%d" % ob])

    def tail(idx):
        t0, ntile, h = items[idx]
        nq = ntile * 128
        ob = 6 + idx % 2
        ph.add(DVE, lambda e: e.reciprocal(out=rinv[0:64, 0:nq], in_=ps[ob][64:128, 0:nq]), ["ps%d" % ob], ["rinv"])
        tt(ph, OT[:, h, 0:nq], ps[ob][0:64, 0:nq], rinv[0:64, 0:nq], ALU.mult, ["ps%d" % ob, "rinv"], ["OT"])

    def outproj(t0, ntile):
        for ti in range(ntile):
            t = t0 + ti
            lc = 1 if t < 2 else 0
            dma(ph, ht[:], k.hsrc(layer, t), [], ["ht"])
            for half in range(2):
                b = half
                for h in range(16):
                    mm(ph, ps[b][:, :], OT[:, h, ti * 128:(ti + 1) * 128], wo[:, h, half * 512:(half + 1) * 512], h == 0, h == 15,
                       ["OT", "wo"], ["ps%d" % b])
                tt(ph, ho[:, half * 512:(half + 1) * 512], ps[b][:, :], k.MG[:, lc, half * 512:(half + 1) * 512], ALU.mult,
                   ["ps%d" % b, "MG"], ["ho"])
            tt(ph, ho[:], ho[:], ht[:], ALU.add, ["ho", "ht"], ["ho"], eng=POOL)
            dma(ph, k.hbuf[t * 128:(t + 1) * 128, :], ho[:], ["ho"], [])

    stage_q(0)
    pend_tail = None
    for idx in range(len(items)):
        t0, ntile, h = items[idx]
        nkt = 2 if t0 == 0 else NT
        base = st["npt"]
        DEPTH = 2
        for kt in range(min(DEPTH, nkt)):
            qk(idx, kt, (base + kt) % 3)
        if pend_tail is not None:
            tail(pend_tail)
            pt0, pnt, phh = items[pend_tail]
            if phh == 15:
                outproj(pt0, pnt)
            pend_tail = None
        if idx + 1 < len(items) and items[idx + 1][2] != 0:
            stage_q(idx + 1)
        for kt in range(nkt):
            if kt + DEPTH < nkt:
                qk(idx, kt + DEPTH, (base + kt + DEPTH) % 3)
            pv(idx, kt, (base + kt) % 3, nkt)
        st["npt"] = base + nkt
        if idx + 1 < len(items) and items[idx + 1][2] == 0:
            tail(idx)
            outproj(t0, ntile)
            stage_q(idx + 1)
        else:
            pend_tail = idx
    if pend_tail is not None:
        tail(pend_tail)
        pt0, pnt, phh = items[pend_tail]
        outproj(pt0, pnt)
    ph.emit()


def phase_ffn_norm(k, layer, moe, t_first):
    ph = Phase(k.ctx)
    ps = k.ps
    nb = norm_bufs(ph)
    if moe:
        i = layer // 2
        rw = ph.sb("rw", [128, 8, 8], F32)
        dma(ph, rw[:], k.d["moe_router"][i].rearrange("(k p) n -> p k n", p=128), [], ["rw"])
        mT32 = [ph.sb("mT32a", [128, 8, 128], F32), ph.sb("mT32b", [128, 8, 128], F32)]
        lg = ph.sb("lg", [128, 8], F32)
        m8 = ph.sb("m8", [128, 8], F32)
        ex = ph.sb("ex", [128, 8], F32)
        ge = ph.sb("ge", [128, 8], F32)
        gs = ph.sb("gs", [128, 4], F32)

    def router(t):
        m32 = mT32[t % 2]
        mk = "mT32_%d" % (t % 2)
        for j in range(8):
            mm(ph, ps[2][:, 0:8], m32[:, j, :], rw[:, j, :], j == 0, j == 7, [mk, "rw"], ["ps2"])
        cp(ph, lg[:], ps[2][:, 0:8], ["ps2"], ["lg"])
        ph.add(DVE, lambda e: e.max(out=m8[:], in_=lg[:]), ["lg"], ["m8"])
        ts(ph, gs[:, 0:1], m8[:, 0:1], -1.0, None, ALU.mult, None, ["m8"], ["gs"])
        act(ph, ex[:], lg[:], AF.Exp, ["lg", "gs"], ["ex"], bias=gs[:, 0:1])
        stt(ph, ge[:], lg[:], m8[:, 1:2], ex[:], ALU.is_ge, ALU.mult, ["lg", "m8", "ex"], ["ge", "gs1"], accum=gs[:, 1:2])
        ph.add(DVE, lambda e: e.reciprocal(out=gs[:, 2:3], in_=gs[:, 1:2]), ["gs1"], ["gs2"])
        ts(ph, k.gates[:, t, :], ge[:], gs[:, 2:3], None, ALU.mult, None, ["ge", "gs2"], ["gates"])

    pend = None
    for t in range(t_first, NT):
        lc = 1 if t < 2 else 0
        norm_tile(k, ph, nb, t, layer, k.MOD[:, lc, 1, :], k.MOD[:, lc, 0, :], k.mT[:, :, t * 128:(t + 1) * 128], "mT",
                  extra_f32=(mT32[t % 2], "mT32_%d" % (t % 2)) if moe else None, mixed=True)
        if moe:
            if pend is not None:
                router(pend)
            pend = t
    if moe and pend is not None:
        router(pend)
    cp(ph, k.MG[:, 0, :], k.MOD[:, 0, 2, :], ["MOD"], ["MG"])
    cp(ph, k.MG[:, 1, :], k.MOD[:, 1, 2, :], ["MOD"], ["MG"])
    ph.emit()


def phase_ffn_experts(k, layer, moe, t_first):
    ph = Phase(k.ctx)
    ps = k.ps
    i = layer // 2
    G = 4
    H = MOE_HIDDEN if moe else FF_HIDDEN
    nch = H // 128
    groups = []
    c0 = 0
    while c0 < nch:
        g = min(G, nch - c0)
        groups.append((c0, g))
        c0 += g
    wg = [ph.sb("wg0", [128, 8, G * 128], BF16), ph.sb("wg1", [128, 8, G * 128], BF16)]
    wu = [ph.sb("wu0", [128, 8, G * 128], BF16), ph.sb("wu1", [128, 8, G * 128], BF16)]
    wd = [ph.sb("wd0", [128, G, D], BF16), ph.sb("wd1", [128, G, D], BF16)]
    hT = [ph.sb("hT0", [128, G, 512], BF16), ph.sb("hT1", [128, G, 512], BF16)]
    sg = [ph.sb("sg0", [128, 512], F32), ph.sb("sg1", [128, 512], F32)]
    ms(ph, k.acc[:], 0.0, ["acc"], eng=POOL)
    tblocks = []
    t = t_first
    while t < NT:
        n = min(4, NT - t)
        tblocks.append((t, n))
        t += n
    nexp = 8 if moe else 1
    work = [(e, c0, g) for e in range(nexp) for (c0, g) in groups]
    if moe:
        srcs = [(k.d["moe_w_gate"][i, e].rearrange("(k p) n -> p k n", p=128),
                 k.d["moe_w_up"][i, e].rearrange("(k p) n -> p k n", p=128),
                 k.d["moe_w_down"][i, e].rearrange("(c p) n -> p c n", p=128)) for e in range(8)]
    else:
        srcs = [(k.d["ff_w_gate"][i].rearrange("(k p) n -> p k n", p=128),
                 k.d["ff_w_up"][i].rearrange("(k p) n -> p k n", p=128),
                 k.d["ff_w_down"][i].rearrange("(c p) n -> p c n", p=128))]
    seq = [(wi_, tb) for wi_ in range(len(work)) for tb in range(len(tblocks))]
    cnt = {"ps": 0}

    def load(wi_):
        e, c0, g = work[wi_]
        s = wi_ % 2
        srcg, srcu, srcd = srcs[e]
        dma(ph, wg[s][:, :, 0:g * 128], srcg[:, :, c0 * 128:(c0 + g) * 128], [], ["wg%d" % s], eng=POOL)
        dma(ph, wu[s][:, :, 0:g * 128], srcu[:, :, c0 * 128:(c0 + g) * 128], [], ["wu%d" % s], eng=POOL)
        dma(ph, wd[s][:, 0:g, :], srcd[:, c0:c0 + g, :], [], ["wd%d" % s], eng=POOL)

    def gu(n):
        wi_, tb = seq[n]
        e, c0, g = work[wi_]
        s = wi_ % 2
        tb0, ntile = tblocks[tb]
        nq, q0 = ntile * 128, tb0 * 128
        hs = n % 2
        for c in range(g):
            bg = (cnt["ps"] % 2) * 2
            cnt["ps"] += 1
            for j in range(8):
                mm(ph, ps[bg][:, 0:nq], wg[s][:, j, c * 128:(c + 1) * 128], k.mT[:, j, q0:q0 + nq], j == 0, j == 7,
                   ["wg%d" % s, "mT"], ["ps%d" % bg])
            for j in range(8):
                mm(ph, ps[bg + 1][:, 0:nq], wu[s][:, j, c * 128:(c + 1) * 128], k.mT[:, j, q0:q0 + nq], j == 0, j == 7,
                   ["wu%d" % s, "mT"], ["ps%d" % (bg + 1)])
            sgt = sg[c % 2]
            act(ph, sgt[:, 0:nq], ps[bg][:, 0:nq], AF.Silu, ["ps%d" % bg], ["sg%d" % (c % 2)])
            tt(ph, hT[hs][:, c, 0:nq], sgt[:, 0:nq], ps[bg + 1][:, 0:nq], ALU.mult, ["sg%d" % (c % 2), "ps%d" % (bg + 1)],
               ["hT%d" % hs])

    hres = [ph.sb("hres0", [128, D], F32), ph.sb("hres1", [128, D], F32)]

    def down(n):
        wi_, tb = seq[n]
        e, c0, g = work[wi_]
        s = wi_ % 2
        tb0, ntile = tblocks[tb]
        hs = n % 2
        last_item = wi_ == len(work) - 1
        for ti in range(ntile):
            tI = tb0 + ti
            if last_item:
                dma(ph, hres[tI % 2][:], k.hsrc(layer, tI, mixed=True), [], ["hres%d" % (tI % 2)])
            for half in range(2):
                b = 4 + ((ti * 2 + half) % 4)
                for c in range(g):
                    mm(ph, ps[b][:, :], hT[hs][:, c, ti * 128:(ti + 1) * 128], wd[s][:, c, half * 512:(half + 1) * 512],
                       c == 0, c == g - 1, ["hT%d" % hs, "wd%d" % s], ["ps%d" % b])
                a_ = k.acc[:, tI, half * 512:(half + 1) * 512]
                if moe:
                    stt(ph, a_, ps[b][:, :], k.gates[:, tI, e:e + 1], a_, ALU.mult, ALU.add, ["ps%d" % b, "acc%d" % tI, "acc"],
                        ["acc%d" % tI])
                else:
                    tt(ph, a_, ps[b][:, :], a_, ALU.add, ["ps%d" % b, "acc%d" % tI, "acc"], ["acc%d" % tI])
            if last_item:
                lc_ = 1 if tI < 2 else 0
                hk_ = "hres%d" % (tI % 2)
                tt(ph, k.acc[:, tI, :], k.acc[:, tI, :], k.MG[:, lc_, :], ALU.mult, ["acc%d" % tI, "acc"], ["acc%d" % tI], eng=POOL)
                tt(ph, hres[tI % 2][:], hres[tI % 2][:], k.acc[:, tI, :], ALU.add, [hk_, "acc%d" % tI], [hk_])
                dma(ph, k.hbuf[tI * 128:(tI + 1) * 128, :], hres[tI % 2][:], [hk_], [])

    load(0)
    for n in range(len(seq)):
        wi_, tb = seq[n]
        gu(n)
        if n >= 1:
            down(n - 1)
        if tb == 0 and wi_ + 1 < len(work):
            load(wi_ + 1)
    down(len(seq) - 1)
    ph.emit()


def phase_ffn_resid(k, layer, t_first):
    ph = Phase(k.ctx)
    ht = [ph.sb("ht0", [128, D], F32), ph.sb("ht1", [128, D], F32)]
    for t in range(t_first, NT):
        lc = 1 if t < 2 else 0
        s = t % 2
        dma(ph, ht[s][:], k.hsrc(layer, t, mixed=True), [], ["ht%d" % s])
        tt(ph, k.acc[:, t, :], k.acc[:, t, :], k.MG[:, lc, :], ALU.mult, [], ["acc%d" % t], eng=POOL)
        tt(ph, ht[s][:], ht[s][:], k.acc[:, t, :], ALU.add, ["ht%d" % s, "acc%d" % t], ["ht%d" % s])
        dma(ph, k.hbuf[t * 128:(t + 1) * 128, :], ht[s][:], ["ht%d" % s], [])
    ph.emit()


def phase_final(k):
    ph = Phase(k.ctx)
    ht = [ph.sb("ht0", [128, D], F32), ph.sb("ht1", [128, D], F32)]
    sq = ph.sb("sq", [128, D], BF16)
    st = ph.sb("st", [128, 4], F32)
    fg = ph.sb("fg", [128, D], F32)
    dma(ph, fg[:], k.d["final_g"].partition_broadcast(128), [], ["fg"])
    for t in range(2, NT):
        s = t % 2
        dma(ph, ht[s][:], k.hbuf[t * 128:(t + 1) * 128, :], [], ["ht%d" % s])
        act(ph, sq[:], ht[s][:], AF.Square, ["ht%d" % s], ["sq", "st"], accum_out=st[:, 0:1])
        act(ph, st[:, 1:2], st[:, 0:1], AF.Sqrt, ["st"], ["st"], scale=1.0 / D, bias=k.eps_t[:, 0:1])
        ph.add(DVE, lambda e: e.reciprocal(out=st[:, 2:3], in_=st[:, 1:2]), ["st"], ["st"])
        stt(ph, ht[s][:], ht[s][:], st[:, 2:3], fg[:], ALU.mult, ALU.mult, ["ht%d" % s, "st", "fg"], ["ht%d" % s])
        dma(ph, k.d["out"][(t - 2) * 128:(t - 1) * 128, :], ht[s][:], ["ht%d" % s], [])
    ph.emit()


def phase_dump(k):
    ph = Phase(k.ctx)
    ht = [ph.sb("ht0", [128, D], F32), ph.sb("ht1", [128, D], F32)]
    for t in range(NT):
        s = t % 2
        dma(ph, ht[s][:], k.hbuf[t * 128:(t + 1) * 128, :], [], ["ht%d" % s])
        dma(ph, k.d["dbg"][t * 128:(t + 1) * 128, :], ht[s][:], ["ht%d" % s], [])
    ph.emit()


def phase_dump_sb(k, aps):
    ph = Phase(k.ctx)
    st = ph.sb("dst", [128, D], F32)
    for n, ap in enumerate(aps):
        w = ap.shape[-1]
        cp(ph, st[0:ap.shape[0], 0:w], ap, [], ["dst"])
        dma(ph, k.d["dbg"][n * 128:n * 128 + ap.shape[0], 0:w], st[0:ap.shape[0], 0:w], ["dst"], [])
    ph.emit()


INPUT_SHAPES = {
    "mod_w": [4, 1024, 6144], "mod_b": [4, 6144], "norm_mix_g": [4, 1024], "norm_ffn_g": [4, 1024], "final_g": [1024],
    "ev_w_in": [2, 1024, 2592], "a_ln_g": [2, 512], "a_ln_b": [2, 512], "a_ws": [2, 4, 128, 128], "a_bs": [2, 4, 128],
    "b_gate_w": [2, 2, 16, 256], "b_gate_b": [2, 2, 256], "b_norm_g": [2, 128], "ev_w_out": [2, 1024, 1024],
    "od_w_in": [2, 1024, 544], "c_q_norm_g": [2, 256], "c_w_uq": [2, 256, 1536], "c_kv_norm_g": [2, 256],
    "c_w_ukv": [2, 256, 2048], "od_w_out": [2, 1024, 1024],
    "ff_w_gate": [2, 1024, 2816], "ff_w_up": [2, 1024, 2816], "ff_w_down": [2, 2816, 1024],
    "moe_router": [2, 1024, 8], "moe_w_gate": [2, 8, 1024, 3584], "moe_w_up": [2, 8, 1024, 3584], "moe_w_down": [2, 8, 3584, 1024],
}
NCST = 2048


def make_consts():
    cst = np.zeros((128, NCST), np.float32)
    j = np.arange(128)[:, None]
    i = np.arange(128)[None, :]
    cst[:, 0:128] = np.eye(128)
    cst[:, 128:256] = (j <= i) * (-1.0 / 16)
    cst[:, 256:384] = (j >= i) * (-1.0 / 16)
    cst[:, 384:512] = (j > i) * (-1.0 / 16)
    cst[:, 512:640] = (j < i) * (-1.0 / 16)
    cst[:, 896:1024] = 1.0
    cst[:, 1024:1536] = np.tile((j <= i).astype(np.float32), (1, 4))
    cst[:, 1536:2048] = np.tile((j >= i).astype(np.float32), (1, 4))
    half = 16
    inv_freq = (10000.0 ** (-np.arange(0, half, 2, dtype=np.float32) / half)).astype(np.float32)
    tpos = np.arange(2048)
    rows = (tpos // 64).astype(np.float32)
    cols = (tpos % 64).astype(np.float32)
    C = np.zeros((32, 2048), np.float32)
    S = np.zeros((32, 2048), np.float32)
    for dd in range(32):
        g, jj = dd // 16, dd % 16
        f = jj % 8
        pos = rows if g == 0 else cols
        ang = (pos * inv_freq[f]).astype(np.float32)
        C[dd] = np.cos(ang)
        S[dd] = (-np.sin(ang)) if jj < 8 else np.sin(ang)
    return cst, C, S


def build(stop=None, dbg=False):
    nc = bass.Bass("TRN2", target_bir_lowering=False)
    k = K()
    k.nc = nc
    shapes = dict(INPUT_SHAPES)
    shapes.update({"x": [2048, D], "ctx": [256, D], "c": [D], "c_ctx": [D], "cst": [128, NCST],
                   "ropeC": [32, 2048], "ropeS": [32, 2048]})

    class LazyD(dict):
        def __missing__(self, n):
            ap = nc.dram_tensor(n, shapes[n], F32, kind="ExternalInput").ap()
            self[n] = ap
            return ap
    k.d = LazyD()
    k.d["out"] = nc.dram_tensor("out", [2048, D], F32, kind="ExternalOutput").ap()
    if dbg:
        k.d["dbg"] = nc.dram_tensor("dbg", [T, D], F32, kind="ExternalOutput").ap()
    k.hbuf = nc.dram_tensor("hbuf", [T, D], F32, kind="Internal").ap()

    def hsrc(layer, t, mixed=False):
        if layer == 0 and not mixed:
            return k.d["ctx"][t * 128:(t + 1) * 128, :] if t < 2 else k.d["x"][(t - 2) * 128:(t - 1) * 128, :]
        return k.hbuf[t * 128:(t + 1) * 128, :]
    k.hsrc = hsrc

    es = ExitStack()
    k.ctx = Ctx(nc, es)
    k.ps = [es.enter_context(nc.psum_tensor("psb%d" % b, [128, 512], F32)) for b in range(8)]
    k.cst = es.enter_context(nc.sbuf_tensor("cst_sb", [128, NCST], F32))
    k.eps_t = es.enter_context(nc.sbuf_tensor("eps_t", [128, 1], F32))
    k.condrep = es.enter_context(nc.sbuf_tensor("condrep", [128, 2, 8, 128], BF16))
    k.MG = es.enter_context(nc.sbuf_tensor("MG", [128, 2, D], F32))
    k.ident = k.cst[:, 0:128]
    k.TRI = [k.cst[:, 128:256], k.cst[:, 256:384]]
    k.STRI = [k.cst[:, 384:512], k.cst[:, 512:640]]
    k.ones = k.cst[:, 896:1024]
    k.one_t = k.cst[:, 896:897]
    k.MASK4 = [k.cst[:, 1024:1536], k.cst[:, 1536:2048]]

    def done(tag):
        return stop is not None and stop == tag

    phase_setup(k)
    if done("setup"):
        phase_dump_sb(k, [k.condrep[:, 0, 0, :], k.condrep[:, 1, 7, :], k.cst[:, 0:1024]])
        es.close()
        nc._used_inputs = [n for n in k.d if n not in ("out", "dbg")]
        return nc
    finished = False
    for layer in range(4):
        i = layer // 2
        need_ctx = layer < 3
        with ExitStack() as es2:
            if layer % 2 == 0:
                k.OB = es2.enter_context(nc.sbuf_tensor("OB_%d" % layer, [128, NT, 512], BF16))
                k.win = es2.enter_context(nc.sbuf_tensor("win_%d" % layer, [128, 8, 2592], BF16))
            else:
                k.KN = es2.enter_context(nc.sbuf_tensor("KN_%d" % layer, [128, 8, T], BF16))
                k.KR = es2.enter_context(nc.sbuf_tensor("KR_%d" % layer, [32, T], BF16))
                k.VA = es2.enter_context(nc.sbuf_tensor("VA_%d" % layer, [128, NT, 16, 64], BF16))
                k.CQT = es2.enter_context(nc.sbuf_tensor("CQT_%d" % layer, [128, 2, T], BF16))
            with nc.sbuf_tensor("MODa_%d" % layer, [128, 2, 3, D], F32) as MOD:
                k.MOD = MOD
                phase_mod(k, layer, 0)
                if done("mod%d" % layer):
                    phase_dump_sb(k, [k.MOD[:, a, b, :] for a in range(2) for b in range(3)])
                    nc._used_inputs = [n for n in k.d if n not in ("out", "dbg")]
                    return nc
                if layer % 2 == 0:
                    phase_even(k, layer, i, 1)
                    if done("evenB%d" % layer):
                        phase_dump_sb(k, [k.OB[:, t, :] for t in range(NT)])
                        nc._used_inputs = [n for n in k.d if n not in ("out", "dbg")]
                        return nc
                    phase_even(k, layer, i, 0)
                else:
                    phase_mla1(k, layer, i)
                    if done("mlaA%d" % layer):
                        phase_dump_sb(k, [k.CQT[:, 0, 0:1024], k.KN[:, 0, 0:1024], k.KR[:, 0:1024]])
                        nc._used_inputs = [n for n in k.d if n not in ("out", "dbg")]
                        return nc
            if layer % 2 == 1:
                phase_mla2(k, layer, i, need_ctx)
        if done("mix%d" % layer):
            break
        t_first = 0 if need_ctx else 2
        moe = layer % 2 == 1
        with ExitStack() as es3:
            k.mT = es3.enter_context(nc.sbuf_tensor("mT_%d" % layer, [128, 8, T], BF16))
            k.acc = es3.enter_context(nc.sbuf_tensor("acc_%d" % layer, [128, NT, D], F32))
            k.gates = es3.enter_context(nc.sbuf_tensor("gates_%d" % layer, [128, NT, 8], F32))
            with nc.sbuf_tensor("MODb_%d" % layer, [128, 2, 3, D], F32) as MOD:
                k.MOD = MOD
                phase_mod(k, layer, 1)
                phase_ffn_norm(k, layer, moe, t_first)
            phase_ffn_experts(k, layer, moe, t_first)
        if done("ffn%d" % layer):
            break
    else:
        finished = True
    if finished or not dbg:
        phase_final(k)
    if dbg:
        phase_dump(k)
    es.close()
    nc._used_inputs = [n for n in k.d if n not in ("out", "dbg")]
    return nc


_CACHE = {}


def kernel(**inputs):
    cst, C, S = make_consts()
    if "nc" not in _CACHE:
        _CACHE["nc"] = build()
    nc = _CACHE["nc"]
    shared = {n: np.ascontiguousarray(inputs[n], dtype=np.float32) for n in INPUT_SHAPES}
    shared["c_ctx"] = np.ascontiguousarray(inputs["c_ctx"], dtype=np.float32)
    shared["cst"] = cst
    shared["ropeC"] = C
    shared["ropeS"] = S
    used = set(nc._used_inputs)
    shared = {n: v for n, v in shared.items() if n in used}
    in_maps = []
    for b in range(8):
        m = dict(shared)
        m["x"] = np.ascontiguousarray(inputs["x"][b], dtype=np.float32)
        m["ctx"] = np.ascontiguousarray(inputs["ctx"][b], dtype=np.float32)
        m["c"] = np.ascontiguousarray(inputs["c"][b], dtype=np.float32)
        in_maps.append(m)
    res = run_bass_kernel_spmd(nc, in_maps, core_ids=list(range(8)))
    return np.stack([np.asarray(r["out"], dtype=np.float32) for r in res.results], axis=0)
```

```python
import numpy as np
from contextlib import ExitStack
import concourse.bass as bass
import concourse.mybir as mybir
from concourse.alu_op_type import AluOpType as ALU
from concourse.bass_utils import run_bass_kernel_spmd

AF = mybir.ActivationFunctionType
F32 = mybir.dt.float32
BF16 = mybir.dt.bfloat16
PE, ACT, DVE, POOL, SP = "tensor", "scalar", "vector", "gpsimd", "sync"
ENGS = [PE, ACT, DVE, POOL, SP]
NDS = 8

D = 1024
NT = 18
T = 2304
EPS = 1e-6
FF_HIDDEN = 2816
MOE_HIDDEN = 3584
C_SCALE = 96 ** -0.5


class Op:
    __slots__ = ("eng", "fn", "deps", "need", "dma", "sig", "waits")

    def __init__(self, eng, fn, dma):
        self.eng, self.fn, self.dma = eng, fn, dma
        self.deps, self.need, self.sig, self.waits = [], False, None, []


class Ctx:
    def __init__(self, nc, es):
        self.nc = nc
        self.esem = {e: es.enter_context(nc.semaphore("s_" + e)) for e in ENGS}
        self.dsem = {e: [es.enter_context(nc.semaphore("d_%s_%d" % (e, i))) for i in range(NDS)] for e in (SP, POOL)}
        self.ecnt = {e: 0 for e in ENGS}
        self.dcnt = {e: 0 for e in self.dsem}
        self.waited = {e: {} for e in ENGS}
        self.nphase = 0


class Phase:
    def __init__(self, ctx):
        self.ctx = ctx
        self.nc = ctx.nc
        self.ops = []
        self.last_w = {}
        self.readers = {}
        self.es = ExitStack()
        self.limit = None

    def sb(self, name, shape, dt):
        return self.es.enter_context(self.nc.sbuf_tensor("%s_p%d" % (name, self.ctx.nphase), list(shape), dt))

    def add(self, eng, fn, R=(), W=(), dma=False):
        if self.limit is not None and len(self.ops) >= self.limit:
            return None
        op = Op(eng, fn, dma)
        deps = {}
        for k in R:
            w = self.last_w.get(k)
            if w is not None:
                deps[id(w)] = (w, True)
            if k.startswith("ps"):
                for r in self.readers.get(k, ()):
                    if r.eng != eng and id(r) not in deps:
                        deps[id(r)] = (r, False)
        for k in W:
            w = self.last_w.get(k)
            if w is not None and id(w) not in deps:
                deps[id(w)] = (w, False)
            for r in self.readers.get(k, ()):
                if id(r) not in deps:
                    deps[id(r)] = (r, False)
        for d, raw in deps.values():
            if (not d.dma) and (not dma) and d.eng == eng:
                if eng == PE:
                    continue
            op.deps.append(d)
            d.need = True
        for k in W:
            self.last_w[k] = op
            self.readers[k] = []
        for k in R:
            if k not in W:
                self.readers.setdefault(k, []).append(op)
        self.ops.append(op)
        return op

    def emit(self):
        c = self.ctx
        nc = self.nc
        per_eng = {e: [] for e in ENGS}
        for op in self.ops:
            pre = None
            if op.dma:
                k = c.dcnt[op.eng]
                c.dcnt[op.eng] += 1
                s = c.dsem[op.eng][k % NDS]
                op.sig = (s, 16 * (k // NDS + 1))
                if k >= NDS:
                    pre = (s, 16 * (k // NDS))
            elif op.need:
                c.ecnt[op.eng] += 1
                op.sig = (c.esem[op.eng], c.ecnt[op.eng])
            w = c.waited[op.eng]
            lst = [pre] if pre is not None else []
            lst += [d.sig for d in op.deps]
            for s, v in lst:
                if w.get(id(s), 0) >= v:
                    continue
                w[id(s)] = v
                op.waits.append((s, v))
            per_eng[op.eng].append(op)
        drains = {e: [] for e in ENGS}
        for e in c.dsem:
            for i, s in enumerate(c.dsem[e]):
                n = (c.dcnt[e] - i + NDS - 1) // NDS if c.dcnt[e] > i else 0
                if n > 0 and c.waited[e].get(id(s), 0) < 16 * n:
                    c.waited[e][id(s)] = 16 * n
                    drains[e].append((s, 16 * n))

        def run(en):
            def body(eng):
                for op in per_eng[en]:
                    for s, v in op.waits:
                        eng.wait_ge(s, v)
                    ins = op.fn(eng)
                    if op.sig is not None:
                        ins.then_inc(op.sig[0], 16 if op.dma else 1)
                for s, v in drains[en]:
                    eng.wait_ge(s, v)
            return body

        with nc.Block() as block:
            block.tensor(run(PE))
            block.scalar(run(ACT))
            block.vector(run(DVE))
            block.gpsimd(run(POOL))
            block.sync(run(SP))
        self.es.close()
        c.nphase += 1


def mm(ph, out, lhsT, rhs, start, stop, R, W):
    return ph.add(PE, lambda e: e.matmul(out, lhsT=lhsT, rhs=rhs, start=start, stop=stop), R, W)


def tr(ph, out, in_, ident, R, W):
    return ph.add(PE, lambda e: e.transpose(out=out, in_=in_, identity=ident), R, W)


def act(ph, out, in_, func, R, W, **kw):
    return ph.add(ACT, lambda e: e.activation(out=out, in_=in_, func=func, **kw), R, W)


def tt(ph, out, a, b, op, R, W, eng=DVE):
    return ph.add(eng, lambda e: e.tensor_tensor(out=out, in0=a, in1=b, op=op), R, W)


def ts(ph, out, a, s1, s2, op0, op1, R, W, eng=DVE):
    if s2 is None:
        return ph.add(eng, lambda e: e.tensor_scalar(out=out, in0=a, scalar1=s1, scalar2=None, op0=op0), R, W)
    return ph.add(eng, lambda e: e.tensor_scalar(out=out, in0=a, scalar1=s1, scalar2=s2, op0=op0, op1=op1), R, W)


def stt(ph, out, a, s, b, op0, op1, R, W, accum=None):
    if accum is None:
        return ph.add(DVE, lambda e: e.scalar_tensor_tensor(out=out, in0=a, scalar=s, in1=b, op0=op0, op1=op1), R, W)
    return ph.add(DVE, lambda e: e.scalar_tensor_tensor(out=out, in0=a, scalar=s, in1=b, op0=op0, op1=op1, accum_out=accum), R, W)


def cp(ph, out, in_, R, W, eng=DVE):
    return ph.add(eng, lambda e: e.tensor_copy(out=out, in_=in_), R, W)


def ms(ph, ap, val, W, eng=DVE):
    return ph.add(eng, lambda e: e.memset(ap, val), (), W)


def dma(ph, out, in_, R, W, eng=SP):
    return ph.add(eng, lambda e: e.dma_start(out=out, in_=in_), R, W, dma=True)


def v4(ap):
    return ap.rearrange("p (a b) -> p a b", b=128)


class K:
    pass


def gelu_tanh(ph, x, out, tmp, tk, R, W):
    tt(ph, tmp, x, x, ALU.mult, R, [tk], eng=POOL)
    ts(ph, tmp, tmp, 0.044715 * 1.5957691216057308, 1.5957691216057308, ALU.mult, ALU.add, [tk], [tk])
    tt(ph, tmp, tmp, x, ALU.mult, [tk] + list(R), [tk], eng=POOL)
    act(ph, tmp, tmp, AF.Sigmoid, [tk], [tk])
    tt(ph, out, tmp, x, ALU.mult, [tk] + list(R), W)


def norm_tile(k, ph, nb, t, layer, Amod, SHmod, nT_out, nT_key, extra_f32=None, mixed=False, prefetched=False):
    s = t % len(nb["ht"])
    ht = nb["ht"][s]
    hk = "ht%d" % s
    if not prefetched:
        dma(ph, ht[:], k.hsrc(layer, t, mixed), [], [hk])
    st = nb["stat"]
    sk = "stat"
    act(ph, nb["sq"][:], ht[:], AF.Square, [hk], ["sq", sk], accum_out=st[:, 0:1])
    act(ph, st[:, 1:2], st[:, 0:1], AF.Ln, [sk], [sk], scale=1.0 / D, bias=k.eps_t[:, 0:1])
    act(ph, st[:, 2:3], st[:, 1:2], AF.Exp, [sk], [sk], scale=-0.5)
    stt(ph, nb["tmp"][:], ht[:], st[:, 2:3], Amod, ALU.mult, ALU.mult, [hk, sk, "MOD"], ["tmp"])
    tt(ph, nb["nrm"][:], nb["tmp"][:], SHmod, ALU.add, ["tmp", "MOD"], ["nrm"])
    for j in range(8):
        b = j // 4
        tr(ph, k.ps[b][:, (j % 4) * 128:(j % 4 + 1) * 128], nb["nrm"][:, j * 128:(j + 1) * 128], k.ident, ["nrm"], ["ps%d" % b])
    act(ph, nT_out[:, 0:4, :], v4(k.ps[0][:, :]), AF.Copy, ["ps0"], [nT_key])
    cp(ph, nT_out[:, 4:8, :], v4(k.ps[1][:, :]), ["ps1"], [nT_key])
    if extra_f32 is not None:
        cp(ph, extra_f32[0][:, 0:4, :], v4(k.ps[0][:, :]), ["ps0"], [extra_f32[1]])
        act(ph, extra_f32[0][:, 4:8, :], v4(k.ps[1][:, :]), AF.Copy, ["ps1"], [extra_f32[1]])
    return hk


def norm_bufs(ph):
    return {
        "ht": [ph.sb("ht0", [128, D], F32), ph.sb("ht1", [128, D], F32)],
        "sq": ph.sb("sq", [128, D], BF16),
        "stat": ph.sb("stat", [128, 4], F32),
        "tmp": ph.sb("tmp", [128, D], F32),
        "nrm": ph.sb("nrm", [128, D], F32),
    }


def phase_setup(k):
    ph = Phase(k.ctx)
    dma(ph, k.cst[:], k.d["cst"][:, :], [], ["cst"])
    ms(ph, k.eps_t[:], EPS, ["eps"])
    craw = ph.sb("craw", [8, 2, 128], F32)
    dma(ph, craw[:, 0, :], k.d["c"].rearrange("(k p) -> k p", p=128), [], ["craw"])
    dma(ph, craw[:, 1, :], k.d["c_ctx"].rearrange("(k p) -> k p", p=128), [], ["craw"])
    cT = ph.sb("cT", [128, 2, 8], F32)
    for lc in range(2):
        tr(ph, k.ps[0][:, lc * 8:(lc + 1) * 8], craw[:, lc, :], k.cst[0:8, 0:8], ["craw", "cst"], ["ps0"])
    act(ph, cT[:].rearrange("p a b -> p (a b)"), k.ps[0][:, 0:16], AF.Silu, ["ps0"], ["cT"])
    for lc in range(2):
        for j in range(8):
            act(ph, k.condrep[:, lc, j, :], k.ones, AF.Copy, ["cT", "cst"], ["condrep"], scale=cT[:, lc, j:j + 1])
    ph.emit()


def phase_mod(k, layer, half):
    ph = Phase(k.ctx)
    wm = [ph.sb("wm0", [128, 8, 512], BF16), ph.sb("wm1", [128, 8, 512], BF16)]
    mb = [ph.sb("mb0", [128, 512], F32), ph.sb("mb1", [128, 512], F32)]
    gb = ph.sb("gb", [128, D], F32)
    gsrc = k.d["norm_mix_g"] if half == 0 else k.d["norm_ffn_g"]
    wsrc = k.d["mod_w"][layer].rearrange("(k p) n -> p k n", p=128)
    for blk in range(6):
        c0 = half * 3072 + blk * 512
        s = blk % 2
        dma(ph, wm[s][:], wsrc[:, :, c0:c0 + 512], [], ["wm%d" % s], eng=POOL)
        if blk == 0:
            dma(ph, gb[:], gsrc[layer].partition_broadcast(128), [], ["gb"])
        dma(ph, mb[s][:], k.d["mod_b"][layer, c0:c0 + 512].partition_broadcast(128), [], ["mb%d" % s])
        for lc in range(2):
            b = (blk * 2 + lc) % 4
            for j in range(8):
                mm(ph, k.ps[b][:, :], k.condrep[:, lc, j, :], wm[s][:, j, :], j == 0, j == 7, ["wm%d" % s], ["ps%d" % b])
            tt(ph, k.MOD[:, lc, blk // 2, (blk % 2) * 512:(blk % 2 + 1) * 512], k.ps[b][:, :], mb[s][:], ALU.add,
               ["ps%d" % b, "mb%d" % s], ["MOD"])
    for lc in range(2):
        stt(ph, k.MOD[:, lc, 1, :], k.MOD[:, lc, 1, :], 1.0, gb[:], ALU.add, ALU.mult, ["MOD", "gb"], ["MOD"])
    ph.emit()


def phase_even(k, layer, i, direction):
    full = direction == 0
    d = direction
    ph = Phase(k.ctx)
    import os
    if os.environ.get("KCUT"):
        ph.limit = int(os.environ["KCUT"])
    ps = k.ps
    nb = norm_bufs(ph)
    nb["ht"].append(ph.sb("ht2", [128, D], F32))
    win = k.win
    if not full:
        wsrc = k.d["ev_w_in"][i].rearrange("(k p) n -> p k n", p=128)
        dma(ph, win[:, :, 1024:2048], wsrc[:, :, 1024:2048], [], ["win"], eng=POOL)
        dma(ph, win[:, :, 2560:2592], wsrc[:, :, 2560:2592], [], ["win"], eng=POOL)
        dma(ph, win[:, :, 0:1024], wsrc[:, :, 0:1024], [], ["win2"], eng=POOL)
        dma(ph, win[:, :, 2048:2560], wsrc[:, :, 2048:2560], [], ["win2"], eng=POOL)
    gateW = ph.sb("gateW", [33, 512], F32)
    ms(ph, gateW[:], 0.0, ["gateW"])
    for dd in range(2):
        dma(ph, gateW[dd * 16:(dd + 1) * 16, dd * 256:(dd + 1) * 256], k.d["b_gate_w"][i, dd], [], ["gateW"])
    dma(ph, gateW[32:33, :], k.d["b_gate_b"][i].rearrange("a b -> (a b)").rearrange("(o n) -> o n", o=1), [], ["gateW"])
    gfa = ph.sb("gfa", [33, 128], F32)
    ms(ph, gfa[32:33, :], 1.0, ["gfa1"])
    S32 = ph.sb("S32", [128, 2, 256], F32)
    Sbf = ph.sb("Sbf", [128, 2, 256], BF16)
    ms(ph, S32[:], 0.0, ["S32"])
    ms(ph, Sbf[:], 0.0, ["Sbf"])
    nT = [ph.sb("nT0", [128, 8, 128], BF16), ph.sb("nT1", [128, 8, 128], BF16)]
    spt = ph.sb("spt", [128, 256], F32)
    Et = ph.sb("Et", [128, 256], F32)
    Eit = ph.sb("Eit", [128, 256], F32)
    ERt = ph.sb("ERt", [128, 256], F32)
    qdTm = ph.sb("qdTm", [128, 2, 2, 128], BF16)
    ms(ph, qdTm[:], 0.0, ["qdT"])
    kiT = ph.sb("kiT", [128, 256], BF16)
    krt = ph.sb("krt", [128, 256], BF16)
    vb = ph.sb("vb", [128, 512], BF16)
    sTt = ph.sb("sTt", [128, 512], BF16)
    if full:
        wout = ph.sb("wout", [128, 8, D], BF16)
        dma(ph, wout[:], k.d["ev_w_out"][i].rearrange("(k p) n -> p k n", p=128), [], ["wout"], eng=POOL)
        wsr = ph.sb("wsr", [128, 4, 128], F32)
        dma(ph, wsr[:], k.d["a_ws"][i].rearrange("h p q -> p h q"), [], ["wsr"])
        wsT = ph.sb("wsT", [128, 4, 128], BF16)
        for h in range(4):
            tr(ph, ps[2][:, h * 128:(h + 1) * 128], wsr[:, h, :], k.ident, ["wsr"], ["ps2"])
        cp(ph, wsT[:].rearrange("p a b -> p (a b)"), ps[2][:, :], ["ps2"], ["wsT"])
        bsB = ph.sb("bsB", [128, 512], F32)
        dma(ph, bsB[:], k.d["a_bs"][i].rearrange("a b -> (a b)").partition_broadcast(128), [], ["bsB"])
        lng = ph.sb("lng", [128, 512], F32)
        lnb = ph.sb("lnb", [128, 512], F32)
        dma(ph, lng[:], k.d["a_ln_g"][i].partition_broadcast(128), [], ["lng"])
        dma(ph, lnb[:], k.d["a_ln_b"][i].partition_broadcast(128), [], ["lnb"])
        bng = ph.sb("bng", [128, 4, 128], F32)
        for h in range(4):
            dma(ph, bng[:, h, :], k.d["b_norm_g"][i].partition_broadcast(128), [], ["bng"])
        xa = ph.sb("xa", [128, 512], F32)
        xt1 = ph.sb("xt1", [128, 512], F32)
        gv = ph.sb("gv", [128, 512], F32)
        bst = ph.sb("bst", [128, 8], F32)
        ost = ph.sb("ost", [128, 12], F32)
        ybt = ph.sb("ybt", [128, 512], F32)
        yT = ph.sb("yT", [128, 8, 128], BF16)
        ho = ph.sb("ho", [128, D], F32)
        osq = ph.sb("osq", [128, 128], BF16)
    TRI = k.TRI[d]
    STRI = k.STRI[d]
    MASK = k.MASK4[d]
    dcol = 127 if d == 0 else 0
    order = list(range(NT)) if d == 0 else [1, 0] + list(range(17, 1, -1))
    if full:
        xa2 = ph.sb("xa2", [128, 512], F32)
        sgr = ph.sb("sgr", [128, 512], F32)

    def rstd_ln_exp(dst, src, n, scale, R, W):
        act(ph, dst, src, AF.Ln, R, W, scale=scale, bias=k.eps_t[:, 0:1])
        act(ph, dst, dst, AF.Exp, W, W, scale=-0.5)

    if full:
        osum2 = [ph.sb("osum_a", [128, 512], F32), ph.sb("osum_b", [128, 512], F32)]
        srg2 = [ph.sb("srg_a", [128, 512], F32), ph.sb("srg_b", [128, 512], F32)]
        uT2 = [ph.sb("uT_a", [128, 512], F32), ph.sb("uT_b", [128, 512], F32)]
        vA2 = [ph.sb("vA_a", [128, 512], BF16), ph.sb("vA_b", [128, 512], BF16)]
        xt2 = ph.sb("xt2", [128, 512], F32)

    def tail(tp):
        o_ = tp % 2
        lcp = 1 if tp < 2 else 0
        for h in range(4):
            mm(ph, ps[3][:, h * 128:(h + 1) * 128], vA2[o_][:, h * 128:(h + 1) * 128], wsT[:, h, :], True, True,
               ["vA%d" % o_, "wsT"], ["ps3"])
        tt(ph, xt2[:], ps[3][:, :], bsB[:], ALU.add, ["ps3", "bsB"], ["xt2"])
        tt(ph, yT[:, 0:4, :].rearrange("p a b -> p (a b)"), xt2[:], uT2[o_][:], ALU.mult, ["xt2", "uT%d" % o_], ["yT"])
        for h in range(4):
            act(ph, osq[:], osum2[o_][:, h * 128:(h + 1) * 128], AF.Square, ["osum%d" % o_], ["osq", "ost"], accum_out=ost[:, h:h + 1])
        rstd_ln_exp(ost[:, 8:12], ost[:, 0:4], 4, 1.0 / 128, ["ost"], ["ost"])
        for h in range(4):
            stt(ph, ybt[:, h * 128:(h + 1) * 128], osum2[o_][:, h * 128:(h + 1) * 128], ost[:, 8 + h:9 + h],
                srg2[o_][:, h * 128:(h + 1) * 128], ALU.mult, ALU.mult, ["osum%d" % o_, "ost", "srg%d" % o_], ["ybt"])
        for c in range(4):
            tr(ph, ps[5][:, c * 128:(c + 1) * 128], ybt[:, c * 128:(c + 1) * 128], k.ident, ["ybt"], ["ps5"])
        act(ph, yT[:, 4:8, :], v4(ps[5][:, :]), AF.Copy, ["ps5"], ["yT"])
        for half in range(2):
            b = 6 if half == 0 else 0
            for j in range(8):
                mm(ph, ps[b][:, :], yT[:, j, :], wout[:, j, half * 512:(half + 1) * 512], j == 0, j == 7, ["yT", "wout"], ["ps%d" % b])
            tt(ph, ho[:, half * 512:(half + 1) * 512], ps[b][:, :], k.MOD[:, lcp, 2, half * 512:(half + 1) * 512], ALU.mult,
               ["ps%d" % b, "MOD"], ["ho"])
        tt(ph, ho[:], ho[:], nb["ht"][tp % 3][:], ALU.add, ["ho", "ht%d" % (tp % 3)], ["ho"])
        dma(ph, k.hbuf[tp * 128:(tp + 1) * 128, :], ho[:], ["ho"], [])

    def do_norm(tt_):
        lc_ = 1 if tt_ < 2 else 0
        return norm_tile(k, ph, nb, tt_, layer, k.MOD[:, lc_, 1, :], k.MOD[:, lc_, 0, :], nT[tt_ % 2], "nT%d" % (tt_ % 2),
                         prefetched=True)

    dma(ph, nb["ht"][order[0] % 3][:], k.hsrc(layer, order[0]), [], ["ht%d" % (order[0] % 3)])
    do_norm(order[0])
    for oi, t in enumerate(order):
        lc = 1 if t < 2 else 0
        nt_ = nT[t % 2]
        nk = "nT%d" % (t % 2)
        hk = "ht%d" % (t % 3)
        tn = order[oi + 1] if oi + 1 < len(order) else None
        if tn is not None:
            dma(ph, nb["ht"][tn % 3][:], k.hsrc(layer, tn), [], ["ht%d" % (tn % 3)])
        for c in range(4):
            for j in range(8):
                mm(ph, ps[2][:, c * 128:(c + 1) * 128], win[:, j, 1024 + c * 128:1024 + (c + 1) * 128], nt_[:, j, :],
                   j == 0, j == 7, ["win", nk], ["ps2"])
        for j in range(8):
            mm(ph, ps[3][0:32, 0:128], win[:, j, 2560:2592], nt_[:, j, :], j == 0, j == 7, ["win", nk], ["ps3"])
        for j in range(8):
            mm(ph, ps[4][:, 0:256], nt_[:, j, :], win[:, j, 1280:1536], j == 0, j == 7, ["win", nk], ["ps4"])
        for j in range(8):
            mm(ph, ps[5][:, :], nt_[:, j, :], win[:, j, 1536:2048], j == 0, j == 7, ["win", nk], ["ps5"])
        if full:
            for j in range(8):
                mm(ph, ps[6][:, :], nt_[:, j, :], win[:, j, 2048:2560], j == 0, j == 7, ["win", nk], ["ps6"])
            for j in range(8):
                mm(ph, ps[7][:, :], nt_[:, j, :], win[:, j, 512:1024], j == 0, j == 7, ["win", nk], ["ps7"])
            for c in range(4):
                for j in range(8):
                    mm(ph, ps[0][:, c * 128:(c + 1) * 128], win[:, j, c * 128:(c + 1) * 128], nt_[:, j, :],
                       j == 0, j == 7, ["win", nk], ["ps0"])
        act(ph, gfa[0:32, :], ps[3][0:32, 0:128], AF.Copy, ["ps3"], ["gfa0"])
        mm(ph, ps[1][:, 0:256], gfa[0:33, :], gateW[0:33, d * 256:(d + 1) * 256], True, True, ["gfa0", "gfa1", "gateW"], ["ps1"])
        act(ph, spt[:], ps[1][:, 0:256], AF.Exp, ["ps1"], ["spt"], scale=-1.0)
        act(ph, spt[:], spt[:], AF.Ln, ["spt"], ["spt"], bias=k.one_t[:, 0:1])
        for hp in range(2):
            mm(ph, ps[3][:, hp * 128:(hp + 1) * 128], spt[:, hp * 128:(hp + 1) * 128], TRI, True, True, ["spt", "cst"], ["ps3"])
        mm(ph, ps[1][:, 256:512], STRI, spt[:, :], True, True, ["spt", "cst"], ["ps1"])
        act(ph, Et[:], ps[3][:, 0:256], AF.Exp, ["ps3"], ["Et"])
        act(ph, Eit[:], ps[3][:, 0:256], AF.Exp, ["ps3"], ["Eit"], scale=-1.0)
        act(ph, ERt[:], ps[1][:, 256:512], AF.Exp, ["ps1"], ["ERt"])
        for hl in range(2):
            r0, r1 = hl * 64, (hl + 1) * 64
            stt(ph, qdTm[r0:r1, :, hl, :], ps[2][r0:r1, 0:256].rearrange("p (a b) -> p a b", b=128), 0.125,
                Et[r0:r1, :].rearrange("p (a b) -> p a b", b=128), ALU.mult, ALU.mult, ["ps2", "Et"], ["qdT"])
        tt(ph, kiT[:], ps[2][:, 256:512], Eit[:], ALU.mult, ["ps2", "Eit"], ["kiT"])
        tt(ph, krt[:], ps[4][:, 0:256], ERt[:], ALU.mult, ["ps4", "ERt"], ["krt"])
        act(ph, vb[:], ps[5][:, :], AF.Copy, ["ps5"], ["vb"])
        for h in range(4):
            hp, hl = h // 2, h % 2
            mm(ph, ps[4][:, h * 128:(h + 1) * 128], kiT[:, hp * 128:(hp + 1) * 128],
               qdTm[:, hp, hl, :], True, True, ["kiT", "qdT"], ["ps4"])
        tt(ph, sTt[:], ps[4][:, :], MASK, ALU.mult, ["ps4", "cst"], ["sTt"])
        for h in range(4):
            hp, hl = h // 2, h % 2
            mm(ph, ps[2][:, h * 128:(h + 1) * 128], sTt[:, h * 128:(h + 1) * 128], vb[:, h * 128:(h + 1) * 128],
               True, False, ["sTt", "vb"], ["ps2"])
            mm(ph, ps[2][:, h * 128:(h + 1) * 128], qdTm[:, hp, hl, :],
               Sbf[:, hp, hl * 128:(hl + 1) * 128], False, True, ["qdT", "Sbf"], ["ps2"])
        csb = 7 if not full else 1
        for hp in range(2):
            mm(ph, ps[csb][:, hp * 256:(hp + 1) * 256], krt[:, hp * 128:(hp + 1) * 128], vb[:, hp * 256:(hp + 1) * 256],
               True, True, ["krt", "vb"], ["ps%d" % csb])
        for hp in range(2):
            stt(ph, S32[:, hp, :], S32[:, hp, :], Et[:, hp * 128 + dcol:hp * 128 + dcol + 1], ps[csb][:, hp * 256:(hp + 1) * 256],
                ALU.mult, ALU.add, ["S32", "Et", "ps%d" % csb], ["S32"])
        act(ph, Sbf[:].rearrange("p a b -> p (a b)"), S32[:].rearrange("p a b -> p (a b)"), AF.Copy, ["S32"], ["Sbf"])
        if not full:
            cp(ph, k.OB[:, t, :], ps[2][:, :], ["ps2"], ["OB%d" % t])
            if tn is not None:
                do_norm(tn)
            continue
        ob_ = t % 2
        tt(ph, osum2[ob_][:], ps[2][:, :], k.OB[:, t, :], ALU.add, ["ps2"], ["osum%d" % ob_])
        act(ph, xa[:], ps[0][:, :], AF.Copy, ["ps0"], ["xa"])
        act(ph, xa2[:], ps[7][:, :], AF.Copy, ["ps7"], ["xa2"])
        act(ph, sgr[:], ps[6][:, :], AF.Sigmoid, ["ps6"], ["sgr"])
        stt(ph, srg2[ob_][:], ps[6][:, :], 1.0, bng[:].rearrange("p a b -> p (a b)"), ALU.mult, ALU.mult, ["ps6", "bng"],
            ["srg%d" % ob_])
        if oi >= 1:
            tail(order[oi - 1])
        if tn is not None:
            do_norm(tn)
        tt(ph, srg2[ob_][:], srg2[ob_][:], sgr[:], ALU.mult, ["srg%d" % ob_, "sgr"], ["srg%d" % ob_], eng=POOL)
        tt(ph, xt1[:], xa[:], xa[:], ALU.mult, ["xa"], ["xt1"])
        ts(ph, xt1[:], xt1[:], 0.044715 * 1.5957691216057308, 1.5957691216057308, ALU.mult, ALU.add, ["xt1"], ["xt1"])
        tt(ph, xt1[:], xt1[:], xa[:], ALU.mult, ["xt1", "xa"], ["xt1"])
        tt(ph, gv[:], xa2[:], xa2[:], ALU.mult, ["xa2"], ["gv"], eng=POOL)
        ts(ph, gv[:], gv[:], 0.044715 * 1.5957691216057308, 1.5957691216057308, ALU.mult, ALU.add, ["gv"], ["gv"])
        tt(ph, gv[:], gv[:], xa2[:], ALU.mult, ["gv", "xa2"], ["gv"], eng=POOL)
        act(ph, xt1[:], xt1[:], AF.Sigmoid, ["xt1"], ["xt1"])
        act(ph, gv[:], gv[:], AF.Sigmoid, ["gv"], ["gv"])
        tt(ph, uT2[ob_][:], xt1[:], xa[:], ALU.mult, ["xt1", "xa"], ["uT%d" % ob_])
        tt(ph, gv[:], gv[:], xa2[:], ALU.mult, ["gv", "xa2"], ["gv"])
        ph.add(DVE, lambda e: e.bn_stats(out=bst[:, 0:6], in_=gv[:]), ["gv"], ["bst"])
        ph.add(DVE, lambda e: e.bn_aggr(out=bst[:, 6:8], in_=bst[:, 0:6]), ["bst"], ["bst"])
        rstd_ln_exp(bst[:, 1:2], bst[:, 7:8], 1, 1.0, ["bst"], ["bst"])
        ts(ph, gv[:], gv[:], bst[:, 6:7], bst[:, 1:2], ALU.subtract, ALU.mult, ["gv", "bst"], ["gv"])
        tt(ph, gv[:], gv[:], lng[:], ALU.mult, ["gv", "lng"], ["gv"], eng=POOL)
        tt(ph, vA2[ob_][:], gv[:], lnb[:], ALU.add, ["gv", "lnb"], ["vA%d" % ob_])
    if full:
        tail(order[-1])
    ph.emit()


def phase_mla1(k, layer, i):
    import os
    ph = Phase(k.ctx)
    if os.environ.get("KCUT1"):
        ph.limit = int(os.environ["KCUT1"])
    ps = k.ps
    nb = norm_bufs(ph)
    wi = ph.sb("wi", [128, 8, 576], BF16)
    dma(ph, wi[:, :, 0:544], k.d["od_w_in"][i].rearrange("(k p) n -> p k n", p=128), [], ["wi"], eng=POOL)
    for g in range(2):
        cp(ph, wi[:, :, 544 + g * 16:544 + g * 16 + 8], wi[:, :, 512 + g * 16 + 8:512 + g * 16 + 16], ["wi"], ["wi"])
        cp(ph, wi[:, :, 544 + g * 16 + 8:544 + g * 16 + 16], wi[:, :, 512 + g * 16:512 + g * 16 + 8], ["wi"], ["wi"])
    wkv = ph.sb("wkv", [128, 2, 2048], BF16)
    wkvsrc = k.d["c_w_ukv"][i].rearrange("(k p) n -> p k n", p=128)
    for hh_ in range(2):
        dma(ph, wkv[:, :, hh_ * 1024:(hh_ + 1) * 1024], wkvsrc[:, :, hh_ * 1024:(hh_ + 1) * 1024], [], ["wkv"], eng=POOL)
    gq = ph.sb("gq", [128, 512], F32)
    dma(ph, gq[:, 0:256], k.d["c_q_norm_g"][i].partition_broadcast(128), [], ["gq"])
    dma(ph, gq[:, 256:512], k.d["c_kv_norm_g"][i].partition_broadcast(128), [], ["gq"])
    nT = [ph.sb("nT0", [128, 8, 128], BF16), ph.sb("nT1", [128, 8, 128], BF16)]
    cst2 = ph.sb("cst2", [128, 8], F32)
    cqn = ph.sb("cqn", [128, 512], F32)
    csq = ph.sb("csq", [128, 256], BF16)
    ckT = ph.sb("ckT", [128, 2, 128], BF16)
    ra = ph.sb("ra", [32, 128], F32)
    rb = ph.sb("rb", [32, 128], F32)
    rC = [ph.sb("rC0", [32, 128], F32), ph.sb("rC1", [32, 128], F32)]
    rS = [ph.sb("rS0", [32, 128], F32), ph.sb("rS1", [32, 128], F32)]
    def do_norm1(tt_):
        lc_ = 1 if tt_ < 2 else 0
        norm_tile(k, ph, nb, tt_, layer, k.MOD[:, lc_, 1, :], k.MOD[:, lc_, 0, :], nT[tt_ % 2], "nT%d" % (tt_ % 2))

    do_norm1(0)
    for t in range(NT):
        lc = 1 if t < 2 else 0
        nt_ = nT[t % 2]
        nk = "nT%d" % (t % 2)
        for j in range(8):
            mm(ph, ps[2][:, :], nt_[:, j, :], wi[:, j, 0:512], j == 0, j == 7, ["wi", nk], ["ps2"])
        for j in range(8):
            mm(ph, ps[3][0:32, 0:128], wi[:, j, 512:544], nt_[:, j, :], j == 0, j == 7, ["wi", nk], ["ps3"])
        for j in range(8):
            mm(ph, ps[3][0:32, 128:256], wi[:, j, 544:576], nt_[:, j, :], j == 0, j == 7, ["wi", nk], ["ps3"])
        for g in range(2):
            act(ph, csq[:], ps[2][:, g * 256:(g + 1) * 256], AF.Square, ["ps2"], ["csq", "cst2"], accum_out=cst2[:, g:g + 1])
        act(ph, cst2[:, 2:4], cst2[:, 0:2], AF.Sqrt, ["cst2"], ["cst2"], scale=1.0 / 256, bias=k.eps_t[:, 0:1])
        ph.add(DVE, lambda e: e.reciprocal(out=cst2[:, 4:6], in_=cst2[:, 2:4]), ["cst2"], ["cst2"])
        for g in range(2):
            stt(ph, cqn[:, g * 256:(g + 1) * 256], ps[2][:, g * 256:(g + 1) * 256], cst2[:, 4 + g:5 + g], gq[:, g * 256:(g + 1) * 256],
                ALU.mult, ALU.mult, ["ps2", "cst2", "gq"], ["cqn"])
        for c in range(4):
            tr(ph, ps[4][:, c * 128:(c + 1) * 128], cqn[:, c * 128:(c + 1) * 128], k.ident, ["cqn"], ["ps4"])
        if t + 1 < NT:
            do_norm1(t + 1)
        act(ph, k.CQT[:, :, t * 128:(t + 1) * 128], v4(ps[4][:, :])[:, 0:2, :], AF.Copy, ["ps4"], ["CQT"])
        cp(ph, ckT[:], v4(ps[4][:, :])[:, 2:4, :], ["ps4"], ["ckT"])
        if t >= 2:
            tl = (t - 2) * 128
            rs_ = t % 2
            dma(ph, rC[rs_][:], k.d["ropeC"][:, tl:tl + 128], [], ["rC%d" % rs_])
            dma(ph, rS[rs_][:], k.d["ropeS"][:, tl:tl + 128], [], ["rS%d" % rs_])
            tt(ph, ra[:], ps[3][0:32, 0:128], rC[rs_][:], ALU.mult, ["ps3", "rC%d" % rs_], ["ra"])
            tt(ph, rb[:], ps[3][0:32, 128:256], rS[rs_][:], ALU.mult, ["ps3", "rS%d" % rs_], ["rb"])
            tt(ph, k.KR[:, t * 128:(t + 1) * 128], ra[:], rb[:], ALU.add, ["ra", "rb"], ["KR"], eng=POOL)
        else:
            cp(ph, k.KR[:, t * 128:(t + 1) * 128], ps[3][0:32, 0:128], ["ps3"], ["KR"])
        for g in range(4):
            b = 5 + (g % 2)
            for hh in range(4):
                h = g * 4 + hh
                po = (h // 8) * 64
                for kc in range(2):
                    mm(ph, ps[b][po:po + 64, hh * 128:(hh + 1) * 128], wkv[:, kc, h * 128:h * 128 + 64], ckT[:, kc, :],
                       kc == 0, kc == 1, ["wkv", "ckT"], ["ps%d" % b])
            po = (g // 2) * 64
            s0 = (g % 2) * 4
            eng_cp = act if g % 2 == 0 else None
            if g % 2 == 0:
                act(ph, k.KN[po:po + 64, s0:s0 + 4, t * 128:(t + 1) * 128], v4(ps[b][po:po + 64, :]), AF.Copy, ["ps%d" % b], ["KN"])
            else:
                cp(ph, k.KN[po:po + 64, s0:s0 + 4, t * 128:(t + 1) * 128], v4(ps[b][po:po + 64, :]), ["ps%d" % b], ["KN"])
        for half in range(2):
            b = 7 if half == 0 else 2
            rhs_v = wkv[:, :, half * 1024:(half + 1) * 1024].rearrange("p k (h x) -> p k h x", x=128)
            for kc in range(2):
                mm(ph, ps[b][:, :].rearrange("p (h x) -> p h x", x=64), ckT[:, kc, :], rhs_v[:, kc, :, 64:128], kc == 0, kc == 1,
                   ["wkv", "ckT"], ["ps%d" % b])
            if half == 0:
                act(ph, k.VA[:, t, 0:8, :], ps[b][:, :].rearrange("p (h x) -> p h x", x=64), AF.Copy, ["ps%d" % b], ["VA"])
            else:
                cp(ph, k.VA[:, t, 8:16, :], ps[b][:, :].rearrange("p (h x) -> p h x", x=64), ["ps%d" % b], ["VA"])
    cp(ph, k.MG[:, 0, :], k.MOD[:, 0, 2, :], ["MOD"], ["MG"])
    cp(ph, k.MG[:, 1, :], k.MOD[:, 1, 2, :], ["MOD"], ["MG"])
    ph.emit()


def phase_mla2(k, layer, i, need_ctx):
    import os
    ph = Phase(k.ctx)
    if os.environ.get("KCUT2"):
        ph.limit = int(os.environ["KCUT2"])
    ps = k.ps
    wq = ph.sb("wq", [128, 2, 1536], BF16)
    wqsrc = k.d["c_w_uq"][i].rearrange("(k p) n -> p k n", p=128)
    for hh_ in range(2):
        dma(ph, wq[:, :, hh_ * 768:(hh_ + 1) * 768], wqsrc[:, :, hh_ * 768:(hh_ + 1) * 768], [], ["wq"], eng=POOL)
    wqs = ph.sb("wqs", [128, 2, 16, 32], BF16)
    wq4 = wq[:].rearrange("p k (h x) -> p k h x", x=96)
    for g in range(2):
        cp(ph, wqs[:, :, :, g * 16:g * 16 + 8], wq4[:, :, :, 64 + g * 16 + 8:64 + g * 16 + 16], ["wq"], ["wqs"])
        cp(ph, wqs[:, :, :, g * 16 + 8:g * 16 + 16], wq4[:, :, :, 64 + g * 16:64 + g * 16 + 8], ["wq"], ["wqs"])
    wo = ph.sb("wo", [64, 16, D], BF16)
    dma(ph, wo[:], k.d["od_w_out"][i].rearrange("(h d) n -> d h n", d=64), [], ["wo"], eng=POOL)
    OT = ph.sb("OT", [64, 16, 512], BF16)
    QN = [ph.sb("QN%d" % q_, [128, 512], BF16) for q_ in range(4)]
    for q_ in range(4):
        ms(ph, QN[q_][:], 0.0, ["QN%d" % q_])
    QR = [ph.sb("QR0", [32, 512], BF16), ph.sb("QR1", [32, 512], BF16)]
    qa = ph.sb("qa", [32, 512], F32)
    qb = ph.sb("qb", [32, 512], F32)
    pT = [ph.sb("pT%d" % q_, [128, 512], BF16) for q_ in range(3)]
    rinv = ph.sb("rinv", [64, 512], F32)
    ht = ph.sb("ht0", [128, D], F32)
    ho = ph.sb("ho0", [128, D], F32)
    rC = ph.sb("rC", [32, 512], F32)
    rS = ph.sb("rS", [32, 512], F32)
    blocks = ([(0, 2)] if need_ctx else []) + [(2 + 4 * b, 4) for b in range(4)]
    items = [(t0, ntile, h) for (t0, ntile) in blocks for h in range(16)]
    st = {"npt": 0}

    def stage_q(idx):
        t0, ntile, h = items[idx]
        nq, q0 = ntile * 128, t0 * 128
        po = (h // 8) * 64
        s = idx % 2
        qs = s + 2 * (h // 8)
        qn, qr = QN[qs], QR[s]
        if h == 0 and t0 >= 2:
            dma(ph, rC[:, 0:nq], k.d["ropeC"][:, q0 - 256:q0 - 256 + nq], [], ["rC"])
            dma(ph, rS[:, 0:nq], k.d["ropeS"][:, q0 - 256:q0 - 256 + nq], [], ["rS"])
        for kc in range(2):
            mm(ph, ps[0][po:po + 64, 0:nq], wq[:, kc, h * 96:h * 96 + 64], k.CQT[:, kc, q0:q0 + nq], kc == 0, kc == 1,
               ["wq", "CQT"], ["ps0"])
        for kc in range(2):
            mm(ph, ps[1][0:32, 0:nq], wq[:, kc, h * 96 + 64:h * 96 + 96], k.CQT[:, kc, q0:q0 + nq], kc == 0, kc == 1,
               ["wq", "CQT"], ["ps1"])
        act(ph, qn[po:po + 64, 0:nq], ps[0][po:po + 64, 0:nq], AF.Copy, ["ps0"], ["QN%d" % qs])
        if t0 >= 2:
            for kc in range(2):
                mm(ph, ps[2][0:32, 0:nq], wqs[:, kc, h, :], k.CQT[:, kc, q0:q0 + nq], kc == 0, kc == 1, ["wqs", "CQT"], ["ps2"])
            tt(ph, qa[:, 0:nq], ps[1][0:32, 0:nq], rC[:, 0:nq], ALU.mult, ["ps1", "rC"], ["qa"])
            tt(ph, qb[:, 0:nq], ps[2][0:32, 0:nq], rS[:, 0:nq], ALU.mult, ["ps2", "rS"], ["qb"])
            tt(ph, qr[:, 0:nq], qa[:, 0:nq], qb[:, 0:nq], ALU.add, ["qa", "qb"], ["QR%d" % s], eng=POOL)
        else:
            cp(ph, qr[:, 0:nq], ps[1][0:32, 0:nq], ["ps1"], ["QR%d" % s])

    def qk(idx, kt, slot):
        t0, ntile, h = items[idx]
        nq = ntile * 128
        s = idx % 2
        qs = s + 2 * (h // 8)
        b = 3 + slot
        mm(ph, ps[b][:, 0:nq], k.KN[:, h % 8, kt * 128:(kt + 1) * 128], QN[qs][:, 0:nq], True, False,
           ["KN", "QN%d" % qs], ["ps%d" % b])
        mm(ph, ps[b][:, 0:nq], k.KR[0:32, kt * 128:(kt + 1) * 128], QR[s][0:32, 0:nq], False, True,
           ["KR", "QR%d" % s], ["ps%d" % b])
        act(ph, pT[slot][:, 0:nq], ps[b][:, 0:nq], AF.Exp, ["ps%d" % b], ["pT%d" % slot], scale=C_SCALE)

    onesb = ph.sb("onesb", [128, 64], BF16)
    ms(ph, onesb[:], 1.0, ["onesb"])

    def pv(idx, kt, slot, nkt):
        t0, ntile, h = items[idx]
        nq = ntile * 128
        ob = 6 + idx % 2
        mm(ph, ps[ob][0:64, 0:nq], k.VA[:, kt, h, :], pT[slot][:, 0:nq], kt == 0, kt == nkt - 1,
           ["VA", "pT%d" % slot], ["ps%d" % ob])
        mm(ph, ps[ob][64:128, 0:nq], onesb[:, :], pT[slot][:, 0:nq], kt == 0, kt == nkt - 1,
           ["onesb", "pT%d" % slot], ["ps%d" % ob])

    def tail(idx):
        t0, ntile, h = items[idx]
        nq = ntile * 128
        ob = 6 + idx % 2
        ph.add(DVE, lambda e: e.reciprocal(out=rinv[0:64, 0:nq], in_=ps[ob][64:128, 0:nq]), ["ps%d" % ob], ["rinv"])
        tt(ph, OT[:, h, 0:nq], ps[ob][0:64, 0:nq], rinv[0:64, 0:nq], ALU.mult, ["ps%d" % ob, "rinv"], ["OT"])

    def outproj(t0, ntile):
        for ti in range(ntile):
            t = t0 + ti
            lc = 1 if t < 2 else 0
            dma(ph, ht[:], k.hsrc(layer, t), [], ["ht"])
            for half in range(2):
                b = half
                for h in range(16):
                    mm(ph, ps[b][:, :], OT[:, h, ti * 128:(ti + 1) * 128], wo[:, h, half * 512:(half + 1) * 512], h == 0, h == 15,
                       ["OT", "wo"], ["ps%d" % b])
                tt(ph, ho[:, half * 512:(half + 1) * 512], ps[b][:, :], k.MG[:, lc, half * 512:(half + 1) * 512], ALU.mult,
                   ["ps%d" % b, "MG"], ["ho"])
            tt(ph, ho[:], ho[:], ht[:], ALU.add, ["ho", "ht"], ["ho"], eng=POOL)
            dma(ph, k.hbuf[t * 128:(t + 1) * 128, :], ho[:], ["ho"], [])

    stage_q(0)
    pend_tail = None
    for idx in range(len(items)):
        t0, ntile, h = items[idx]
        nkt = 2 if t0 == 0 else NT
        base = st["npt"]
        DEPTH = 2
        for kt in range(min(DEPTH, nkt)):
            qk(idx, kt, (base + kt) % 3)
        if pend_tail is not None:
            tail(pend_tail)
            pt0, pnt, phh = items[pend_tail]
            if phh == 15:
                outproj(pt0, pnt)
            pend_tail = None
        if idx + 1 < len(items) and items[idx + 1][2] != 0:
            stage_q(idx + 1)
        for kt in range(nkt):
            if kt + DEPTH < nkt:
                qk(idx, kt + DEPTH, (base + kt + DEPTH) % 3)
            pv(idx, kt, (base + kt) % 3, nkt)
        st["npt"] = base + nkt
        if idx + 1 < len(items) and items[idx + 1][2] == 0:
            tail(idx)
            outproj(t0, ntile)
            stage_q(idx + 1)
        else:
            pend_tail = idx
    if pend_tail is not None:
        tail(pend_tail)
        pt0, pnt, phh = items[pend_tail]
        outproj(pt0, pnt)
    ph.emit()


def phase_ffn_norm(k, layer, moe, t_first, pre=None):
    ph = Phase(k.ctx)
    ps = k.ps
    nb = norm_bufs(ph)
    if pre is not None:
        i_ = layer // 2
        dma(ph, pre[0][:, :, 0:512], k.d["ff_w_gate"][i_].rearrange("(k p) n -> p k n", p=128)[:, :, 0:512], [], ["pre0"], eng=POOL)
        dma(ph, pre[1][:, :, 0:512], k.d["ff_w_up"][i_].rearrange("(k p) n -> p k n", p=128)[:, :, 0:512], [], ["pre1"], eng=POOL)
        dma(ph, pre[2][:, 0:4, :], k.d["ff_w_down"][i_].rearrange("(c p) n -> p c n", p=128)[:, 0:4, :], [], ["pre2"], eng=POOL)
    if moe:
        i = layer // 2
        rw = ph.sb("rw", [128, 8, 8], F32)
        dma(ph, rw[:], k.d["moe_router"][i].rearrange("(k p) n -> p k n", p=128), [], ["rw"])
        mT32 = [ph.sb("mT32a", [128, 8, 128], F32), ph.sb("mT32b", [128, 8, 128], F32)]
        lg = ph.sb("lg", [128, 8], F32)
        m8 = ph.sb("m8", [128, 8], F32)
        ex = ph.sb("ex", [128, 8], F32)
        ge = ph.sb("ge", [128, 8], F32)
        gs = ph.sb("gs", [128, 4], F32)

    def router(t):
        m32 = mT32[t % 2]
        mk = "mT32_%d" % (t % 2)
        for j in range(8):
            mm(ph, ps[2][:, 0:8], m32[:, j, :], rw[:, j, :], j == 0, j == 7, [mk, "rw"], ["ps2"])
        cp(ph, lg[:], ps[2][:, 0:8], ["ps2"], ["lg"])
        ph.add(DVE, lambda e: e.max(out=m8[:], in_=lg[:]), ["lg"], ["m8"])
        ts(ph, gs[:, 0:1], m8[:, 0:1], -1.0, None, ALU.mult, None, ["m8"], ["gs"])
        act(ph, ex[:], lg[:], AF.Exp, ["lg", "gs"], ["ex"], bias=gs[:, 0:1])
        stt(ph, ge[:], lg[:], m8[:, 1:2], ex[:], ALU.is_ge, ALU.mult, ["lg", "m8", "ex"], ["ge", "gs1"], accum=gs[:, 1:2])
        ph.add(DVE, lambda e: e.reciprocal(out=gs[:, 2:3], in_=gs[:, 1:2]), ["gs1"], ["gs2"])
        ts(ph, k.gates[:, t, :], ge[:], gs[:, 2:3], None, ALU.mult, None, ["ge", "gs2"], ["gates"])

    pend = None
    for t in range(t_first, NT):
        lc = 1 if t < 2 else 0
        norm_tile(k, ph, nb, t, layer, k.MOD[:, lc, 1, :], k.MOD[:, lc, 0, :], k.mT[:, :, t * 128:(t + 1) * 128], "mT",
                  extra_f32=(mT32[t % 2], "mT32_%d" % (t % 2)) if moe else None, mixed=True)
        if moe:
            if pend is not None:
                router(pend)
            pend = t
    if moe and pend is not None:
        router(pend)
    cp(ph, k.MG[:, 0, :], k.MOD[:, 0, 2, :], ["MOD"], ["MG"])
    cp(ph, k.MG[:, 1, :], k.MOD[:, 1, 2, :], ["MOD"], ["MG"])
    ph.emit()


def phase_ffn_experts(k, layer, moe, t_first, pre=None):
    ph = Phase(k.ctx)
    ps = k.ps
    i = layer // 2
    G = 4
    H = MOE_HIDDEN if moe else FF_HIDDEN
    nch = H // 128
    groups = []
    c0 = 0
    while c0 < nch:
        g = min(G, nch - c0)
        groups.append((c0, g))
        c0 += g
    if pre is None:
        wg = [ph.sb("wg0", [128, 8, G * 128], BF16), ph.sb("wg1", [128, 8, G * 128], BF16)]
        wu = [ph.sb("wu0", [128, 8, G * 128], BF16), ph.sb("wu1", [128, 8, G * 128], BF16)]
        wd = [ph.sb("wd0", [128, G, D], BF16), ph.sb("wd1", [128, G, D], BF16)]
    else:
        wg = [pre[0], ph.sb("wg1", [128, 8, G * 128], BF16)]
        wu = [pre[1], ph.sb("wu1", [128, 8, G * 128], BF16)]
        wd = [pre[2], ph.sb("wd1", [128, G, D], BF16)]
    hT = [ph.sb("hT0", [128, G, 512], BF16), ph.sb("hT1", [128, G, 512], BF16)]
    sg = [ph.sb("sg0", [128, 512], F32), ph.sb("sg1", [128, 512], F32)]
    ms(ph, k.acc[:], 0.0, ["acc"], eng=POOL)
    tblocks = []
    t = t_first
    while t < NT:
        n = min(4, NT - t)
        tblocks.append((t, n))
        t += n
    nexp = 8 if moe else 1
    work = [(e, c0, g) for e in range(nexp) for (c0, g) in groups]
    if moe:
        srcs = [(k.d["moe_w_gate"][i, e].rearrange("(k p) n -> p k n", p=128),
                 k.d["moe_w_up"][i, e].rearrange("(k p) n -> p k n", p=128),
                 k.d["moe_w_down"][i, e].rearrange("(c p) n -> p c n", p=128)) for e in range(8)]
    else:
        srcs = [(k.d["ff_w_gate"][i].rearrange("(k p) n -> p k n", p=128),
                 k.d["ff_w_up"][i].rearrange("(k p) n -> p k n", p=128),
                 k.d["ff_w_down"][i].rearrange("(c p) n -> p c n", p=128))]
    seq = [(wi_, tb) for wi_ in range(len(work)) for tb in range(len(tblocks))]
    cnt = {"ps": 0}

    def load(wi_):
        e, c0, g = work[wi_]
        s = wi_ % 2
        srcg, srcu, srcd = srcs[e]
        dma(ph, wg[s][:, :, 0:g * 128], srcg[:, :, c0 * 128:(c0 + g) * 128], [], ["wg%d" % s], eng=POOL)
        dma(ph, wu[s][:, :, 0:g * 128], srcu[:, :, c0 * 128:(c0 + g) * 128], [], ["wu%d" % s], eng=POOL)
        dma(ph, wd[s][:, 0:g, :], srcd[:, c0:c0 + g, :], [], ["wd%d" % s], eng=POOL)

    def gu(n):
        wi_, tb = seq[n]
        e, c0, g = work[wi_]
        s = wi_ % 2
        tb0, ntile = tblocks[tb]
        nq, q0 = ntile * 128, tb0 * 128
        hs = n % 2
        for c in range(g):
            bg = (cnt["ps"] % 2) * 2
            cnt["ps"] += 1
            for j in range(8):
                mm(ph, ps[bg][:, 0:nq], wg[s][:, j, c * 128:(c + 1) * 128], k.mT[:, j, q0:q0 + nq], j == 0, j == 7,
                   ["wg%d" % s, "mT"], ["ps%d" % bg])
            for j in range(8):
                mm(ph, ps[bg + 1][:, 0:nq], wu[s][:, j, c * 128:(c + 1) * 128], k.mT[:, j, q0:q0 + nq], j == 0, j == 7,
                   ["wu%d" % s, "mT"], ["ps%d" % (bg + 1)])
            sgt = sg[c % 2]
            act(ph, sgt[:, 0:nq], ps[bg][:, 0:nq], AF.Silu, ["ps%d" % bg], ["sg%d" % (c % 2)])
            tt(ph, hT[hs][:, c, 0:nq], sgt[:, 0:nq], ps[bg + 1][:, 0:nq], ALU.mult, ["sg%d" % (c % 2), "ps%d" % (bg + 1)],
               ["hT%d" % hs])

    hres = [ph.sb("hres0", [128, D], F32), ph.sb("hres1", [128, D], F32)]

    def down(n):
        wi_, tb = seq[n]
        e, c0, g = work[wi_]
        s = wi_ % 2
        tb0, ntile = tblocks[tb]
        hs = n % 2
        last_item = wi_ == len(work) - 1
        for ti in range(ntile):
            tI = tb0 + ti
            if last_item:
                dma(ph, hres[tI % 2][:], k.hsrc(layer, tI, mixed=True), [], ["hres%d" % (tI % 2)])
            for half in range(2):
                b = 4 + ((ti * 2 + half) % 4)
                for c in range(g):
                    mm(ph, ps[b][:, :], hT[hs][:, c, ti * 128:(ti + 1) * 128], wd[s][:, c, half * 512:(half + 1) * 512],
                       c == 0, c == g - 1, ["hT%d" % hs, "wd%d" % s], ["ps%d" % b])
                a_ = k.acc[:, tI, half * 512:(half + 1) * 512]
                if moe:
                    stt(ph, a_, ps[b][:, :], k.gates[:, tI, e:e + 1], a_, ALU.mult, ALU.add, ["ps%d" % b, "acc%d" % tI, "acc"],
                        ["acc%d" % tI])
                else:
                    tt(ph, a_, ps[b][:, :], a_, ALU.add, ["ps%d" % b, "acc%d" % tI, "acc"], ["acc%d" % tI])
            if last_item:
                lc_ = 1 if tI < 2 else 0
                hk_ = "hres%d" % (tI % 2)
                tt(ph, k.acc[:, tI, :], k.acc[:, tI, :], k.MG[:, lc_, :], ALU.mult, ["acc%d" % tI, "acc"], ["acc%d" % tI], eng=POOL)
                tt(ph, hres[tI % 2][:], hres[tI % 2][:], k.acc[:, tI, :], ALU.add, [hk_, "acc%d" % tI], [hk_])
                dma(ph, k.hbuf[tI * 128:(tI + 1) * 128, :], hres[tI % 2][:], [hk_], [])

    if pre is None:
        load(0)
    for n in range(len(seq)):
        wi_, tb = seq[n]
        gu(n)
        if n >= 1:
            down(n - 1)
        if tb == 0 and wi_ + 1 < len(work):
            load(wi_ + 1)
    down(len(seq) - 1)
    ph.emit()


def phase_ffn_resid(k, layer, t_first):
    ph = Phase(k.ctx)
    ht = [ph.sb("ht0", [128, D], F32), ph.sb("ht1", [128, D], F32)]
    for t in range(t_first, NT):
        lc = 1 if t < 2 else 0
        s = t % 2
        dma(ph, ht[s][:], k.hsrc(layer, t, mixed=True), [], ["ht%d" % s])
        tt(ph, k.acc[:, t, :], k.acc[:, t, :], k.MG[:, lc, :], ALU.mult, [], ["acc%d" % t], eng=POOL)
        tt(ph, ht[s][:], ht[s][:], k.acc[:, t, :], ALU.add, ["ht%d" % s, "acc%d" % t], ["ht%d" % s])
        dma(ph, k.hbuf[t * 128:(t + 1) * 128, :], ht[s][:], ["ht%d" % s], [])
    ph.emit()


def phase_final(k):
    ph = Phase(k.ctx)
    ht = [ph.sb("ht0", [128, D], F32), ph.sb("ht1", [128, D], F32)]
    sq = ph.sb("sq", [128, D], BF16)
    st = ph.sb("st", [128, 4], F32)
    fg = ph.sb("fg", [128, D], F32)
    dma(ph, fg[:], k.d["final_g"].partition_broadcast(128), [], ["fg"])
    for t in range(2, NT):
        s = t % 2
        dma(ph, ht[s][:], k.hbuf[t * 128:(t + 1) * 128, :], [], ["ht%d" % s])
        act(ph, sq[:], ht[s][:], AF.Square, ["ht%d" % s], ["sq", "st"], accum_out=st[:, 0:1])
        act(ph, st[:, 1:2], st[:, 0:1], AF.Sqrt, ["st"], ["st"], scale=1.0 / D, bias=k.eps_t[:, 0:1])
        ph.add(DVE, lambda e: e.reciprocal(out=st[:, 2:3], in_=st[:, 1:2]), ["st"], ["st"])
        stt(ph, ht[s][:], ht[s][:], st[:, 2:3], fg[:], ALU.mult, ALU.mult, ["ht%d" % s, "st", "fg"], ["ht%d" % s])
        dma(ph, k.d["out"][(t - 2) * 128:(t - 1) * 128, :], ht[s][:], ["ht%d" % s], [])
    ph.emit()


def phase_dump(k):
    ph = Phase(k.ctx)
    ht = [ph.sb("ht0", [128, D], F32), ph.sb("ht1", [128, D], F32)]
    for t in range(NT):
        s = t % 2
        dma(ph, ht[s][:], k.hbuf[t * 128:(t + 1) * 128, :], [], ["ht%d" % s])
        dma(ph, k.d["dbg"][t * 128:(t + 1) * 128, :], ht[s][:], ["ht%d" % s], [])
    ph.emit()


def phase_dump_sb(k, aps):
    ph = Phase(k.ctx)
    st = ph.sb("dst", [128, D], F32)
    for n, ap in enumerate(aps):
        w = ap.shape[-1]
        cp(ph, st[0:ap.shape[0], 0:w], ap, [], ["dst"])
        dma(ph, k.d["dbg"][n * 128:n * 128 + ap.shape[0], 0:w], st[0:ap.shape[0], 0:w], ["dst"], [])
    ph.emit()


INPUT_SHAPES = {
    "mod_w": [4, 1024, 6144], "mod_b": [4, 6144], "norm_mix_g": [4, 1024], "norm_ffn_g": [4, 1024], "final_g": [1024],
    "ev_w_in": [2, 1024, 2592], "a_ln_g": [2, 512], "a_ln_b": [2, 512], "a_ws": [2, 4, 128, 128], "a_bs": [2, 4, 128],
    "b_gate_w": [2, 2, 16, 256], "b_gate_b": [2, 2, 256], "b_norm_g": [2, 128], "ev_w_out": [2, 1024, 1024],
    "od_w_in": [2, 1024, 544], "c_q_norm_g": [2, 256], "c_w_uq": [2, 256, 1536], "c_kv_norm_g": [2, 256],
    "c_w_ukv": [2, 256, 2048], "od_w_out": [2, 1024, 1024],
    "ff_w_gate": [2, 1024, 2816], "ff_w_up": [2, 1024, 2816], "ff_w_down": [2, 2816, 1024],
    "moe_router": [2, 1024, 8], "moe_w_gate": [2, 8, 1024, 3584], "moe_w_up": [2, 8, 1024, 3584], "moe_w_down": [2, 8, 3584, 1024],
}
NCST = 2048


def make_consts():
    cst = np.zeros((128, NCST), np.float32)
    j = np.arange(128)[:, None]
    i = np.arange(128)[None, :]
    cst[:, 0:128] = np.eye(128)
    cst[:, 128:256] = (j <= i) * (-1.0 / 16)
    cst[:, 256:384] = (j >= i) * (-1.0 / 16)
    cst[:, 384:512] = (j > i) * (-1.0 / 16)
    cst[:, 512:640] = (j < i) * (-1.0 / 16)
    cst[:, 896:1024] = 1.0
    cst[:, 1024:1536] = np.tile((j <= i).astype(np.float32), (1, 4))
    cst[:, 1536:2048] = np.tile((j >= i).astype(np.float32), (1, 4))
    half = 16
    inv_freq = (10000.0 ** (-np.arange(0, half, 2, dtype=np.float32) / half)).astype(np.float32)
    tpos = np.arange(2048)
    rows = (tpos // 64).astype(np.float32)
    cols = (tpos % 64).astype(np.float32)
    C = np.zeros((32, 2048), np.float32)
    S = np.zeros((32, 2048), np.float32)
    for dd in range(32):
        g, jj = dd // 16, dd % 16
        f = jj % 8
        pos = rows if g == 0 else cols
        ang = (pos * inv_freq[f]).astype(np.float32)
        C[dd] = np.cos(ang)
        S[dd] = (-np.sin(ang)) if jj < 8 else np.sin(ang)
    return cst, C, S


def build(stop=None, dbg=False):
    nc = bass.Bass("TRN2", target_bir_lowering=False)
    k = K()
    k.nc = nc
    shapes = dict(INPUT_SHAPES)
    shapes.update({"x": [2048, D], "ctx": [256, D], "c": [D], "c_ctx": [D], "cst": [128, NCST],
                   "ropeC": [32, 2048], "ropeS": [32, 2048]})

    class LazyD(dict):
        def __missing__(self, n):
            ap = nc.dram_tensor(n, shapes[n], F32, kind="ExternalInput").ap()
            self[n] = ap
            return ap
    k.d = LazyD()
    k.d["out"] = nc.dram_tensor("out", [2048, D], F32, kind="ExternalOutput").ap()
    if dbg:
        k.d["dbg"] = nc.dram_tensor("dbg", [T, D], F32, kind="ExternalOutput").ap()
    k.hbuf = nc.dram_tensor("hbuf", [T, D], F32, kind="Internal").ap()

    def hsrc(layer, t, mixed=False):
        if layer == 0 and not mixed:
            return k.d["ctx"][t * 128:(t + 1) * 128, :] if t < 2 else k.d["x"][(t - 2) * 128:(t - 1) * 128, :]
        return k.hbuf[t * 128:(t + 1) * 128, :]
    k.hsrc = hsrc

    es = ExitStack()
    k.ctx = Ctx(nc, es)
    k.ps = [es.enter_context(nc.psum_tensor("psb%d" % b, [128, 512], F32)) for b in range(8)]
    k.cst = es.enter_context(nc.sbuf_tensor("cst_sb", [128, NCST], F32))
    k.eps_t = es.enter_context(nc.sbuf_tensor("eps_t", [128, 1], F32))
    k.condrep = es.enter_context(nc.sbuf_tensor("condrep", [128, 2, 8, 128], BF16))
    k.MG = es.enter_context(nc.sbuf_tensor("MG", [128, 2, D], F32))
    k.ident = k.cst[:, 0:128]
    k.TRI = [k.cst[:, 128:256], k.cst[:, 256:384]]
    k.STRI = [k.cst[:, 384:512], k.cst[:, 512:640]]
    k.ones = k.cst[:, 896:1024]
    k.one_t = k.cst[:, 896:897]
    k.MASK4 = [k.cst[:, 1024:1536], k.cst[:, 1536:2048]]

    def done(tag):
        return stop is not None and stop == tag

    phase_setup(k)
    if done("setup"):
        phase_dump_sb(k, [k.condrep[:, 0, 0, :], k.condrep[:, 1, 7, :], k.cst[:, 0:1024]])
        es.close()
        nc._used_inputs = [n for n in k.d if n not in ("out", "dbg")]
        return nc
    finished = False
    for layer in range(4):
        i = layer // 2
        need_ctx = layer < 3
        with ExitStack() as es2:
            if layer % 2 == 0:
                k.OB = es2.enter_context(nc.sbuf_tensor("OB_%d" % layer, [128, NT, 512], BF16))
                k.win = es2.enter_context(nc.sbuf_tensor("win_%d" % layer, [128, 8, 2592], BF16))
            else:
                k.KN = es2.enter_context(nc.sbuf_tensor("KN_%d" % layer, [128, 8, T], BF16))
                k.KR = es2.enter_context(nc.sbuf_tensor("KR_%d" % layer, [32, T], BF16))
                k.VA = es2.enter_context(nc.sbuf_tensor("VA_%d" % layer, [128, NT, 16, 64], BF16))
                k.CQT = es2.enter_context(nc.sbuf_tensor("CQT_%d" % layer, [128, 2, T], BF16))
            with nc.sbuf_tensor("MODa_%d" % layer, [128, 2, 3, D], F32) as MOD:
                k.MOD = MOD
                phase_mod(k, layer, 0)
                if done("mod%d" % layer):
                    phase_dump_sb(k, [k.MOD[:, a, b, :] for a in range(2) for b in range(3)])
                    nc._used_inputs = [n for n in k.d if n not in ("out", "dbg")]
                    return nc
                if layer % 2 == 0:
                    phase_even(k, layer, i, 1)
                    if done("evenB%d" % layer):
                        phase_dump_sb(k, [k.OB[:, t, :] for t in range(NT)])
                        nc._used_inputs = [n for n in k.d if n not in ("out", "dbg")]
                        return nc
                    phase_even(k, layer, i, 0)
                else:
                    phase_mla1(k, layer, i)
                    if done("mlaA%d" % layer):
                        phase_dump_sb(k, [k.CQT[:, 0, 0:1024], k.KN[:, 0, 0:1024], k.KR[:, 0:1024]])
                        nc._used_inputs = [n for n in k.d if n not in ("out", "dbg")]
                        return nc
            if layer % 2 == 1:
                phase_mla2(k, layer, i, need_ctx)
        if done("mix%d" % layer):
            break
        t_first = 0 if need_ctx else 2
        moe = layer % 2 == 1
        with ExitStack() as es3:
            k.mT = es3.enter_context(nc.sbuf_tensor("mT_%d" % layer, [128, 8, T], BF16))
            k.acc = es3.enter_context(nc.sbuf_tensor("acc_%d" % layer, [128, NT, D], F32))
            k.gates = es3.enter_context(nc.sbuf_tensor("gates_%d" % layer, [128, NT, 8], F32))
            pre = None
            if not moe:
                pre = [es3.enter_context(nc.sbuf_tensor("pre_wg_%d" % layer, [128, 8, 512], BF16)),
                       es3.enter_context(nc.sbuf_tensor("pre_wu_%d" % layer, [128, 8, 512], BF16)),
                       es3.enter_context(nc.sbuf_tensor("pre_wd_%d" % layer, [128, 4, D], BF16))]
            with nc.sbuf_tensor("MODb_%d" % layer, [128, 2, 3, D], F32) as MOD:
                k.MOD = MOD
                phase_mod(k, layer, 1)
                phase_ffn_norm(k, layer, moe, t_first, pre)
            phase_ffn_experts(k, layer, moe, t_first, pre)
        if done("ffn%d" % layer):
            break
    else:
        finished = True
    if finished or not dbg:
        phase_final(k)
    if dbg:
        phase_dump(k)
    es.close()
    nc._used_inputs = [n for n in k.d if n not in ("out", "dbg")]
    return nc


_CACHE = {}


def kernel(**inputs):
    cst, C, S = make_consts()
    if "nc" not in _CACHE:
        _CACHE["nc"] = build()
    nc = _CACHE["nc"]
    shared = {n: np.ascontiguousarray(inputs[n], dtype=np.float32) for n in INPUT_SHAPES}
    shared["c_ctx"] = np.ascontiguousarray(inputs["c_ctx"], dtype=np.float32)
    shared["cst"] = cst
    shared["ropeC"] = C
    shared["ropeS"] = S
    used = set(nc._used_inputs)
    shared = {n: v for n, v in shared.items() if n in used}
    in_maps = []
    for b in range(8):
        m = dict(shared)
        m["x"] = np.ascontiguousarray(inputs["x"][b], dtype=np.float32)
        m["ctx"] = np.ascontiguousarray(inputs["ctx"][b], dtype=np.float32)
        m["c"] = np.ascontiguousarray(inputs["c"][b], dtype=np.float32)
        in_maps.append(m)
    res = run_bass_kernel_spmd(nc, in_maps, core_ids=list(range(8)))
    return np.stack([np.asarray(r["out"], dtype=np.float32) for r in res.results], axis=0)
```

```python
import numpy as np
from contextlib import ExitStack
import concourse.bass as bass
import concourse.mybir as mybir
from concourse.alu_op_type import AluOpType as ALU
from concourse.bass_utils import run_bass_kernel_spmd

AF = mybir.ActivationFunctionType
F32 = mybir.dt.float32
BF16 = mybir.dt.bfloat16
PE, ACT, DVE, POOL, SP = "tensor", "scalar", "vector", "gpsimd", "sync"
ENGS = [PE, ACT, DVE, POOL, SP]
NDS = 8

D = 1024
NT = 18
T = 2304
EPS = 1e-6
FF_HIDDEN = 2816
MOE_HIDDEN = 3584
C_SCALE = 96 ** -0.5


class Op:
    __slots__ = ("eng", "fn", "deps", "need", "dma", "sig", "waits")

    def __init__(self, eng, fn, dma):
        self.eng, self.fn, self.dma = eng, fn, dma
        self.deps, self.need, self.sig, self.waits = [], False, None, []


class Ctx:
    def __init__(self, nc, es):
        self.nc = nc
        self.esem = {e: es.enter_context(nc.semaphore("s_" + e)) for e in ENGS}
        self.dsem = {e: [es.enter_context(nc.semaphore("d_%s_%d" % (e, i))) for i in range(NDS)] for e in (SP, POOL)}
        self.ecnt = {e: 0 for e in ENGS}
        self.dcnt = {e: 0 for e in self.dsem}
        self.waited = {e: {} for e in ENGS}
        self.nphase = 0


class Phase:
    def __init__(self, ctx):
        self.ctx = ctx
        self.nc = ctx.nc
        self.ops = []
        self.last_w = {}
        self.readers = {}
        self.es = ExitStack()
        self.limit = None

    def sb(self, name, shape, dt):
        return self.es.enter_context(self.nc.sbuf_tensor("%s_p%d" % (name, self.ctx.nphase), list(shape), dt))

    def add(self, eng, fn, R=(), W=(), dma=False):
        if self.limit is not None and len(self.ops) >= self.limit:
            return None
        op = Op(eng, fn, dma)
        deps = {}
        for k in R:
            w = self.last_w.get(k)
            if w is not None:
                deps[id(w)] = (w, True)
            if k.startswith("ps"):
                for r in self.readers.get(k, ()):
                    if r.eng != eng and id(r) not in deps:
                        deps[id(r)] = (r, False)
        for k in W:
            w = self.last_w.get(k)
            if w is not None and id(w) not in deps:
                deps[id(w)] = (w, False)
            for r in self.readers.get(k, ()):
                if id(r) not in deps:
                    deps[id(r)] = (r, False)
        for d, raw in deps.values():
            if (not d.dma) and (not dma) and d.eng == eng:
                if eng == PE:
                    continue
            op.deps.append(d)
            d.need = True
        for k in W:
            self.last_w[k] = op
            self.readers[k] = []
        for k in R:
            if k not in W:
                self.readers.setdefault(k, []).append(op)
        self.ops.append(op)
        return op

    def emit(self):
        c = self.ctx
        nc = self.nc
        per_eng = {e: [] for e in ENGS}
        for op in self.ops:
            pre = None
            if op.dma:
                k = c.dcnt[op.eng]
                c.dcnt[op.eng] += 1
                s = c.dsem[op.eng][k % NDS]
                op.sig = (s, 16 * (k // NDS + 1))
                if k >= NDS:
                    pre = (s, 16 * (k // NDS))
            elif op.need:
                c.ecnt[op.eng] += 1
                op.sig = (c.esem[op.eng], c.ecnt[op.eng])
            w = c.waited[op.eng]
            lst = [pre] if pre is not None else []
            lst += [d.sig for d in op.deps]
            for s, v in lst:
                if w.get(id(s), 0) >= v:
                    continue
                w[id(s)] = v
                op.waits.append((s, v))
            per_eng[op.eng].append(op)
        drains = {e: [] for e in ENGS}
        for e in c.dsem:
            for i, s in enumerate(c.dsem[e]):
                n = (c.dcnt[e] - i + NDS - 1) // NDS if c.dcnt[e] > i else 0
                if n > 0 and c.waited[e].get(id(s), 0) < 16 * n:
                    c.waited[e][id(s)] = 16 * n
                    drains[e].append((s, 16 * n))

        def run(en):
            def body(eng):
                for op in per_eng[en]:
                    for s, v in op.waits:
                        eng.wait_ge(s, v)
                    ins = op.fn(eng)
                    if op.sig is not None:
                        ins.then_inc(op.sig[0], 16 if op.dma else 1)
                for s, v in drains[en]:
                    eng.wait_ge(s, v)
            return body

        with nc.Block() as block:
            block.tensor(run(PE))
            block.scalar(run(ACT))
            block.vector(run(DVE))
            block.gpsimd(run(POOL))
            block.sync(run(SP))
        self.es.close()
        c.nphase += 1


def mm(ph, out, lhsT, rhs, start, stop, R, W):
    return ph.add(PE, lambda e: e.matmul(out, lhsT=lhsT, rhs=rhs, start=start, stop=stop), R, W)


def tr(ph, out, in_, ident, R, W):
    return ph.add(PE, lambda e: e.transpose(out=out, in_=in_, identity=ident), R, W)


def act(ph, out, in_, func, R, W, **kw):
    return ph.add(ACT, lambda e: e.activation(out=out, in_=in_, func=func, **kw), R, W)


def tt(ph, out, a, b, op, R, W, eng=DVE):
    return ph.add(eng, lambda e: e.tensor_tensor(out=out, in0=a, in1=b, op=op), R, W)


def ts(ph, out, a, s1, s2, op0, op1, R, W, eng=DVE):
    if s2 is None:
        return ph.add(eng, lambda e: e.tensor_scalar(out=out, in0=a, scalar1=s1, scalar2=None, op0=op0), R, W)
    return ph.add(eng, lambda e: e.tensor_scalar(out=out, in0=a, scalar1=s1, scalar2=s2, op0=op0, op1=op1), R, W)


def stt(ph, out, a, s, b, op0, op1, R, W, accum=None):
    if accum is None:
        return ph.add(DVE, lambda e: e.scalar_tensor_tensor(out=out, in0=a, scalar=s, in1=b, op0=op0, op1=op1), R, W)
    return ph.add(DVE, lambda e: e.scalar_tensor_tensor(out=out, in0=a, scalar=s, in1=b, op0=op0, op1=op1, accum_out=accum), R, W)


def cp(ph, out, in_, R, W, eng=DVE):
    return ph.add(eng, lambda e: e.tensor_copy(out=out, in_=in_), R, W)


def ms(ph, ap, val, W, eng=DVE):
    return ph.add(eng, lambda e: e.memset(ap, val), (), W)


def dma(ph, out, in_, R, W, eng=SP):
    return ph.add(eng, lambda e: e.dma_start(out=out, in_=in_), R, W, dma=True)


def v4(ap):
    return ap.rearrange("p (a b) -> p a b", b=128)


class K:
    pass


def gelu_tanh(ph, x, out, tmp, tk, R, W):
    tt(ph, tmp, x, x, ALU.mult, R, [tk], eng=POOL)
    ts(ph, tmp, tmp, 0.044715 * 1.5957691216057308, 1.5957691216057308, ALU.mult, ALU.add, [tk], [tk])
    tt(ph, tmp, tmp, x, ALU.mult, [tk] + list(R), [tk], eng=POOL)
    act(ph, tmp, tmp, AF.Sigmoid, [tk], [tk])
    tt(ph, out, tmp, x, ALU.mult, [tk] + list(R), W)


def norm_tile(k, ph, nb, t, layer, Amod, SHmod, nT_out, nT_key, extra_f32=None, mixed=False, prefetched=False):
    s = t % len(nb["ht"])
    ht = nb["ht"][s]
    hk = "ht%d" % s
    if not prefetched:
        dma(ph, ht[:], k.hsrc(layer, t, mixed), [], [hk])
    st = nb["stat"]
    sk = "stat"
    act(ph, nb["sq"][:], ht[:], AF.Square, [hk], ["sq", sk], accum_out=st[:, 0:1])
    act(ph, st[:, 1:2], st[:, 0:1], AF.Ln, [sk], [sk], scale=1.0 / D, bias=k.eps_t[:, 0:1])
    act(ph, st[:, 2:3], st[:, 1:2], AF.Exp, [sk], [sk], scale=-0.5)
    stt(ph, nb["tmp"][:], ht[:], st[:, 2:3], Amod, ALU.mult, ALU.mult, [hk, sk, "MOD"], ["tmp"])
    tt(ph, nb["nrm"][:], nb["tmp"][:], SHmod, ALU.add, ["tmp", "MOD"], ["nrm"])
    for j in range(8):
        b = j // 4
        tr(ph, k.ps[b][:, (j % 4) * 128:(j % 4 + 1) * 128], nb["nrm"][:, j * 128:(j + 1) * 128], k.ident, ["nrm"], ["ps%d" % b])
    act(ph, nT_out[:, 0:4, :], v4(k.ps[0][:, :]), AF.Copy, ["ps0"], [nT_key])
    cp(ph, nT_out[:, 4:8, :], v4(k.ps[1][:, :]), ["ps1"], [nT_key])
    if extra_f32 is not None:
        cp(ph, extra_f32[0][:, 0:4, :], v4(k.ps[0][:, :]), ["ps0"], [extra_f32[1]])
        act(ph, extra_f32[0][:, 4:8, :], v4(k.ps[1][:, :]), AF.Copy, ["ps1"], [extra_f32[1]])
    return hk


def norm_bufs(ph):
    return {
        "ht": [ph.sb("ht0", [128, D], F32), ph.sb("ht1", [128, D], F32)],
        "sq": ph.sb("sq", [128, D], BF16),
        "stat": ph.sb("stat", [128, 4], F32),
        "tmp": ph.sb("tmp", [128, D], F32),
        "nrm": ph.sb("nrm", [128, D], F32),
    }


def phase_setup(k):
    ph = Phase(k.ctx)
    dma(ph, k.cst[:], k.d["cst"][:, :], [], ["cst"])
    ms(ph, k.eps_t[:], EPS, ["eps"])
    craw = ph.sb("craw", [8, 2, 128], F32)
    dma(ph, craw[:, 0, :], k.d["c"].rearrange("(k p) -> k p", p=128), [], ["craw"])
    dma(ph, craw[:, 1, :], k.d["c_ctx"].rearrange("(k p) -> k p", p=128), [], ["craw"])
    cT = ph.sb("cT", [128, 2, 8], F32)
    for lc in range(2):
        tr(ph, k.ps[0][:, lc * 8:(lc + 1) * 8], craw[:, lc, :], k.cst[0:8, 0:8], ["craw", "cst"], ["ps0"])
    act(ph, cT[:].rearrange("p a b -> p (a b)"), k.ps[0][:, 0:16], AF.Silu, ["ps0"], ["cT"])
    for lc in range(2):
        for j in range(8):
            act(ph, k.condrep[:, lc, j, :], k.ones, AF.Copy, ["cT", "cst"], ["condrep"], scale=cT[:, lc, j:j + 1])
    ph.emit()


def phase_mod(k, layer, half):
    ph = Phase(k.ctx)
    wm = [ph.sb("wm0", [128, 8, 512], BF16), ph.sb("wm1", [128, 8, 512], BF16)]
    mb = [ph.sb("mb0", [128, 512], F32), ph.sb("mb1", [128, 512], F32)]
    gb = ph.sb("gb", [128, D], F32)
    gsrc = k.d["norm_mix_g"] if half == 0 else k.d["norm_ffn_g"]
    wsrc = k.d["mod_w"][layer].rearrange("(k p) n -> p k n", p=128)
    for blk in range(6):
        c0 = half * 3072 + blk * 512
        s = blk % 2
        dma(ph, wm[s][:], wsrc[:, :, c0:c0 + 512], [], ["wm%d" % s], eng=POOL)
        if blk == 0:
            dma(ph, gb[:], gsrc[layer].partition_broadcast(128), [], ["gb"])
        dma(ph, mb[s][:], k.d["mod_b"][layer, c0:c0 + 512].partition_broadcast(128), [], ["mb%d" % s])
        for lc in range(2):
            b = (blk * 2 + lc) % 4
            for j in range(8):
                mm(ph, k.ps[b][:, :], k.condrep[:, lc, j, :], wm[s][:, j, :], j == 0, j == 7, ["wm%d" % s], ["ps%d" % b])
            tt(ph, k.MOD[:, lc, blk // 2, (blk % 2) * 512:(blk % 2 + 1) * 512], k.ps[b][:, :], mb[s][:], ALU.add,
               ["ps%d" % b, "mb%d" % s], ["MOD"])
    for lc in range(2):
        stt(ph, k.MOD[:, lc, 1, :], k.MOD[:, lc, 1, :], 1.0, gb[:], ALU.add, ALU.mult, ["MOD", "gb"], ["MOD"])
    ph.emit()


def phase_even(k, layer, i, direction):
    full = direction == 0
    d = direction
    ph = Phase(k.ctx)
    import os
    if os.environ.get("KCUT"):
        ph.limit = int(os.environ["KCUT"])
    ps = k.ps
    nb = norm_bufs(ph)
    nb["ht"].append(ph.sb("ht2", [128, D], F32))
    win = k.win
    if not full:
        wsrc = k.d["ev_w_in"][i].rearrange("(k p) n -> p k n", p=128)
        dma(ph, win[:, :, 1024:2048], wsrc[:, :, 1024:2048], [], ["win"], eng=POOL)
        dma(ph, win[:, :, 2560:2592], wsrc[:, :, 2560:2592], [], ["win"], eng=POOL)
        dma(ph, win[:, :, 0:1024], wsrc[:, :, 0:1024], [], ["win2"], eng=POOL)
        dma(ph, win[:, :, 2048:2560], wsrc[:, :, 2048:2560], [], ["win2"], eng=POOL)
    gateW = ph.sb("gateW", [33, 512], F32)
    ms(ph, gateW[:], 0.0, ["gateW"])
    for dd in range(2):
        dma(ph, gateW[dd * 16:(dd + 1) * 16, dd * 256:(dd + 1) * 256], k.d["b_gate_w"][i, dd], [], ["gateW"])
    dma(ph, gateW[32:33, :], k.d["b_gate_b"][i].rearrange("a b -> (a b)").rearrange("(o n) -> o n", o=1), [], ["gateW"])
    gfa = ph.sb("gfa", [33, 128], F32)
    ms(ph, gfa[32:33, :], 1.0, ["gfa1"])
    S32 = ph.sb("S32", [128, 2, 256], F32)
    Sbf = ph.sb("Sbf", [128, 2, 256], BF16)
    ms(ph, S32[:], 0.0, ["S32"])
    ms(ph, Sbf[:], 0.0, ["Sbf"])
    nT = [ph.sb("nT0", [128, 8, 128], BF16), ph.sb("nT1", [128, 8, 128], BF16)]
    spt = ph.sb("spt", [128, 256], F32)
    Et = ph.sb("Et", [128, 256], F32)
    Eit = ph.sb("Eit", [128, 256], F32)
    ERt = ph.sb("ERt", [128, 256], F32)
    qdTm = ph.sb("qdTm", [128, 2, 2, 128], BF16)
    ms(ph, qdTm[:], 0.0, ["qdT"])
    kiT = ph.sb("kiT", [128, 256], BF16)
    krt = ph.sb("krt", [128, 256], BF16)
    vb = ph.sb("vb", [128, 512], BF16)
    sTt = ph.sb("sTt", [128, 512], BF16)
    if full:
        wout = ph.sb("wout", [128, 8, D], BF16)
        dma(ph, wout[:], k.d["ev_w_out"][i].rearrange("(k p) n -> p k n", p=128), [], ["wout"], eng=POOL)
        wsr = ph.sb("wsr", [128, 4, 128], F32)
        dma(ph, wsr[:], k.d["a_ws"][i].rearrange("h p q -> p h q"), [], ["wsr"])
        wsT = ph.sb("wsT", [128, 4, 128], BF16)
        for h in range(4):
            tr(ph, ps[2][:, h * 128:(h + 1) * 128], wsr[:, h, :], k.ident, ["wsr"], ["ps2"])
        cp(ph, wsT[:].rearrange("p a b -> p (a b)"), ps[2][:, :], ["ps2"], ["wsT"])
        bsB = ph.sb("bsB", [128, 512], F32)
        dma(ph, bsB[:], k.d["a_bs"][i].rearrange("a b -> (a b)").partition_broadcast(128), [], ["bsB"])
        lng = ph.sb("lng", [128, 512], F32)
        lnb = ph.sb("lnb", [128, 512], F32)
        dma(ph, lng[:], k.d["a_ln_g"][i].partition_broadcast(128), [], ["lng"])
        dma(ph, lnb[:], k.d["a_ln_b"][i].partition_broadcast(128), [], ["lnb"])
        bng = ph.sb("bng", [128, 4, 128], F32)
        for h in range(4):
            dma(ph, bng[:, h, :], k.d["b_norm_g"][i].partition_broadcast(128), [], ["bng"])
        xa = ph.sb("xa", [128, 512], F32)
        xt1 = ph.sb("xt1", [128, 512], F32)
        gv = ph.sb("gv", [128, 512], F32)
        bst = ph.sb("bst", [128, 8], F32)
        ost = ph.sb("ost", [128, 12], F32)
        ybt = ph.sb("ybt", [128, 512], F32)
        yT = ph.sb("yT", [128, 8, 128], BF16)
        ho = ph.sb("ho", [128, D], F32)
        osq = ph.sb("osq", [128, 128], BF16)
    TRI = k.TRI[d]
    STRI = k.STRI[d]
    MASK = k.MASK4[d]
    dcol = 127 if d == 0 else 0
    order = list(range(NT)) if d == 0 else [1, 0] + list(range(17, 1, -1))
    if full:
        xa2 = ph.sb("xa2", [128, 512], F32)
        sgr = ph.sb("sgr", [128, 512], F32)

    def rstd_ln_exp(dst, src, n, scale, R, W):
        act(ph, dst, src, AF.Ln, R, W, scale=scale, bias=k.eps_t[:, 0:1])
        act(ph, dst, dst, AF.Exp, W, W, scale=-0.5)

    if full:
        osum2 = [ph.sb("osum_a", [128, 512], F32), ph.sb("osum_b", [128, 512], F32)]
        srg2 = [ph.sb("srg_a", [128, 512], F32), ph.sb("srg_b", [128, 512], F32)]
        uT2 = [ph.sb("uT_a", [128, 512], F32), ph.sb("uT_b", [128, 512], F32)]
        vA2 = [ph.sb("vA_a", [128, 512], BF16), ph.sb("vA_b", [128, 512], BF16)]
        xt2 = ph.sb("xt2", [128, 512], F32)

    def tail(tp):
        o_ = tp % 2
        lcp = 1 if tp < 2 else 0
        for h in range(4):
            mm(ph, ps[3][:, h * 128:(h + 1) * 128], vA2[o_][:, h * 128:(h + 1) * 128], wsT[:, h, :], True, True,
               ["vA%d" % o_, "wsT"], ["ps3"])
        tt(ph, xt2[:], ps[3][:, :], bsB[:], ALU.add, ["ps3", "bsB"], ["xt2"])
        tt(ph, yT[:, 0:4, :].rearrange("p a b -> p (a b)"), xt2[:], uT2[o_][:], ALU.mult, ["xt2", "uT%d" % o_], ["yT"])
        for h in range(4):
            act(ph, osq[:], osum2[o_][:, h * 128:(h + 1) * 128], AF.Square, ["osum%d" % o_], ["osq", "ost"], accum_out=ost[:, h:h + 1])
        rstd_ln_exp(ost[:, 8:12], ost[:, 0:4], 4, 1.0 / 128, ["ost"], ["ost"])
        for h in range(4):
            stt(ph, ybt[:, h * 128:(h + 1) * 128], osum2[o_][:, h * 128:(h + 1) * 128], ost[:, 8 + h:9 + h],
                srg2[o_][:, h * 128:(h + 1) * 128], ALU.mult, ALU.mult, ["osum%d" % o_, "ost", "srg%d" % o_], ["ybt"])
        for c in range(4):
            tr(ph, ps[5][:, c * 128:(c + 1) * 128], ybt[:, c * 128:(c + 1) * 128], k.ident, ["ybt"], ["ps5"])
        act(ph, yT[:, 4:8, :], v4(ps[5][:, :]), AF.Copy, ["ps5"], ["yT"])
        for half in range(2):
            b = 6 if half == 0 else 0
            for j in range(8):
                mm(ph, ps[b][:, :], yT[:, j, :], wout[:, j, half * 512:(half + 1) * 512], j == 0, j == 7, ["yT", "wout"], ["ps%d" % b])
            tt(ph, ho[:, half * 512:(half + 1) * 512], ps[b][:, :], k.MOD[:, lcp, 2, half * 512:(half + 1) * 512], ALU.mult,
               ["ps%d" % b, "MOD"], ["ho"])
        tt(ph, ho[:], ho[:], nb["ht"][tp % 3][:], ALU.add, ["ho", "ht%d" % (tp % 3)], ["ho"])
        dma(ph, k.hbuf[tp * 128:(tp + 1) * 128, :], ho[:], ["ho"], [])

    def do_norm(tt_):
        lc_ = 1 if tt_ < 2 else 0
        return norm_tile(k, ph, nb, tt_, layer, k.MOD[:, lc_, 1, :], k.MOD[:, lc_, 0, :], nT[tt_ % 2], "nT%d" % (tt_ % 2),
                         prefetched=True)

    dma(ph, nb["ht"][order[0] % 3][:], k.hsrc(layer, order[0]), [], ["ht%d" % (order[0] % 3)])
    do_norm(order[0])
    for oi, t in enumerate(order):
        lc = 1 if t < 2 else 0
        nt_ = nT[t % 2]
        nk = "nT%d" % (t % 2)
        hk = "ht%d" % (t % 3)
        tn = order[oi + 1] if oi + 1 < len(order) else None
        if tn is not None:
            dma(ph, nb["ht"][tn % 3][:], k.hsrc(layer, tn), [], ["ht%d" % (tn % 3)])
        for c in range(4):
            for j in range(8):
                mm(ph, ps[2][:, c * 128:(c + 1) * 128], win[:, j, 1024 + c * 128:1024 + (c + 1) * 128], nt_[:, j, :],
                   j == 0, j == 7, ["win", nk], ["ps2"])
        for j in range(8):
            mm(ph, ps[3][0:32, 0:128], win[:, j, 2560:2592], nt_[:, j, :], j == 0, j == 7, ["win", nk], ["ps3"])
        for j in range(8):
            mm(ph, ps[4][:, 0:256], nt_[:, j, :], win[:, j, 1280:1536], j == 0, j == 7, ["win", nk], ["ps4"])
        for j in range(8):
            mm(ph, ps[5][:, :], nt_[:, j, :], win[:, j, 1536:2048], j == 0, j == 7, ["win", nk], ["ps5"])
        if full:
            for j in range(8):
                mm(ph, ps[6][:, :], nt_[:, j, :], win[:, j, 2048:2560], j == 0, j == 7, ["win", nk], ["ps6"])
            for j in range(8):
                mm(ph, ps[7][:, :], nt_[:, j, :], win[:, j, 512:1024], j == 0, j == 7, ["win", nk], ["ps7"])
            for c in range(4):
                for j in range(8):
                    mm(ph, ps[0][:, c * 128:(c + 1) * 128], win[:, j, c * 128:(c + 1) * 128], nt_[:, j, :],
                       j == 0, j == 7, ["win", nk], ["ps0"])
        act(ph, gfa[0:32, :], ps[3][0:32, 0:128], AF.Copy, ["ps3"], ["gfa0"])
        mm(ph, ps[1][:, 0:256], gfa[0:33, :], gateW[0:33, d * 256:(d + 1) * 256], True, True, ["gfa0", "gfa1", "gateW"], ["ps1"])
        act(ph, spt[:], ps[1][:, 0:256], AF.Exp, ["ps1"], ["spt"], scale=-1.0)
        act(ph, spt[:], spt[:], AF.Ln, ["spt"], ["spt"], bias=k.one_t[:, 0:1])
        for hp in range(2):
            mm(ph, ps[3][:, hp * 128:(hp + 1) * 128], spt[:, hp * 128:(hp + 1) * 128], TRI, True, True, ["spt", "cst"], ["ps3"])
        mm(ph, ps[1][:, 256:512], STRI, spt[:, :], True, True, ["spt", "cst"], ["ps1"])
        act(ph, Et[:], ps[3][:, 0:256], AF.Exp, ["ps3"], ["Et"])
        act(ph, Eit[:], ps[3][:, 0:256], AF.Exp, ["ps3"], ["Eit"], scale=-1.0)
        act(ph, ERt[:], ps[1][:, 256:512], AF.Exp, ["ps1"], ["ERt"])
        for hl in range(2):
            r0, r1 = hl * 64, (hl + 1) * 64
            stt(ph, qdTm[r0:r1, :, hl, :], ps[2][r0:r1, 0:256].rearrange("p (a b) -> p a b", b=128), 0.125,
                Et[r0:r1, :].rearrange("p (a b) -> p a b", b=128), ALU.mult, ALU.mult, ["ps2", "Et"], ["qdT"])
        tt(ph, kiT[:], ps[2][:, 256:512], Eit[:], ALU.mult, ["ps2", "Eit"], ["kiT"])
        tt(ph, krt[:], ps[4][:, 0:256], ERt[:], ALU.mult, ["ps4", "ERt"], ["krt"])
        act(ph, vb[:], ps[5][:, :], AF.Copy, ["ps5"], ["vb"])
        for h in range(4):
            hp, hl = h // 2, h % 2
            mm(ph, ps[4][:, h * 128:(h + 1) * 128], kiT[:, hp * 128:(hp + 1) * 128],
               qdTm[:, hp, hl, :], True, True, ["kiT", "qdT"], ["ps4"])
        tt(ph, sTt[:], ps[4][:, :], MASK, ALU.mult, ["ps4", "cst"], ["sTt"])
        for h in range(4):
            hp, hl = h // 2, h % 2
            mm(ph, ps[2][:, h * 128:(h + 1) * 128], sTt[:, h * 128:(h + 1) * 128], vb[:, h * 128:(h + 1) * 128],
               True, False, ["sTt", "vb"], ["ps2"])
            mm(ph, ps[2][:, h * 128:(h + 1) * 128], qdTm[:, hp, hl, :],
               Sbf[:, hp, hl * 128:(hl + 1) * 128], False, True, ["qdT", "Sbf"], ["ps2"])
        csb = 7 if not full else 1
        for hp in range(2):
            mm(ph, ps[csb][:, hp * 256:(hp + 1) * 256], krt[:, hp * 128:(hp + 1) * 128], vb[:, hp * 256:(hp + 1) * 256],
               True, True, ["krt", "vb"], ["ps%d" % csb])
        for hp in range(2):
            stt(ph, S32[:, hp, :], S32[:, hp, :], Et[:, hp * 128 + dcol:hp * 128 + dcol + 1], ps[csb][:, hp * 256:(hp + 1) * 256],
                ALU.mult, ALU.add, ["S32", "Et", "ps%d" % csb], ["S32"])
        act(ph, Sbf[:].rearrange("p a b -> p (a b)"), S32[:].rearrange("p a b -> p (a b)"), AF.Copy, ["S32"], ["Sbf"])
        if not full:
            cp(ph, k.OB[:, t, :], ps[2][:, :], ["ps2"], ["OB%d" % t])
            if tn is not None:
                do_norm(tn)
            continue
        ob_ = t % 2
        tt(ph, osum2[ob_][:], ps[2][:, :], k.OB[:, t, :], ALU.add, ["ps2"], ["osum%d" % ob_])
        act(ph, xa[:], ps[0][:, :], AF.Copy, ["ps0"], ["xa"])
        act(ph, xa2[:], ps[7][:, :], AF.Copy, ["ps7"], ["xa2"])
        act(ph, sgr[:], ps[6][:, :], AF.Sigmoid, ["ps6"], ["sgr"])
        stt(ph, srg2[ob_][:], ps[6][:, :], 1.0, bng[:].rearrange("p a b -> p (a b)"), ALU.mult, ALU.mult, ["ps6", "bng"],
            ["srg%d" % ob_])
        if oi >= 1:
            tail(order[oi - 1])
        if tn is not None:
            do_norm(tn)
        tt(ph, srg2[ob_][:], srg2[ob_][:], sgr[:], ALU.mult, ["srg%d" % ob_, "sgr"], ["srg%d" % ob_], eng=POOL)
        tt(ph, xt1[:], xa[:], xa[:], ALU.mult, ["xa"], ["xt1"])
        ts(ph, xt1[:], xt1[:], 0.044715 * 1.5957691216057308, 1.5957691216057308, ALU.mult, ALU.add, ["xt1"], ["xt1"])
        tt(ph, xt1[:], xt1[:], xa[:], ALU.mult, ["xt1", "xa"], ["xt1"])
        tt(ph, gv[:], xa2[:], xa2[:], ALU.mult, ["xa2"], ["gv"], eng=POOL)
        ts(ph, gv[:], gv[:], 0.044715 * 1.5957691216057308, 1.5957691216057308, ALU.mult, ALU.add, ["gv"], ["gv"])
        tt(ph, gv[:], gv[:], xa2[:], ALU.mult, ["gv", "xa2"], ["gv"], eng=POOL)
        act(ph, xt1[:], xt1[:], AF.Sigmoid, ["xt1"], ["xt1"])
        act(ph, gv[:], gv[:], AF.Sigmoid, ["gv"], ["gv"])
        tt(ph, uT2[ob_][:], xt1[:], xa[:], ALU.mult, ["xt1", "xa"], ["uT%d" % ob_])
        tt(ph, gv[:], gv[:], xa2[:], ALU.mult, ["gv", "xa2"], ["gv"])
        ph.add(DVE, lambda e: e.bn_stats(out=bst[:, 0:6], in_=gv[:]), ["gv"], ["bst"])
        ph.add(DVE, lambda e: e.bn_aggr(out=bst[:, 6:8], in_=bst[:, 0:6]), ["bst"], ["bst"])
        rstd_ln_exp(bst[:, 1:2], bst[:, 7:8], 1, 1.0, ["bst"], ["bst"])
        ts(ph, gv[:], gv[:], bst[:, 6:7], bst[:, 1:2], ALU.subtract, ALU.mult, ["gv", "bst"], ["gv"])
        tt(ph, gv[:], gv[:], lng[:], ALU.mult, ["gv", "lng"], ["gv"], eng=POOL)
        tt(ph, vA2[ob_][:], gv[:], lnb[:], ALU.add, ["gv", "lnb"], ["vA%d" % ob_])
    if full:
        tail(order[-1])
    ph.emit()


def phase_mla1(k, layer, i):
    import os
    ph = Phase(k.ctx)
    if os.environ.get("KCUT1"):
        ph.limit = int(os.environ["KCUT1"])
    ps = k.ps
    nb = norm_bufs(ph)
    wi = ph.sb("wi", [128, 8, 576], BF16)
    dma(ph, wi[:, :, 0:544], k.d["od_w_in"][i].rearrange("(k p) n -> p k n", p=128), [], ["wi"], eng=POOL)
    for g in range(2):
        cp(ph, wi[:, :, 544 + g * 16:544 + g * 16 + 8], wi[:, :, 512 + g * 16 + 8:512 + g * 16 + 16], ["wi"], ["wi"])
        cp(ph, wi[:, :, 544 + g * 16 + 8:544 + g * 16 + 16], wi[:, :, 512 + g * 16:512 + g * 16 + 8], ["wi"], ["wi"])
    wkv = ph.sb("wkv", [128, 2, 2048], BF16)
    wkvsrc = k.d["c_w_ukv"][i].rearrange("(k p) n -> p k n", p=128)
    for hh_ in range(2):
        dma(ph, wkv[:, :, hh_ * 1024:(hh_ + 1) * 1024], wkvsrc[:, :, hh_ * 1024:(hh_ + 1) * 1024], [], ["wkv"], eng=POOL)
    gq = ph.sb("gq", [128, 512], F32)
    dma(ph, gq[:, 0:256], k.d["c_q_norm_g"][i].partition_broadcast(128), [], ["gq"])
    dma(ph, gq[:, 256:512], k.d["c_kv_norm_g"][i].partition_broadcast(128), [], ["gq"])
    nT = [ph.sb("nT0", [128, 8, 128], BF16), ph.sb("nT1", [128, 8, 128], BF16)]
    cst2 = ph.sb("cst2", [128, 8], F32)
    cqn = ph.sb("cqn", [128, 512], F32)
    csq = ph.sb("csq", [128, 256], BF16)
    ckT = ph.sb("ckT", [128, 2, 128], BF16)
    ra = ph.sb("ra", [32, 128], F32)
    rb = ph.sb("rb", [32, 128], F32)
    rC = [ph.sb("rC0", [32, 128], F32), ph.sb("rC1", [32, 128], F32)]
    rS = [ph.sb("rS0", [32, 128], F32), ph.sb("rS1", [32, 128], F32)]
    def do_norm1(tt_):
        lc_ = 1 if tt_ < 2 else 0
        norm_tile(k, ph, nb, tt_, layer, k.MOD[:, lc_, 1, :], k.MOD[:, lc_, 0, :], nT[tt_ % 2], "nT%d" % (tt_ % 2))

    do_norm1(0)
    for t in range(NT):
        lc = 1 if t < 2 else 0
        nt_ = nT[t % 2]
        nk = "nT%d" % (t % 2)
        for j in range(8):
            mm(ph, ps[2][:, :], nt_[:, j, :], wi[:, j, 0:512], j == 0, j == 7, ["wi", nk], ["ps2"])
        for j in range(8):
            mm(ph, ps[3][0:32, 0:128], wi[:, j, 512:544], nt_[:, j, :], j == 0, j == 7, ["wi", nk], ["ps3"])
        for j in range(8):
            mm(ph, ps[3][0:32, 128:256], wi[:, j, 544:576], nt_[:, j, :], j == 0, j == 7, ["wi", nk], ["ps3"])
        for g in range(2):
            act(ph, csq[:], ps[2][:, g * 256:(g + 1) * 256], AF.Square, ["ps2"], ["csq", "cst2"], accum_out=cst2[:, g:g + 1])
        act(ph, cst2[:, 2:4], cst2[:, 0:2], AF.Sqrt, ["cst2"], ["cst2"], scale=1.0 / 256, bias=k.eps_t[:, 0:1])
        ph.add(DVE, lambda e: e.reciprocal(out=cst2[:, 4:6], in_=cst2[:, 2:4]), ["cst2"], ["cst2"])
        for g in range(2):
            stt(ph, cqn[:, g * 256:(g + 1) * 256], ps[2][:, g * 256:(g + 1) * 256], cst2[:, 4 + g:5 + g], gq[:, g * 256:(g + 1) * 256],
                ALU.mult, ALU.mult, ["ps2", "cst2", "gq"], ["cqn"])
        for c in range(4):
            tr(ph, ps[4][:, c * 128:(c + 1) * 128], cqn[:, c * 128:(c + 1) * 128], k.ident, ["cqn"], ["ps4"])
        if t + 1 < NT:
            do_norm1(t + 1)
        act(ph, k.CQT[:, :, t * 128:(t + 1) * 128], v4(ps[4][:, :])[:, 0:2, :], AF.Copy, ["ps4"], ["CQT"])
        cp(ph, ckT[:], v4(ps[4][:, :])[:, 2:4, :], ["ps4"], ["ckT"])
        if t >= 2:
            tl = (t - 2) * 128
            rs_ = t % 2
            dma(ph, rC[rs_][:], k.d["ropeC"][:, tl:tl + 128], [], ["rC%d" % rs_])
            dma(ph, rS[rs_][:], k.d["ropeS"][:, tl:tl + 128], [], ["rS%d" % rs_])
            tt(ph, ra[:], ps[3][0:32, 0:128], rC[rs_][:], ALU.mult, ["ps3", "rC%d" % rs_], ["ra"])
            tt(ph, rb[:], ps[3][0:32, 128:256], rS[rs_][:], ALU.mult, ["ps3", "rS%d" % rs_], ["rb"])
            tt(ph, k.KR[:, t * 128:(t + 1) * 128], ra[:], rb[:], ALU.add, ["ra", "rb"], ["KR"], eng=POOL)
        else:
            cp(ph, k.KR[:, t * 128:(t + 1) * 128], ps[3][0:32, 0:128], ["ps3"], ["KR"])
        for g in range(4):
            b = 5 + (g % 2)
            for hh in range(4):
                h = g * 4 + hh
                po = (h // 8) * 64
                for kc in range(2):
                    mm(ph, ps[b][po:po + 64, hh * 128:(hh + 1) * 128], wkv[:, kc, h * 128:h * 128 + 64], ckT[:, kc, :],
                       kc == 0, kc == 1, ["wkv", "ckT"], ["ps%d" % b])
            po = (g // 2) * 64
            s0 = (g % 2) * 4
            eng_cp = act if g % 2 == 0 else None
            if g % 2 == 0:
                act(ph, k.KN[po:po + 64, s0:s0 + 4, t * 128:(t + 1) * 128], v4(ps[b][po:po + 64, :]), AF.Copy, ["ps%d" % b], ["KN"])
            else:
                cp(ph, k.KN[po:po + 64, s0:s0 + 4, t * 128:(t + 1) * 128], v4(ps[b][po:po + 64, :]), ["ps%d" % b], ["KN"])
        for half in range(2):
            b = 7 if half == 0 else 2
            rhs_v = wkv[:, :, half * 1024:(half + 1) * 1024].rearrange("p k (h x) -> p k h x", x=128)
            for kc in range(2):
                mm(ph, ps[b][:, :].rearrange("p (h x) -> p h x", x=64), ckT[:, kc, :], rhs_v[:, kc, :, 64:128], kc == 0, kc == 1,
                   ["wkv", "ckT"], ["ps%d" % b])
            if half == 0:
                act(ph, k.VA[:, t, 0:8, :], ps[b][:, :].rearrange("p (h x) -> p h x", x=64), AF.Copy, ["ps%d" % b], ["VA"])
            else:
                cp(ph, k.VA[:, t, 8:16, :], ps[b][:, :].rearrange("p (h x) -> p h x", x=64), ["ps%d" % b], ["VA"])
    cp(ph, k.MG[:, 0, :], k.MOD[:, 0, 2, :], ["MOD"], ["MG"])
    cp(ph, k.MG[:, 1, :], k.MOD[:, 1, 2, :], ["MOD"], ["MG"])
    ph.emit()


def phase_mla2(k, layer, i, need_ctx):
    import os
    ph = Phase(k.ctx)
    if os.environ.get("KCUT2"):
        ph.limit = int(os.environ["KCUT2"])
    ps = k.ps
    wq = ph.sb("wq", [128, 2, 1536], BF16)
    wqsrc = k.d["c_w_uq"][i].rearrange("(k p) n -> p k n", p=128)
    for hh_ in range(2):
        dma(ph, wq[:, :, hh_ * 768:(hh_ + 1) * 768], wqsrc[:, :, hh_ * 768:(hh_ + 1) * 768], [], ["wq"], eng=POOL)
    wqs = ph.sb("wqs", [128, 2, 16, 32], BF16)
    wq4 = wq[:].rearrange("p k (h x) -> p k h x", x=96)
    for g in range(2):
        cp(ph, wqs[:, :, :, g * 16:g * 16 + 8], wq4[:, :, :, 64 + g * 16 + 8:64 + g * 16 + 16], ["wq"], ["wqs"])
        cp(ph, wqs[:, :, :, g * 16 + 8:g * 16 + 16], wq4[:, :, :, 64 + g * 16:64 + g * 16 + 8], ["wq"], ["wqs"])
    wo = ph.sb("wo", [64, 16, D], BF16)
    dma(ph, wo[:], k.d["od_w_out"][i].rearrange("(h d) n -> d h n", d=64), [], ["wo"], eng=POOL)
    OT = ph.sb("OT", [64, 16, 512], BF16)
    QN = [ph.sb("QN%d" % q_, [128, 512], BF16) for q_ in range(4)]
    for q_ in range(4):
        ms(ph, QN[q_][:], 0.0, ["QN%d" % q_])
    QR = [ph.sb("QR0", [32, 512], BF16), ph.sb("QR1", [32, 512], BF16)]
    qa = ph.sb("qa", [32, 512], F32)
    qb = ph.sb("qb", [32, 512], F32)
    pT = [ph.sb("pT%d" % q_, [128, 512], BF16) for q_ in range(3)]
    rinv = ph.sb("rinv", [64, 512], F32)
    ht = ph.sb("ht0", [128, D], F32)
    ho = ph.sb("ho0", [128, D], F32)
    rC = ph.sb("rC", [32, 512], F32)
    rS = ph.sb("rS", [32, 512], F32)
    blocks = ([(0, 2)] if need_ctx else []) + [(2 + 4 * b, 4) for b in range(4)]
    items = [(t0, ntile, h) for (t0, ntile) in blocks for h in range(16)]
    st = {"npt": 0}

    def stage_q(idx):
        t0, ntile, h = items[idx]
        nq, q0 = ntile * 128, t0 * 128
        po = (h // 8) * 64
        s = idx % 2
        qs = s + 2 * (h // 8)
        qn, qr = QN[qs], QR[s]
        if h == 0 and t0 >= 2:
            dma(ph, rC[:, 0:nq], k.d["ropeC"][:, q0 - 256:q0 - 256 + nq], [], ["rC"])
            dma(ph, rS[:, 0:nq], k.d["ropeS"][:, q0 - 256:q0 - 256 + nq], [], ["rS"])
        for kc in range(2):
            mm(ph, ps[0][po:po + 64, 0:nq], wq[:, kc, h * 96:h * 96 + 64], k.CQT[:, kc, q0:q0 + nq], kc == 0, kc == 1,
               ["wq", "CQT"], ["ps0"])
        for kc in range(2):
            mm(ph, ps[1][0:32, 0:nq], wq[:, kc, h * 96 + 64:h * 96 + 96], k.CQT[:, kc, q0:q0 + nq], kc == 0, kc == 1,
               ["wq", "CQT"], ["ps1"])
        act(ph, qn[po:po + 64, 0:nq], ps[0][po:po + 64, 0:nq], AF.Copy, ["ps0"], ["QN%d" % qs])
        if t0 >= 2:
            for kc in range(2):
                mm(ph, ps[2][0:32, 0:nq], wqs[:, kc, h, :], k.CQT[:, kc, q0:q0 + nq], kc == 0, kc == 1, ["wqs", "CQT"], ["ps2"])
            tt(ph, qa[:, 0:nq], ps[1][0:32, 0:nq], rC[:, 0:nq], ALU.mult, ["ps1", "rC"], ["qa"])
            tt(ph, qb[:, 0:nq], ps[2][0:32, 0:nq], rS[:, 0:nq], ALU.mult, ["ps2", "rS"], ["qb"])
            tt(ph, qr[:, 0:nq], qa[:, 0:nq], qb[:, 0:nq], ALU.add, ["qa", "qb"], ["QR%d" % s], eng=POOL)
        else:
            cp(ph, qr[:, 0:nq], ps[1][0:32, 0:nq], ["ps1"], ["QR%d" % s])

    def qk(idx, kt, slot):
        t0, ntile, h = items[idx]
        nq = ntile * 128
        s = idx % 2
        qs = s + 2 * (h // 8)
        b = 3 + slot
        mm(ph, ps[b][:, 0:nq], k.KN[:, h % 8, kt * 128:(kt + 1) * 128], QN[qs][:, 0:nq], True, False,
           ["KN", "QN%d" % qs], ["ps%d" % b])
        mm(ph, ps[b][:, 0:nq], k.KR[0:32, kt * 128:(kt + 1) * 128], QR[s][0:32, 0:nq], False, True,
           ["KR", "QR%d" % s], ["ps%d" % b])
        act(ph, pT[slot][:, 0:nq], ps[b][:, 0:nq], AF.Exp, ["ps%d" % b], ["pT%d" % slot], scale=C_SCALE)

    onesb = ph.sb("onesb", [128, 64], BF16)
    ms(ph, onesb[:], 1.0, ["onesb"])

    def pv(idx, kt, slot, nkt):
        t0, ntile, h = items[idx]
        nq = ntile * 128
        ob = 6 + idx % 2
        mm(ph, ps[ob][0:64, 0:nq], k.VA[:, kt, h, :], pT[slot][:, 0:nq], kt == 0, kt == nkt - 1,
           ["VA", "pT%d" % slot], ["ps%d" % ob])
        mm(ph, ps[ob][64:128, 0:nq], onesb[:, :], pT[slot][:, 0:nq], kt == 0, kt == nkt - 1,
           ["onesb", "pT%d" % slot], ["ps%d" % ob])

    def tail(idx):
        t0, ntile, h = items[idx]
        nq = ntile * 128
        ob = 6 + idx % 2
        ph.add(DVE, lambda e: e.reciprocal(out=rinv[0:64, 0:nq], in_=ps[ob][64:128, 0:nq]), ["ps%d" % ob], ["rinv"])
        tt(ph, OT[:, h, 0:nq], ps[ob][0:64, 0:nq], rinv[0:64, 0:nq], ALU.mult, ["ps%d" % ob, "rinv"], ["OT"])

    def outproj(t0, ntile):
        for ti in range(ntile):
            t = t0 + ti
            lc = 1 if t < 2 else 0
            dma(ph, ht[:], k.hsrc(layer, t), [], ["ht"])
            for half in range(2):
                b = half
                for h in range(16):
                    mm(ph, ps[b][:, :], OT[:, h, ti * 128:(ti + 1) * 128], wo[:, h, half * 512:(half + 1) * 512], h == 0, h == 15,
                       ["OT", "wo"], ["ps%d" % b])
                tt(ph, ho[:, half * 512:(half + 1) * 512], ps[b][:, :], k.MG[:, lc, half * 512:(half + 1) * 512], ALU.mult,
                   ["ps%d" % b, "MG"], ["ho"])
            tt(ph, ho[:], ho[:], ht[:], ALU.add, ["ho", "ht"], ["ho"], eng=POOL)
            dma(ph, k.hbuf[t * 128:(t + 1) * 128, :], ho[:], ["ho"], [])

    stage_q(0)
    pend_tail = None
    for idx in range(len(items)):
        t0, ntile, h = items[idx]
        nkt = 2 if t0 == 0 else NT
        base = st["npt"]
        DEPTH = 2
        for kt in range(min(DEPTH, nkt)):
            qk(idx, kt, (base + kt) % 3)
        if pend_tail is not None:
            tail(pend_tail)
            pt0, pnt, phh = items[pend_tail]
            if phh == 15:
                outproj(pt0, pnt)
            pend_tail = None
        if idx + 1 < len(items) and items[idx + 1][2] != 0:
            stage_q(idx + 1)
        for kt in range(nkt):
            if kt + DEPTH < nkt:
                qk(idx, kt + DEPTH, (base + kt + DEPTH) % 3)
            pv(idx, kt, (base + kt) % 3, nkt)
        st["npt"] = base + nkt
        if idx + 1 < len(items) and items[idx + 1][2] == 0:
            tail(idx)
            outproj(t0, ntile)
            stage_q(idx + 1)
        else:
            pend_tail = idx
    if pend_tail is not None:
        tail(pend_tail)
        pt0, pnt, phh = items[pend_tail]
        outproj(pt0, pnt)
    ph.emit()


def phase_ffn_norm(k, layer, moe, t_first, pre=None):
    ph = Phase(k.ctx)
    ps = k.ps
    nb = norm_bufs(ph)
    if pre is not None:
        i_ = layer // 2
        if moe:
            sg_, su_, sd_ = k.d["moe_w_gate"][i_, 0], k.d["moe_w_up"][i_, 0], k.d["moe_w_down"][i_, 0]
        else:
            sg_, su_, sd_ = k.d["ff_w_gate"][i_], k.d["ff_w_up"][i_], k.d["ff_w_down"][i_]
        dma(ph, pre[0][:, :, 0:512], sg_.rearrange("(k p) n -> p k n", p=128)[:, :, 0:512], [], ["pre0"], eng=POOL)
        dma(ph, pre[1][:, :, 0:512], su_.rearrange("(k p) n -> p k n", p=128)[:, :, 0:512], [], ["pre1"], eng=POOL)
        dma(ph, pre[2][:, 0:4, :], sd_.rearrange("(c p) n -> p c n", p=128)[:, 0:4, :], [], ["pre2"], eng=POOL)
    if moe:
        i = layer // 2
        rw = ph.sb("rw", [128, 8, 8], F32)
        dma(ph, rw[:], k.d["moe_router"][i].rearrange("(k p) n -> p k n", p=128), [], ["rw"])
        mT32 = [ph.sb("mT32a", [128, 8, 128], F32), ph.sb("mT32b", [128, 8, 128], F32)]
        lg = ph.sb("lg", [128, 8], F32)
        m8 = ph.sb("m8", [128, 8], F32)
        ex = ph.sb("ex", [128, 8], F32)
        ge = ph.sb("ge", [128, 8], F32)
        gs = ph.sb("gs", [128, 4], F32)

    def router(t):
        m32 = mT32[t % 2]
        mk = "mT32_%d" % (t % 2)
        for j in range(8):
            mm(ph, ps[2][:, 0:8], m32[:, j, :], rw[:, j, :], j == 0, j == 7, [mk, "rw"], ["ps2"])
        cp(ph, lg[:], ps[2][:, 0:8], ["ps2"], ["lg"])
        ph.add(DVE, lambda e: e.max(out=m8[:], in_=lg[:]), ["lg"], ["m8"])
        ts(ph, gs[:, 0:1], m8[:, 0:1], -1.0, None, ALU.mult, None, ["m8"], ["gs"])
        act(ph, ex[:], lg[:], AF.Exp, ["lg", "gs"], ["ex"], bias=gs[:, 0:1])
        stt(ph, ge[:], lg[:], m8[:, 1:2], ex[:], ALU.is_ge, ALU.mult, ["lg", "m8", "ex"], ["ge", "gs1"], accum=gs[:, 1:2])
        ph.add(DVE, lambda e: e.reciprocal(out=gs[:, 2:3], in_=gs[:, 1:2]), ["gs1"], ["gs2"])
        ts(ph, k.gates[:, t, :], ge[:], gs[:, 2:3], None, ALU.mult, None, ["ge", "gs2"], ["gates"])

    pend = None
    for t in range(t_first, NT):
        lc = 1 if t < 2 else 0
        norm_tile(k, ph, nb, t, layer, k.MOD[:, lc, 1, :], k.MOD[:, lc, 0, :], k.mT[:, :, t * 128:(t + 1) * 128], "mT",
                  extra_f32=(mT32[t % 2], "mT32_%d" % (t % 2)) if moe else None, mixed=True)
        if moe:
            if pend is not None:
                router(pend)
            pend = t
    if moe and pend is not None:
        router(pend)
    cp(ph, k.MG[:, 0, :], k.MOD[:, 0, 2, :], ["MOD"], ["MG"])
    cp(ph, k.MG[:, 1, :], k.MOD[:, 1, 2, :], ["MOD"], ["MG"])
    ph.emit()


def phase_ffn_experts(k, layer, moe, t_first, pre=None):
    ph = Phase(k.ctx)
    ps = k.ps
    i = layer // 2
    G = 4
    H = MOE_HIDDEN if moe else FF_HIDDEN
    nch = H // 128
    groups = []
    c0 = 0
    while c0 < nch:
        g = min(G, nch - c0)
        groups.append((c0, g))
        c0 += g
    if pre is None:
        wg = [ph.sb("wg0", [128, 8, G * 128], BF16), ph.sb("wg1", [128, 8, G * 128], BF16)]
        wu = [ph.sb("wu0", [128, 8, G * 128], BF16), ph.sb("wu1", [128, 8, G * 128], BF16)]
        wd = [ph.sb("wd0", [128, G, D], BF16), ph.sb("wd1", [128, G, D], BF16)]
    else:
        wg = [pre[0], ph.sb("wg1", [128, 8, G * 128], BF16)]
        wu = [pre[1], ph.sb("wu1", [128, 8, G * 128], BF16)]
        wd = [pre[2], ph.sb("wd1", [128, G, D], BF16)]
    hT = [ph.sb("hT0", [128, G, 512], BF16), ph.sb("hT1", [128, G, 512], BF16)]
    sg = [ph.sb("sg0", [128, 512], F32), ph.sb("sg1", [128, 512], F32)]
    ms(ph, k.acc[:], 0.0, ["acc"], eng=POOL)
    tblocks = []
    t = t_first
    while t < NT:
        n = min(4, NT - t)
        tblocks.append((t, n))
        t += n
    nexp = 8 if moe else 1
    work = [(e, c0, g) for e in range(nexp) for (c0, g) in groups]
    if moe:
        srcs = [(k.d["moe_w_gate"][i, e].rearrange("(k p) n -> p k n", p=128),
                 k.d["moe_w_up"][i, e].rearrange("(k p) n -> p k n", p=128),
                 k.d["moe_w_down"][i, e].rearrange("(c p) n -> p c n", p=128)) for e in range(8)]
    else:
        srcs = [(k.d["ff_w_gate"][i].rearrange("(k p) n -> p k n", p=128),
                 k.d["ff_w_up"][i].rearrange("(k p) n -> p k n", p=128),
                 k.d["ff_w_down"][i].rearrange("(c p) n -> p c n", p=128))]
    seq = [(wi_, tb) for wi_ in range(len(work)) for tb in range(len(tblocks))]
    cnt = {"ps": 0}

    def load(wi_):
        e, c0, g = work[wi_]
        s = wi_ % 2
        srcg, srcu, srcd = srcs[e]
        dma(ph, wg[s][:, :, 0:g * 128], srcg[:, :, c0 * 128:(c0 + g) * 128], [], ["wg%d" % s], eng=POOL)
        dma(ph, wu[s][:, :, 0:g * 128], srcu[:, :, c0 * 128:(c0 + g) * 128], [], ["wu%d" % s], eng=POOL)
        dma(ph, wd[s][:, 0:g, :], srcd[:, c0:c0 + g, :], [], ["wd%d" % s], eng=POOL)

    def gu(n):
        wi_, tb = seq[n]
        e, c0, g = work[wi_]
        s = wi_ % 2
        tb0, ntile = tblocks[tb]
        nq, q0 = ntile * 128, tb0 * 128
        hs = n % 2
        for c in range(g):
            bg = (cnt["ps"] % 2) * 2
            cnt["ps"] += 1
            for j in range(8):
                mm(ph, ps[bg][:, 0:nq], wg[s][:, j, c * 128:(c + 1) * 128], k.mT[:, j, q0:q0 + nq], j == 0, j == 7,
                   ["wg%d" % s, "mT"], ["ps%d" % bg])
            for j in range(8):
                mm(ph, ps[bg + 1][:, 0:nq], wu[s][:, j, c * 128:(c + 1) * 128], k.mT[:, j, q0:q0 + nq], j == 0, j == 7,
                   ["wu%d" % s, "mT"], ["ps%d" % (bg + 1)])
            sgt = sg[c % 2]
            act(ph, sgt[:, 0:nq], ps[bg][:, 0:nq], AF.Silu, ["ps%d" % bg], ["sg%d" % (c % 2)])
            tt(ph, hT[hs][:, c, 0:nq], sgt[:, 0:nq], ps[bg + 1][:, 0:nq], ALU.mult, ["sg%d" % (c % 2), "ps%d" % (bg + 1)],
               ["hT%d" % hs])

    hres = [ph.sb("hres0", [128, D], F32), ph.sb("hres1", [128, D], F32)]

    def down(n):
        wi_, tb = seq[n]
        e, c0, g = work[wi_]
        s = wi_ % 2
        tb0, ntile = tblocks[tb]
        hs = n % 2
        last_item = wi_ == len(work) - 1
        for ti in range(ntile):
            tI = tb0 + ti
            if last_item:
                dma(ph, hres[tI % 2][:], k.hsrc(layer, tI, mixed=True), [], ["hres%d" % (tI % 2)])
            for half in range(2):
                b = 4 + ((ti * 2 + half) % 4)
                for c in range(g):
                    mm(ph, ps[b][:, :], hT[hs][:, c, ti * 128:(ti + 1) * 128], wd[s][:, c, half * 512:(half + 1) * 512],
                       c == 0, c == g - 1, ["hT%d" % hs, "wd%d" % s], ["ps%d" % b])
                a_ = k.acc[:, tI, half * 512:(half + 1) * 512]
                if moe:
                    stt(ph, a_, ps[b][:, :], k.gates[:, tI, e:e + 1], a_, ALU.mult, ALU.add, ["ps%d" % b, "acc%d" % tI, "acc"],
                        ["acc%d" % tI])
                else:
                    tt(ph, a_, ps[b][:, :], a_, ALU.add, ["ps%d" % b, "acc%d" % tI, "acc"], ["acc%d" % tI])
            if last_item:
                lc_ = 1 if tI < 2 else 0
                hk_ = "hres%d" % (tI % 2)
                tt(ph, k.acc[:, tI, :], k.acc[:, tI, :], k.MG[:, lc_, :], ALU.mult, ["acc%d" % tI, "acc"], ["acc%d" % tI], eng=POOL)
                tt(ph, hres[tI % 2][:], hres[tI % 2][:], k.acc[:, tI, :], ALU.add, [hk_, "acc%d" % tI], [hk_])
                dma(ph, k.hbuf[tI * 128:(tI + 1) * 128, :], hres[tI % 2][:], [hk_], [])

    if pre is None:
        load(0)
    for n in range(len(seq)):
        wi_, tb = seq[n]
        gu(n)
        if n >= 1:
            down(n - 1)
        if tb == 0 and wi_ + 1 < len(work):
            load(wi_ + 1)
    down(len(seq) - 1)
    ph.emit()


def phase_ffn_resid(k, layer, t_first):
    ph = Phase(k.ctx)
    ht = [ph.sb("ht0", [128, D], F32), ph.sb("ht1", [128, D], F32)]
    for t in range(t_first, NT):
        lc = 1 if t < 2 else 0
        s = t % 2
        dma(ph, ht[s][:], k.hsrc(layer, t, mixed=True), [], ["ht%d" % s])
        tt(ph, k.acc[:, t, :], k.acc[:, t, :], k.MG[:, lc, :], ALU.mult, [], ["acc%d" % t], eng=POOL)
        tt(ph, ht[s][:], ht[s][:], k.acc[:, t, :], ALU.add, ["ht%d" % s, "acc%d" % t], ["ht%d" % s])
        dma(ph, k.hbuf[t * 128:(t + 1) * 128, :], ht[s][:], ["ht%d" % s], [])
    ph.emit()


def phase_final(k):
    ph = Phase(k.ctx)
    ht = [ph.sb("ht0", [128, D], F32), ph.sb("ht1", [128, D], F32)]
    sq = ph.sb("sq", [128, D], BF16)
    st = ph.sb("st", [128, 4], F32)
    fg = ph.sb("fg", [128, D], F32)
    dma(ph, fg[:], k.d["final_g"].partition_broadcast(128), [], ["fg"])
    for t in range(2, NT):
        s = t % 2
        dma(ph, ht[s][:], k.hbuf[t * 128:(t + 1) * 128, :], [], ["ht%d" % s])
        act(ph, sq[:], ht[s][:], AF.Square, ["ht%d" % s], ["sq", "st"], accum_out=st[:, 0:1])
        act(ph, st[:, 1:2], st[:, 0:1], AF.Sqrt, ["st"], ["st"], scale=1.0 / D, bias=k.eps_t[:, 0:1])
        ph.add(DVE, lambda e: e.reciprocal(out=st[:, 2:3], in_=st[:, 1:2]), ["st"], ["st"])
        stt(ph, ht[s][:], ht[s][:], st[:, 2:3], fg[:], ALU.mult, ALU.mult, ["ht%d" % s, "st", "fg"], ["ht%d" % s])
        dma(ph, k.d["out"][(t - 2) * 128:(t - 1) * 128, :], ht[s][:], ["ht%d" % s], [])
    ph.emit()


def phase_dump(k):
    ph = Phase(k.ctx)
    ht = [ph.sb("ht0", [128, D], F32), ph.sb("ht1", [128, D], F32)]
    for t in range(NT):
        s = t % 2
        dma(ph, ht[s][:], k.hbuf[t * 128:(t + 1) * 128, :], [], ["ht%d" % s])
        dma(ph, k.d["dbg"][t * 128:(t + 1) * 128, :], ht[s][:], ["ht%d" % s], [])
    ph.emit()


def phase_dump_sb(k, aps):
    ph = Phase(k.ctx)
    st = ph.sb("dst", [128, D], F32)
    for n, ap in enumerate(aps):
        w = ap.shape[-1]
        cp(ph, st[0:ap.shape[0], 0:w], ap, [], ["dst"])
        dma(ph, k.d["dbg"][n * 128:n * 128 + ap.shape[0], 0:w], st[0:ap.shape[0], 0:w], ["dst"], [])
    ph.emit()


INPUT_SHAPES = {
    "mod_w": [4, 1024, 6144], "mod_b": [4, 6144], "norm_mix_g": [4, 1024], "norm_ffn_g": [4, 1024], "final_g": [1024],
    "ev_w_in": [2, 1024, 2592], "a_ln_g": [2, 512], "a_ln_b": [2, 512], "a_ws": [2, 4, 128, 128], "a_bs": [2, 4, 128],
    "b_gate_w": [2, 2, 16, 256], "b_gate_b": [2, 2, 256], "b_norm_g": [2, 128], "ev_w_out": [2, 1024, 1024],
    "od_w_in": [2, 1024, 544], "c_q_norm_g": [2, 256], "c_w_uq": [2, 256, 1536], "c_kv_norm_g": [2, 256],
    "c_w_ukv": [2, 256, 2048], "od_w_out": [2, 1024, 1024],
    "ff_w_gate": [2, 1024, 2816], "ff_w_up": [2, 1024, 2816], "ff_w_down": [2, 2816, 1024],
    "moe_router": [2, 1024, 8], "moe_w_gate": [2, 8, 1024, 3584], "moe_w_up": [2, 8, 1024, 3584], "moe_w_down": [2, 8, 3584, 1024],
}
NCST = 2048


def make_consts():
    cst = np.zeros((128, NCST), np.float32)
    j = np.arange(128)[:, None]
    i = np.arange(128)[None, :]
    cst[:, 0:128] = np.eye(128)
    cst[:, 128:256] = (j <= i) * (-1.0 / 16)
    cst[:, 256:384] = (j >= i) * (-1.0 / 16)
    cst[:, 384:512] = (j > i) * (-1.0 / 16)
    cst[:, 512:640] = (j < i) * (-1.0 / 16)
    cst[:, 896:1024] = 1.0
    cst[:, 1024:1536] = np.tile((j <= i).astype(np.float32), (1, 4))
    cst[:, 1536:2048] = np.tile((j >= i).astype(np.float32), (1, 4))
    half = 16
    inv_freq = (10000.0 ** (-np.arange(0, half, 2, dtype=np.float32) / half)).astype(np.float32)
    tpos = np.arange(2048)
    rows = (tpos // 64).astype(np.float32)
    cols = (tpos % 64).astype(np.float32)
    C = np.zeros((32, 2048), np.float32)
    S = np.zeros((32, 2048), np.float32)
    for dd in range(32):
        g, jj = dd // 16, dd % 16
        f = jj % 8
        pos = rows if g == 0 else cols
        ang = (pos * inv_freq[f]).astype(np.float32)
        C[dd] = np.cos(ang)
        S[dd] = (-np.sin(ang)) if jj < 8 else np.sin(ang)
    return cst, C, S


def build(stop=None, dbg=False):
    nc = bass.Bass("TRN2", target_bir_lowering=False)
    k = K()
    k.nc = nc
    shapes = dict(INPUT_SHAPES)
    shapes.update({"x": [2048, D], "ctx": [256, D], "c": [D], "c_ctx": [D], "cst": [128, NCST],
                   "ropeC": [32, 2048], "ropeS": [32, 2048]})

    class LazyD(dict):
        def __missing__(self, n):
            ap = nc.dram_tensor(n, shapes[n], F32, kind="ExternalInput").ap()
            self[n] = ap
            return ap
    k.d = LazyD()
    k.d["out"] = nc.dram_tensor("out", [2048, D], F32, kind="ExternalOutput").ap()
    if dbg:
        k.d["dbg"] = nc.dram_tensor("dbg", [T, D], F32, kind="ExternalOutput").ap()
    k.hbuf = nc.dram_tensor("hbuf", [T, D], F32, kind="Internal").ap()

    def hsrc(layer, t, mixed=False):
        if layer == 0 and not mixed:
            return k.d["ctx"][t * 128:(t + 1) * 128, :] if t < 2 else k.d["x"][(t - 2) * 128:(t - 1) * 128, :]
        return k.hbuf[t * 128:(t + 1) * 128, :]
    k.hsrc = hsrc

    es = ExitStack()
    k.ctx = Ctx(nc, es)
    k.ps = [es.enter_context(nc.psum_tensor("psb%d" % b, [128, 512], F32)) for b in range(8)]
    k.cst = es.enter_context(nc.sbuf_tensor("cst_sb", [128, NCST], F32))
    k.eps_t = es.enter_context(nc.sbuf_tensor("eps_t", [128, 1], F32))
    k.condrep = es.enter_context(nc.sbuf_tensor("condrep", [128, 2, 8, 128], BF16))
    k.MG = es.enter_context(nc.sbuf_tensor("MG", [128, 2, D], F32))
    k.ident = k.cst[:, 0:128]
    k.TRI = [k.cst[:, 128:256], k.cst[:, 256:384]]
    k.STRI = [k.cst[:, 384:512], k.cst[:, 512:640]]
    k.ones = k.cst[:, 896:1024]
    k.one_t = k.cst[:, 896:897]
    k.MASK4 = [k.cst[:, 1024:1536], k.cst[:, 1536:2048]]

    def done(tag):
        return stop is not None and stop == tag

    phase_setup(k)
    if done("setup"):
        phase_dump_sb(k, [k.condrep[:, 0, 0, :], k.condrep[:, 1, 7, :], k.cst[:, 0:1024]])
        es.close()
        nc._used_inputs = [n for n in k.d if n not in ("out", "dbg")]
        return nc
    finished = False
    for layer in range(4):
        i = layer // 2
        need_ctx = layer < 3
        with ExitStack() as es2:
            if layer % 2 == 0:
                k.OB = es2.enter_context(nc.sbuf_tensor("OB_%d" % layer, [128, NT, 512], BF16))
                k.win = es2.enter_context(nc.sbuf_tensor("win_%d" % layer, [128, 8, 2592], BF16))
            else:
                k.KN = es2.enter_context(nc.sbuf_tensor("KN_%d" % layer, [128, 8, T], BF16))
                k.KR = es2.enter_context(nc.sbuf_tensor("KR_%d" % layer, [32, T], BF16))
                k.VA = es2.enter_context(nc.sbuf_tensor("VA_%d" % layer, [128, NT, 16, 64], BF16))
                k.CQT = es2.enter_context(nc.sbuf_tensor("CQT_%d" % layer, [128, 2, T], BF16))
            with nc.sbuf_tensor("MODa_%d" % layer, [128, 2, 3, D], F32) as MOD:
                k.MOD = MOD
                phase_mod(k, layer, 0)
                if done("mod%d" % layer):
                    phase_dump_sb(k, [k.MOD[:, a, b, :] for a in range(2) for b in range(3)])
                    nc._used_inputs = [n for n in k.d if n not in ("out", "dbg")]
                    return nc
                if layer % 2 == 0:
                    phase_even(k, layer, i, 1)
                    if done("evenB%d" % layer):
                        phase_dump_sb(k, [k.OB[:, t, :] for t in range(NT)])
                        nc._used_inputs = [n for n in k.d if n not in ("out", "dbg")]
                        return nc
                    phase_even(k, layer, i, 0)
                else:
                    phase_mla1(k, layer, i)
                    if done("mlaA%d" % layer):
                        phase_dump_sb(k, [k.CQT[:, 0, 0:1024], k.KN[:, 0, 0:1024], k.KR[:, 0:1024]])
                        nc._used_inputs = [n for n in k.d if n not in ("out", "dbg")]
                        return nc
            if layer % 2 == 1:
                phase_mla2(k, layer, i, need_ctx)
        if done("mix%d" % layer):
            break
        t_first = 0 if need_ctx else 2
        moe = layer % 2 == 1
        with ExitStack() as es3:
            k.mT = es3.enter_context(nc.sbuf_tensor("mT_%d" % layer, [128, 8, T], BF16))
            k.acc = es3.enter_context(nc.sbuf_tensor("acc_%d" % layer, [128, NT, D], F32))
            k.gates = es3.enter_context(nc.sbuf_tensor("gates_%d" % layer, [128, NT, 8], F32))
            pre = None
            if True:
                pre = [es3.enter_context(nc.sbuf_tensor("pre_wg_%d" % layer, [128, 8, 512], BF16)),
                       es3.enter_context(nc.sbuf_tensor("pre_wu_%d" % layer, [128, 8, 512], BF16)),
                       es3.enter_context(nc.sbuf_tensor("pre_wd_%d" % layer, [128, 4, D], BF16))]
            with nc.sbuf_tensor("MODb_%d" % layer, [128, 2, 3, D], F32) as MOD:
                k.MOD = MOD
                phase_mod(k, layer, 1)
                phase_ffn_norm(k, layer, moe, t_first, pre)
            phase_ffn_experts(k, layer, moe, t_first, pre)
        if done("ffn%d" % layer):
            break
    else:
        finished = True
    if finished or not dbg:
        phase_final(k)
    if dbg:
        phase_dump(k)
    es.close()
    nc._used_inputs = [n for n in k.d if n not in ("out", "dbg")]
    return nc


_CACHE = {}


def kernel(**inputs):
    cst, C, S = make_consts()
    if "nc" not in _CACHE:
        _CACHE["nc"] = build()
    nc = _CACHE["nc"]
    shared = {n: np.ascontiguousarray(inputs[n], dtype=np.float32) for n in INPUT_SHAPES}
    shared["c_ctx"] = np.ascontiguousarray(inputs["c_ctx"], dtype=np.float32)
    shared["cst"] = cst
    shared["ropeC"] = C
    shared["ropeS"] = S
    used = set(nc._used_inputs)
    shared = {n: v for n, v in shared.items() if n in used}
    in_maps = []
    for b in range(8):
        m = dict(shared)
        m["x"] = np.ascontiguousarray(inputs["x"][b], dtype=np.float32)
        m["ctx"] = np.ascontiguousarray(inputs["ctx"][b], dtype=np.float32)
        m["c"] = np.ascontiguousarray(inputs["c"][b], dtype=np.float32)
        in_maps.append(m)
    res = run_bass_kernel_spmd(nc, in_maps, core_ids=list(range(8)))
    return np.stack([np.asarray(r["out"], dtype=np.float32) for r in res.results], axis=0)
```
